# Optimizing a Trainium2 kernel written in Bass

```python
import math
import jax
import jax.numpy as jnp
from jax import lax
import numpy as np

D_MODEL = 2048
BATCH = 16
SEQ = 2048
DEPTH = 4

GRID_W = 64
CTX_LEN = 256

N_MIXERS = 3
N_ATTN_LAYERS = (DEPTH + 2) // 3
N_DN_LAYERS = (DEPTH + 1) // 3
N_HG_LAYERS = DEPTH // 3
N_MOD = 6

DEEPNORM_ALPHA = (2.0 * DEPTH) ** 0.25
DEEPNORM_BETA = (8.0 * DEPTH) ** -0.25
NORM_EPS = 1e-6

ATTN_HD = 128
ATTN_HQ = D_MODEL // ATTN_HD
ATTN_HKV = ATTN_HQ // 4
ATTN_GROUP = ATTN_HQ // ATTN_HKV
ATTN_Q_BLOCK = 128
ATTN_IN = (ATTN_HQ + 2 * ATTN_HKV) * ATTN_HD
ROPE_BASE = 10000.0
ROPE_FREQS = ATTN_HD // 4

DN_DK = 128
DN_DV = 128
DN_HQK = D_MODEL // DN_DK
DN_HV = 2 * DN_HQK
DN_QK_W = DN_HQK * DN_DK
DN_V_W = DN_HV * DN_DV
DN_CONV_CH = 2 * DN_QK_W + DN_V_W
DN_IN = DN_CONV_CH + DN_V_W + 4 * DN_HV
DN_CONV_W = 5
DN_CHUNK = 64

HG_DK = 128
HG_DV = 128
HG_H = D_MODEL // HG_DV
HG_W = HG_H * HG_DK
HG_VW = HG_H * HG_DV
HG_IN = 3 * HG_W + 2 * HG_VW
HG_CHUNK = 16

N_EXPERTS = 16
N_GROUPS = 4
EXPERTS_PER_GROUP = N_EXPERTS // N_GROUPS
TOP_K = 2
D_EXPERT = 1024
EXPERT_BLOCK = 256

kernel_name = 'hybrid_interleaved_dit_moe'


def _layer_norm(x, g, b):
    xf = x.astype(jnp.float32)
    xc = xf - xf.mean(-1, keepdims=True)
    var = jnp.mean(xc * xc, -1, keepdims=True)
    y = xc * lax.rsqrt(var + NORM_EPS) * g.astype(jnp.float32) + b.astype(jnp.float32)
    return y.astype(x.dtype)


def _rms_norm(x, g):
    xf = x.astype(jnp.float32)
    y = xf * lax.rsqrt(jnp.mean(xf * xf, -1, keepdims=True) + NORM_EPS) * g.astype(jnp.float32)
    return y.astype(x.dtype)


def _l2_normalize(x):
    return x * lax.rsqrt(jnp.sum(x * x, -1, keepdims=True) + NORM_EPS)


def _rev(t, on):
    return t[:, ::-1] if on else t


def _to_chunks(t, chunk):
    b, l, h = t.shape[:3]
    t = t.reshape(b, l // chunk, chunk, h, *t.shape[3:])
    return jnp.moveaxis(t, (1, 3), (0, 2))


def _from_chunks(t):
    n, b, h, c = t.shape[:4]
    return jnp.moveaxis(t, (0, 2), (1, 3)).reshape(b, n * c, h, *t.shape[4:])


def _axial_rope_tables(n_tokens):
    rows = n_tokens // GRID_W
    row = jnp.repeat(jnp.arange(rows), GRID_W).astype(jnp.float32)
    col = (jnp.arange(rows * GRID_W) % GRID_W).astype(jnp.float32)
    inv_freq = ROPE_BASE ** (-jnp.arange(ROPE_FREQS, dtype=jnp.float32) / ROPE_FREQS)
    ang = jnp.stack([row[:, None] * inv_freq, col[:, None] * inv_freq], axis=1)
    return jnp.cos(ang), jnp.sin(ang)


def _apply_axial_rope(x, cos, sin):
    b, l, h, _ = x.shape
    xr = x.astype(jnp.float32).reshape(b, l, h, 2, 2, ROPE_FREQS)
    x1, x2 = xr[..., 0, :], xr[..., 1, :]
    cs, sn = cos[None, :, None], sin[None, :, None]
    out = jnp.stack([x1 * cs - x2 * sn, x2 * cs + x1 * sn], axis=-2)
    return out.reshape(x.shape).astype(x.dtype)


def _gqa_attend(qb, k, v):
    s = jnp.einsum('bqkgd,bskd->bkgqs', qb, k).astype(jnp.float32) * (ATTN_HD ** -0.5)
    p = jax.nn.softmax(s, axis=-1).astype(v.dtype)
    return jnp.einsum('bkgqs,bskd->bqkgd', p, v)


def attention_mixer(h_lat, h_ctx, w_in, q_g, k_g, w_out, ctx_out):
    def project(h):
        b, l, _ = h.shape
        q, k, v = jnp.split(h @ w_in, [ATTN_HQ * ATTN_HD, (ATTN_HQ + ATTN_HKV) * ATTN_HD], axis=-1)
        q = _rms_norm(q.reshape(b, l, ATTN_HQ, ATTN_HD), q_g)
        k = _rms_norm(k.reshape(b, l, ATTN_HKV, ATTN_HD), k_g)
        return q, k, v.reshape(b, l, ATTN_HKV, ATTN_HD)

    q_l, k_l, v_l = project(h_lat)
    q_c, k_c, v_c = project(h_ctx)
    b, l, _ = h_lat.shape
    lc = h_ctx.shape[1]
    cos, sin = _axial_rope_tables(l)
    q_l = _apply_axial_rope(q_l, cos, sin)
    k_l = _apply_axial_rope(k_l, cos, sin)
    k_all = jnp.concatenate([k_l, k_c], axis=1)
    v_all = jnp.concatenate([v_l, v_c], axis=1)
    n_blk = l // ATTN_Q_BLOCK
    q_blocks = q_l.reshape(b, n_blk, ATTN_Q_BLOCK, ATTN_HKV, ATTN_GROUP, ATTN_HD).swapaxes(0, 1)
    o_l = lax.map(lambda qb: _gqa_attend(qb, k_all, v_all), q_blocks)
    y_l = o_l.swapaxes(0, 1).reshape(b, l, ATTN_HQ * ATTN_HD) @ w_out
    y_c = None
    if ctx_out:
        o_c = _gqa_attend(q_c.reshape(b, lc, ATTN_HKV, ATTN_GROUP, ATTN_HD), k_c, v_c)
        y_c = o_c.reshape(b, lc, ATTN_HQ * ATTN_HD) @ w_out
    return y_l, y_c


def _centred_depthwise_conv(x, w):
    width = w.shape[0]
    pad = width // 2
    l = x.shape[1]
    xp = jnp.pad(x, ((0, 0), (pad, pad), (0, 0)))
    return sum(xp[:, j:j + l] * w[j] for j in range(width))


def _gated_delta_rule(q, k, v, g, beta, s0):
    c = DN_CHUNK
    dv = v.shape[-1]
    qc, kc, vc = _to_chunks(q, c), _to_chunks(k, c), _to_chunks(v, c)
    gc = jnp.cumsum(_to_chunks(g, c), axis=-1)
    bc = _to_chunks(beta, c)
    idx = jnp.arange(c)
    lower_incl = idx[:, None] >= idx[None, :]
    strict = idx[:, None] > idx[None, :]
    decay = jnp.exp(jnp.where(lower_incl, gc[..., :, None] - gc[..., None, :], -jnp.inf))
    kb = kc * bc[..., None]
    m = jnp.where(strict, jnp.einsum('nbhid,nbhjd->nbhij', kb, kc) * decay, 0.0)
    a = m + jnp.eye(c, dtype=m.dtype)
    rhs = jnp.concatenate([vc * bc[..., None], kb * jnp.exp(gc)[..., None]], axis=-1)
    sol = lax.linalg.triangular_solve(a, rhs, left_side=True, lower=True, unit_diagonal=True)
    u, w = sol[..., :dv], sol[..., dv:]
    qk = jnp.where(lower_incl, jnp.einsum('nbhid,nbhjd->nbhij', qc, kc) * decay, 0.0)
    q_dec = qc * jnp.exp(gc)[..., None]
    k_dec = kc * jnp.exp(gc[..., -1:] - gc)[..., None]
    g_last = jnp.exp(gc[..., -1])

    def step(s, xs):
        u_n, w_n, qk_n, qd_n, kd_n, gl_n = xs
        v_new = u_n - jnp.einsum('bhck,bhkv->bhcv', w_n, s)
        o_n = jnp.einsum('bhck,bhkv->bhcv', qd_n, s) + jnp.einsum('bhij,bhjv->bhiv', qk_n, v_new)
        s = s * gl_n[..., None, None] + jnp.einsum('bhck,bhcv->bhkv', kd_n, v_new)
        return s, o_n

    s_final, o = lax.scan(step, s0, (u, w, qk, q_dec, k_dec, g_last))
    return _from_chunks(o), s_final


def deltanet_mixer(h_lat, h_ctx, w_in, conv_w, a_log, dt_bias, norm_g, w_out, ctx_out):
    def project(h):
        b, l, _ = h.shape
        p = h @ w_in
        qkv = jax.nn.silu(_centred_depthwise_conv(p[..., :DN_CONV_CH], conv_w)).astype(jnp.float32)
        q = qkv[..., :DN_QK_W].reshape(b, l, DN_HQK, DN_DK)
        k = qkv[..., DN_QK_W:2 * DN_QK_W].reshape(b, l, DN_HQK, DN_DK)
        v = qkv[..., 2 * DN_QK_W:].reshape(b, l, DN_HV, DN_DV)
        q = jnp.repeat(_l2_normalize(q) * (DN_DK ** -0.5), DN_HV // DN_HQK, axis=2)
        k = jnp.repeat(_l2_normalize(k), DN_HV // DN_HQK, axis=2)
        z = p[..., DN_CONV_CH:DN_CONV_CH + DN_V_W]
        ba = p[..., DN_CONV_CH + DN_V_W:].astype(jnp.float32).reshape(b, l, 2, 2, DN_HV)
        return q, k, v, z, ba

    q_l, k_l, v_l, z_l, ba_l = project(h_lat)
    q_c, k_c, v_c, z_c, ba_c = project(h_ctx)
    b = h_lat.shape[0]
    a_log = a_log.astype(jnp.float32)
    dt_bias = dt_bias.astype(jnp.float32)

    def gates(ba, d):
        beta = jax.nn.sigmoid(ba[:, :, 0, d])
        g = -jnp.exp(a_log[d]) * jax.nn.softplus(ba[:, :, 1, d] + dt_bias[d])
        return g, beta

    outs_l, outs_c = [], []
    for d in range(2):
        r = d == 1
        g_c, b_c = gates(ba_c, d)
        g_l, b_l = gates(ba_l, d)
        s0 = jnp.zeros((b, DN_HV, DN_DK, DN_DV), jnp.float32)
        o_c, s_ctx = _gated_delta_rule(_rev(q_c, r), _rev(k_c, r), _rev(v_c, r), _rev(g_c, r), _rev(b_c, r), s0)
        o_l, _ = _gated_delta_rule(_rev(q_l, r), _rev(k_l, r), _rev(v_l, r), _rev(g_l, r), _rev(b_l, r), s_ctx)
        outs_c.append(_rev(o_c, r))
        outs_l.append(_rev(o_l, r))

    def readout(o, z):
        bb, l = o.shape[:2]
        o = _rms_norm(o, norm_g) * jax.nn.silu(z.astype(jnp.float32)).reshape(bb, l, DN_HV, DN_DV)
        return o.reshape(bb, l, DN_V_W).astype(w_out.dtype) @ w_out

    y_l = readout(outs_l[0] + outs_l[1], z_l)
    y_c = readout(outs_c[0] + outs_c[1], z_c) if ctx_out else None
    return y_l, y_c


def _gla_chunked(q, k, v, logf, s0):
    c = HG_CHUNK
    qc, kc, vc = _to_chunks(q, c), _to_chunks(k, c), _to_chunks(v, c)
    gcum = jnp.cumsum(_to_chunks(logf, c), axis=-2)
    q_dec = qc * jnp.exp(gcum)
    k_inv = kc * jnp.exp(-gcum)
    idx = jnp.arange(c)
    lower_incl = idx[:, None] >= idx[None, :]
    intra = jnp.where(lower_incl, jnp.einsum('nbhik,nbhjk->nbhij', q_dec, k_inv), 0.0)
    o_intra = jnp.einsum('nbhij,nbhjv->nbhiv', intra, vc)
    k_dec = kc * jnp.exp(gcum[..., -1:, :] - gcum)
    f_last = jnp.exp(gcum[..., -1, :])

    def step(s, xs):
        oi, qd, kd, vn, fl = xs
        o = oi + jnp.einsum('bhck,bhkv->bhcv', qd, s)
        s = s * fl[..., None] + jnp.einsum('bhck,bhcv->bhkv', kd, vn)
        return s, o

    s_final, o = lax.scan(step, s0, (o_intra, q_dec, k_dec, vc, f_last))
    return _from_chunks(o), s_final


def hgrn2_mixer(h_lat, h_ctx, w_in, lower_bound, norm_g, w_out, ctx_out):
    lb = lower_bound.astype(jnp.float32)

    def project(h):
        b, l, _ = h.shape
        p = (h @ w_in).astype(jnp.float32)
        q, i, gate, f = jnp.split(p, [HG_W, HG_W + HG_VW, HG_W + 2 * HG_VW], axis=-1)
        fg = lb + (1.0 - lb) * jax.nn.sigmoid(f.reshape(b, l, 2, HG_W))
        keys = (1.0 - fg).reshape(b, l, 2, HG_H, HG_DK)
        logf = jnp.log(fg).reshape(b, l, 2, HG_H, HG_DK)
        return q.reshape(b, l, HG_H, HG_DK), i.reshape(b, l, HG_H, HG_DV), gate, keys, logf

    q_l, i_l, gt_l, k_l, lf_l = project(h_lat)
    q_c, i_c, gt_c, k_c, lf_c = project(h_ctx)
    b = h_lat.shape[0]
    outs_l, outs_c = [], []
    for d in range(2):
        r = d == 1
        s0 = jnp.zeros((b, HG_H, HG_DK, HG_DV), jnp.float32)
        o_c, s_ctx = _gla_chunked(_rev(q_c, r), _rev(k_c[:, :, d], r), _rev(i_c, r), _rev(lf_c[:, :, d], r), s0)
        o_l, _ = _gla_chunked(_rev(q_l, r), _rev(k_l[:, :, d], r), _rev(i_l, r), _rev(lf_l[:, :, d], r), s_ctx)
        outs_c.append(_rev(o_c, r))
        outs_l.append(_rev(o_l, r))

    def readout(o, gate):
        bb, l = o.shape[:2]
        o = _rms_norm(o, norm_g).reshape(bb, l, HG_VW) * jax.nn.sigmoid(gate)
        return o.astype(w_out.dtype) @ w_out

    y_l = readout(outs_l[0] + outs_l[1], gt_l)
    y_c = readout(outs_c[0] + outs_c[1], gt_c) if ctx_out else None
    return y_l, y_c


def _grouped_moe(h, router_w, router_b, w_gate, w_up, w_down):
    n_tok, d = h.shape
    scores = jax.nn.sigmoid(h.astype(jnp.float32) @ router_w.astype(jnp.float32))
    sel = (scores + router_b.astype(jnp.float32)).reshape(n_tok, N_GROUPS, EXPERTS_PER_GROUP)
    group_score = lax.top_k(sel, TOP_K)[0].sum(-1)
    group = jnp.argmax(group_score, axis=-1)
    in_group = jnp.take_along_axis(sel, group[:, None, None], axis=1)[:, 0]
    local = lax.top_k(in_group, TOP_K)[1]
    expert = (group[:, None] * EXPERTS_PER_GROUP + local).astype(jnp.int32)
    gate = jnp.take_along_axis(scores, expert, axis=1)
    gate = (gate / gate.sum(-1, keepdims=True)).astype(h.dtype)

    n_pairs = n_tok * TOP_K
    e_flat = expert.reshape(-1)
    order = jnp.argsort(e_flat)
    e_sorted = e_flat[order]
    tok_sorted = (order // TOP_K).astype(jnp.int32)
    gate_sorted = gate.reshape(-1)[order]
    counts = jnp.bincount(e_flat, length=N_EXPERTS)
    padded = (counts + EXPERT_BLOCK - 1) // EXPERT_BLOCK * EXPERT_BLOCK
    start = jnp.cumsum(counts) - counts
    pend = jnp.cumsum(padded)
    pstart = pend - padded
    dest = pstart[e_sorted] + jnp.arange(n_pairs) - start[e_sorted]
    n_blocks = -(-(n_pairs + N_EXPERTS * (EXPERT_BLOCK - 1)) // EXPERT_BLOCK)
    n_rows = n_blocks * EXPERT_BLOCK
    tok_buf = jnp.full((n_rows,), n_tok, jnp.int32).at[dest].set(tok_sorted)
    gate_buf = jnp.zeros((n_rows,), h.dtype).at[dest].set(gate_sorted)
    block_expert = jnp.minimum(jnp.searchsorted(pend, jnp.arange(n_blocks) * EXPERT_BLOCK, side='right'), N_EXPERTS - 1)
    h_pad = jnp.concatenate([h, jnp.zeros((1, d), h.dtype)], axis=0)
    xb = h_pad[tok_buf].reshape(n_blocks, EXPERT_BLOCK, d)

    def expert_ffn(args):
        xe, e = args
        return (jax.nn.silu(xe @ w_gate[e]) * (xe @ w_up[e])) @ w_down[e]

    yb = lax.map(expert_ffn, (xb, block_expert)).reshape(n_rows, d)
    out = jnp.zeros((n_tok + 1, d), h.dtype).at[tok_buf].add(yb * gate_buf[:, None])
    return out[:n_tok]


def setup_inputs(seed: int = 0) -> dict:
    key = jax.random.key(seed)
    keys = iter(jax.random.split(key, 40))
    f32 = jnp.float32

    def nrm(shape, scale):
        return jax.random.normal(next(keys), shape, f32) * scale

    def gain(shape):
        return 1.0 + nrm(shape, 0.1)

    a_decay = jax.random.uniform(next(keys), (N_DN_LAYERS, 2, DN_HV), f32, 1.0, 16.0)
    dt = jnp.exp(jax.random.uniform(next(keys), (N_DN_LAYERS, 2, DN_HV), f32, math.log(1e-3), math.log(1e-1)))
    return {
        'x': nrm((BATCH, SEQ, D_MODEL), 1.0),
        'c': nrm((BATCH, D_MODEL), 1.0),
        'ctx': nrm((BATCH, CTX_LEN, D_MODEL), 1.0),
        'c_ctx': nrm((D_MODEL,), 1.0),
        'ada_w': nrm((DEPTH, D_MODEL, N_MOD * D_MODEL), 0.5 * D_MODEL ** -0.5),
        'ada_b': nrm((DEPTH, N_MOD * D_MODEL), 0.02),
        'ln_g': gain((DEPTH, 2, D_MODEL)),
        'ln_b': nrm((DEPTH, 2, D_MODEL), 0.02),
        'attn_w_in': nrm((N_ATTN_LAYERS, D_MODEL, ATTN_IN), D_MODEL ** -0.5),
        'attn_q_g': gain((N_ATTN_LAYERS, ATTN_HD)),
        'attn_k_g': gain((N_ATTN_LAYERS, ATTN_HD)),
        'attn_w_out': nrm((N_ATTN_LAYERS, ATTN_HQ * ATTN_HD, D_MODEL), DEEPNORM_BETA * (ATTN_HQ * ATTN_HD) ** -0.5),
        'dn_w_in': nrm((N_DN_LAYERS, D_MODEL, DN_IN), D_MODEL ** -0.5),
        'dn_conv': nrm((N_DN_LAYERS, DN_CONV_W, DN_CONV_CH), DN_CONV_W ** -0.5),
        'dn_a_log': jnp.log(a_decay),
        'dn_dt_bias': dt + jnp.log(-jnp.expm1(-dt)),
        'dn_norm_g': gain((N_DN_LAYERS, DN_DV)),
        'dn_w_out': nrm((N_DN_LAYERS, DN_V_W, D_MODEL), DEEPNORM_BETA * DN_V_W ** -0.5),
        'hg_w_in': nrm((N_HG_LAYERS, D_MODEL, HG_IN), D_MODEL ** -0.5),
        'hg_lb': gain((2, DEPTH, HG_W)),
        'hg_norm_g': gain((N_HG_LAYERS, HG_DV)),
        'hg_w_out': nrm((N_HG_LAYERS, HG_VW, D_MODEL), DEEPNORM_BETA * HG_VW ** -0.5),
        'router_w': nrm((D_MODEL, N_EXPERTS), D_MODEL ** -0.5),
        'router_b': nrm((N_EXPERTS,), 0.01),
        'moe_w_gate': nrm((DEPTH, N_EXPERTS, D_MODEL, D_EXPERT), D_MODEL ** -0.5),
        'moe_w_up': nrm((DEPTH, N_EXPERTS, D_MODEL, D_EXPERT), D_MODEL ** -0.5),
        'moe_w_down': nrm((DEPTH, N_EXPERTS, D_EXPERT, D_MODEL), DEEPNORM_BETA * D_EXPERT ** -0.5),
    }


def reference(x, c, ctx, c_ctx, ada_w, ada_b, ln_g, ln_b, attn_w_in, attn_q_g, attn_k_g, attn_w_out,
              dn_w_in, dn_conv, dn_a_log, dn_dt_bias, dn_norm_g, dn_w_out,
              hg_w_in, hg_lb, hg_norm_g, hg_w_out, router_w, router_b, moe_w_gate, moe_w_up, moe_w_down):
    b, l, d = x.shape
    lc = ctx.shape[1]
    lb_cum = jnp.cumsum(jax.nn.softmax(hg_lb.astype(jnp.float32), axis=1), axis=1)
    lb_cum = lb_cum - lb_cum[:, :1]
    for i in range(DEPTH):
        last = i == DEPTH - 1
        mod_l = (jax.nn.silu(c) @ ada_w[i] + ada_b[i]).reshape(b, N_MOD, 1, d)
        mod_c = (jax.nn.silu(c_ctx) @ ada_w[i] + ada_b[i]).reshape(N_MOD, d)
        h_l = x * (1.0 + mod_l[:, 1]) + mod_l[:, 0]
        h_c = ctx * (1.0 + mod_c[1]) + mod_c[0]
        kind, j = i % N_MIXERS, i // N_MIXERS
        if kind == 0:
            y_l, y_c = attention_mixer(h_l, h_c, attn_w_in[j], attn_q_g[j], attn_k_g[j], attn_w_out[j], not last)
        elif kind == 1:
            y_l, y_c = deltanet_mixer(h_l, h_c, dn_w_in[j], dn_conv[j], dn_a_log[j], dn_dt_bias[j],
                                      dn_norm_g[j], dn_w_out[j], not last)
        else:
            y_l, y_c = hgrn2_mixer(h_l, h_c, hg_w_in[j], lb_cum[:, i], hg_norm_g[j], hg_w_out[j], not last)
        x = _layer_norm(DEEPNORM_ALPHA * x + mod_l[:, 2] * y_l, ln_g[i, 0], ln_b[i, 0])
        h2_l = x * (1.0 + mod_l[:, 4]) + mod_l[:, 3]
        if last:
            f_l = _grouped_moe(h2_l.reshape(b * l, d), router_w, router_b,
                               moe_w_gate[i], moe_w_up[i], moe_w_down[i]).reshape(b, l, d)
        else:
            ctx = _layer_norm(DEEPNORM_ALPHA * ctx + mod_c[2] * y_c, ln_g[i, 0], ln_b[i, 0])
            h2_c = ctx * (1.0 + mod_c[4]) + mod_c[3]
            tokens = jnp.concatenate([h2_c.reshape(b * lc, d), h2_l.reshape(b * l, d)], axis=0)
            f = _grouped_moe(tokens, router_w, router_b, moe_w_gate[i], moe_w_up[i], moe_w_down[i])
            f_c = f[:b * lc].reshape(b, lc, d)
            f_l = f[b * lc:].reshape(b, l, d)
            ctx = _layer_norm(DEEPNORM_ALPHA * ctx + mod_c[5] * f_c, ln_g[i, 1], ln_b[i, 1])
        x = _layer_norm(DEEPNORM_ALPHA * x + mod_l[:, 5] * f_l, ln_g[i, 1], ln_b[i, 1])
    return x
```

```python
import numpy as np
import concourse.bass as bass
import concourse.mybir as mybir
from concourse.bass_utils import run_bass_kernel_spmd

F32 = mybir.dt.float32
BF16 = mybir.dt.bfloat16
I32 = mybir.dt.int32
AF = mybir.ActivationFunctionType
ALU = mybir.AluOpType
AX = mybir.AxisListType

ENGS = ['pe', 'act', 'dve', 'pool', 'sp']
NDS = 24
DRAM_NAMES = set()


class Sched:
    def __init__(self, nc):
        self.nc = nc
        self.eng = dict(pe=nc.tensor, act=nc.scalar, dve=nc.vector, pool=nc.gpsimd, sp=nc.sync)
        self.sems = {}
        self.cnt = {}
        for e in ENGS:
            self.sems[e] = nc.alloc_semaphore("sem_" + e)
            self.cnt[e] = 0
        self.dslots = {}
        for q in ['sp', 'pool', 'act']:
            self.dslots[q] = []
            for i in range(NDS):
                sid = "d_%s_%d" % (q, i)
                self.sems[sid] = nc.alloc_semaphore(sid)
                self.cnt[sid] = 0
                self.dslots[q].append(sid)
        self.dnext = {q: 0 for q in self.dslots}
        self.seen = {e: {} for e in ENGS}
        self.res = {}
        self.n_inst = 0
        self.n_wait = 0
        self.out_events = []

    def _need(self, e, need, sid, val):
        if val <= 0:
            return
        if sid == e and e == 'pe':
            return
        if self.seen[e].get(sid, 0) >= val:
            return
        if need.get(sid, 0) < val:
            need[sid] = val

    def _collect(self, e, reads, writes, par=False):
        need = {}
        for k in reads:
            r = self.res.get(k)
            if r is None:
                continue
            for sid, val in r['w'].items():
                self._need(e, need, sid, val)
            if k.startswith('psb'):
                for sid, val in r['r'].items():
                    if sid != e:
                        self._need(e, need, sid, val)
        for k in writes:
            r = self.res.get(k)
            if r is None:
                continue
            if not par:
                for sid, val in r['w'].items():
                    self._need(e, need, sid, val)
            for sid, val in r['r'].items():
                self._need(e, need, sid, val)
        return need

    def _emit_waits(self, e, need):
        eng = self.eng[e]
        for sid, val in need.items():
            mult = 16 if sid.startswith('d_') else 1
            eng.wait_ge(self.sems[sid], val * mult)
            self.seen[e][sid] = val
            self.n_wait += 1

    def _record(self, ev, reads, writes, par=False):
        sid, val = ev
        for k in reads:
            r = self.res.setdefault(k, {'w': {}, 'r': {}})
            if r['r'].get(sid, 0) < val:
                r['r'][sid] = val
        for k in writes:
            if par:
                r = self.res.setdefault(k, {'w': {}, 'r': {}})
                if r['w'].get(sid, 0) < val:
                    r['w'][sid] = val
            else:
                self.res[k] = {'w': {sid: val}, 'r': {}}

    @staticmethod
    def keys_of(aps):
        ks = []
        for a in aps:
            if a is None:
                continue
            if isinstance(a, str) or isinstance(a, tuple):
                ks.append(a)
            elif hasattr(a, 'tensor'):
                ks.append(a.tensor.name)
            elif hasattr(a, 'name'):
                ks.append(a.name)
        return ks

    def op(self, e, fn, reads=(), writes=()):
        reads = self.keys_of(reads)
        writes = self.keys_of(writes)
        need = self._collect(e, reads, writes)
        self._emit_waits(e, need)
        inst = fn(self.eng[e])
        self.cnt[e] += 1
        inst.then_inc(self.sems[e], 1)
        self._record((e, self.cnt[e]), reads, writes)
        self.n_inst += 1
        return inst

    def dma(self, q, out, in_, reads=None, writes=None, is_output=False, par=None, **kw):
        reads = self.keys_of([in_] if reads is None else reads)
        writes = self.keys_of([out] if writes is None else writes)
        if par is None:
            par = all(k in DRAM_NAMES for k in writes)
        slot = self.dslots[q][self.dnext[q]]
        self.dnext[q] = (self.dnext[q] + 1) % NDS
        need = self._collect(q, reads, writes, par=par)
        self._need(q, need, slot, self.cnt[slot])
        self._emit_waits(q, need)
        inst = self.eng[q].dma_start(out=out, in_=in_, **kw)
        self.cnt[slot] += 1
        inst.then_inc(self.sems[slot], 16)
        ev = (slot, self.cnt[slot])
        self._record(ev, reads, writes, par=par)
        if is_output:
            self.out_events.append(ev)
        self.n_inst += 1
        return inst

    def barrier(self):
        for e in ENGS:
            need = {}
            for sid, c in self.cnt.items():
                if c > 0:
                    self._need(e, need, sid, c)
            self._emit_waits(e, need)

    def finish(self):
        need = {}
        for sid, c in self.cnt.items():
            if c > 0 and self.seen['sp'].get(sid, 0) < c:
                need[sid] = c
        self._emit_waits('sp', need)

    def mm(self, out, lhsT, rhs, start=True, stop=True, **kw):
        return self.op('pe', lambda t: t.matmul(out, lhsT, rhs, start=start, stop=stop, **kw),
                       reads=[lhsT, rhs], writes=[out])

    def transpose(self, out, in_, ident):
        return self.op('pe', lambda t: t.transpose(out, in_, ident), reads=[in_, ident], writes=[out])

    def act(self, out, in_, func, scale=None, bias=None, accum_out=None, e='act'):
        kw = {}
        rd = [in_]
        if scale is not None:
            kw['scale'] = scale
            if not isinstance(scale, (int, float)):
                rd.append(scale)
        if bias is not None:
            kw['bias'] = bias
            if not isinstance(bias, (int, float)):
                rd.append(bias)
        wr = [out]
        if accum_out is not None:
            kw['accum_out'] = accum_out
            wr.append(accum_out)
        return self.op('act', lambda t: t.activation(out, in_, func, **kw), reads=rd, writes=wr)

    def tt(self, e, out, in0, in1, op):
        return self.op(e, lambda t: t.tensor_tensor(out, in0, in1, op), reads=[in0, in1], writes=[out])

    def ts(self, e, out, in0, s1, op0, s2=None, op1=None, accum_out=None):
        rd = [in0]
        if not isinstance(s1, (int, float)):
            rd.append(s1)
        if s2 is not None and not isinstance(s2, (int, float)):
            rd.append(s2)
        kw = {}
        wr = [out]
        if accum_out is not None:
            kw['accum_out'] = accum_out
            wr.append(accum_out)
        if op1 is None:
            return self.op(e, lambda t: t.tensor_scalar(out, in0, s1, None, op0, **kw), reads=rd, writes=wr)
        return self.op(e, lambda t: t.tensor_scalar(out, in0, s1, s2, op0, op1, **kw), reads=rd, writes=wr)

    def stt(self, out, in0, scalar, in1, op0, op1, e='dve'):
        rd = [in0, in1]
        if not isinstance(scalar, (int, float)):
            rd.append(scalar)
        return self.op(e, lambda t: t.scalar_tensor_tensor(out, in0, scalar, in1, op0, op1),
                       reads=rd, writes=[out])

    def copy(self, e, out, in_):
        if e == 'act':
            return self.act(out, in_, AF.Copy)
        return self.op(e, lambda t: t.tensor_copy(out, in_), reads=[in_], writes=[out])

    def memset(self, e, ap, val):
        return self.op(e, lambda t: t.memset(ap, val), reads=[], writes=[ap])
import math
import numpy as np

D = 2048
LC = 256
L = 2048
T = LC + L
NT = T // 128
DEPTH = 4
ALPHA = (2.0 * DEPTH) ** 0.25
EPS = 1e-6
NMOD = 6


class G:
    moe_dbg = 0


def uname(g, base):
    g.uid += 1
    return "%s_%d" % (base, g.uid)


def sb(g, base, shape, dt):
    return g.nc.alloc_sbuf_tensor(uname(g, base), list(shape), dt)


class Phase:
    def __init__(self, g):
        self.g = g

    def __enter__(self):
        g = self.g
        g.S.barrier()
        self.stack = []
        g.phase_stack.append(self)
        return self

    def alloc(self, base, shape, dt):
        g = self.g
        cm = g.nc.sbuf_tensor(uname(g, base), list(shape), dt)
        t = cm.__enter__()
        self.stack.append(cm)
        return t

    def __exit__(self, *a):
        g = self.g
        g.S.barrier()
        for cm in reversed(self.stack):
            cm.__exit__(None, None, None)
        g.phase_stack.pop()
        return False


def init_globals(nc, nb):
    g = G()
    g.nc = nc
    g.S = Sched(nc)
    g.uid = 0
    g.nb = nb
    g.phase_stack = []
    g.ps = [nc.alloc_psum_tensor("psb%d" % i, [128, 512], F32) for i in range(8)]
    g.din = {}
    return g


def dram_in(g, name, shape, dt=F32):
    if name not in g.din:
        g.din[name] = g.nc.dram_tensor(name, list(shape), dt, kind="ExternalInput").ap()
    return g.din[name]


def dram_scratch(g, name, shape, dt=F32):
    DRAM_NAMES.add(name)
    return g.nc.dram_tensor(name, list(shape), dt, kind="Internal").ap()


def load_consts(g):
    nc, S = g.nc, g.S
    c = {}
    ident_d = dram_in(g, "c_ident", [128, 128])
    c['ident'] = nc.alloc_sbuf_tensor("ident", [128, 128], F32)
    S.dma('sp', c['ident'][:], ident_d[:, :])
    tri_d = dram_in(g, "c_triu", [128, 128])
    c['triu'] = nc.alloc_sbuf_tensor("triu", [128, 128], F32)
    S.dma('sp', c['triu'][:], tri_d[:, :])
    tris_d = dram_in(g, "c_trius", [128, 128])
    c['trius'] = nc.alloc_sbuf_tensor("trius", [128, 128], F32)
    S.dma('sp', c['trius'][:], tris_d[:, :])
    c['ones_b'] = nc.alloc_sbuf_tensor("ones_b", [128, 128], BF16)
    S.memset('dve', c['ones_b'][:], 1.0)
    c['ones_f'] = nc.alloc_sbuf_tensor("ones_f", [128, 128], F32)
    S.memset('dve', c['ones_f'][:], 1.0)
    g.c = c
    return c


def phase_mod(g, i, c_all, ada_w, ada_b, MOD):
    nc, S = g.nc, g.S
    R = g.nb + 1
    NW = NMOD * D
    with Phase(g) as ph:
        cT = ph.alloc("cT", [128, 16, R], F32)
        sT = ph.alloc("sT", [128, 16, R], BF16)
        for r in range(R):
            S.dma('sp', cT[:, :, r], c_all[r, :].rearrange("(kc p) -> p kc", p=128),
                  allow_slow_non_contiguous=True)
        S.act(sT[:], cT[:], AF.Silu)
        bias = ph.alloc("mbias", [R, NW], F32)
        S.dma('sp', bias[:], ada_b[None, :].broadcast_to([R, NW]))
        modt = ph.alloc("modt", [R, NW], F32)
        wb = [ph.alloc("mw%d" % k, [128, 16, 512], BF16) for k in range(2)]
        wv = ada_w.rearrange("(kc p) n -> p kc n", p=128)
        for n in range(NW // 512):
            w = wb[n % 2]
            for kc in range(16):
                S.dma('pool', w[:, kc, :], wv[:, kc, n * 512:(n + 1) * 512])
            ps = g.ps[n % 2]
            for kc in range(16):
                S.mm(ps[0:R, :], sT[:, kc, :], w[:, kc, :], start=(kc == 0), stop=(kc == 15))
            S.tt('dve', modt[:, n * 512:(n + 1) * 512], ps[0:R, :], bias[:, n * 512:(n + 1) * 512], ALU.add)
        for m in (1, 4):
            S.ts('dve', modt[:, m * D:(m + 1) * D], modt[:, m * D:(m + 1) * D], 1.0, ALU.add)
        S.dma('sp', MOD[:, :], modt[:])


def load_mod_cols(g, ph, MOD, r, m_scale, m_shift):
    S = g.S
    sc = ph.alloc("msc", [128, 16], F32)
    sh = ph.alloc("msh", [128, 16], F32)
    S.dma('sp', sc[:], MOD[r, m_scale * D:(m_scale + 1) * D].rearrange("(kc p) -> p kc", p=128),
          allow_slow_non_contiguous=True)
    S.dma('sp', sh[:], MOD[r, m_shift * D:(m_shift + 1) * D].rearrange("(kc p) -> p kc", p=128),
          allow_slow_non_contiguous=True)
    return sc, sh


def load_row_bcast(g, ph, name, row_ap, n=D):
    t = ph.alloc(name, [128, n], F32)
    g.S.dma('sp', t[:], row_ap[None, :].broadcast_to([128, n]))
    return t


def build_hT_tile(g, xt, hT_dst, sc, sh, psbase=0, hT32=None):
    S, c = g.S, g.c
    for gb in range(4):
        ps = g.ps[psbase + gb]
        for j in range(4):
            kc = gb * 4 + j
            S.transpose(ps[:, j * 128:(j + 1) * 128], xt[:, kc * 128:(kc + 1) * 128], c['ident'][:])
        for j in range(4):
            kc = gb * 4 + j
            src = ps[:, j * 128:(j + 1) * 128]
            dsts = [hT_dst] + ([hT32] if hT32 is not None else [])
            for dst in dsts:
                if gb % 2 == 0:
                    S.act(dst[:, kc, :], src, AF.Identity, scale=sc[:, kc:kc + 1], bias=sh[:, kc:kc + 1])
                else:
                    S.ts('dve', dst[:, kc, :], src, sc[:, kc:kc + 1], ALU.mult, sh[:, kc:kc + 1], ALU.add)


def load_w_bf16(g, wsb, w_dram, ncols, col0=0, chunk=1024):
    S = g.S
    K = w_dram.shape[0]
    wv = w_dram.rearrange("(kc p) n -> p kc n", p=128)
    for kc in range(K // 128):
        for c0 in range(0, ncols, chunk):
            cw = min(chunk, ncols - c0)
            S.dma('pool', wsb[:, kc, c0:c0 + cw], wv[:, kc, col0 + c0:col0 + c0 + cw])


def mod_row(b, t, nb):
    return nb if t < 2 else b


def resid_ln_tile(g, ph, bufs, ychunks, X_rows, gate_t, lng_t, lnb_t, q='sp'):
    S, nc = g.S, g.nc
    xt, zt, st, mv, rs = bufs
    S.dma(q, xt[:], X_rows)
    for dc in range(4):
        sl = slice(dc * 512, (dc + 1) * 512)
        S.tt('dve', zt[:, sl], ychunks[dc], gate_t[:, sl], ALU.mult)
    S.stt(zt[:], xt[:], ALPHA, zt[:], ALU.mult, ALU.add, e='dve')
    for dc in range(4):
        S.op('dve', lambda t, dc=dc: t.bn_stats(st[:, dc * 6:(dc + 1) * 6], zt[:, dc * 512:(dc + 1) * 512]),
             reads=[zt], writes=[st])
    S.op('dve', lambda t: t.bn_aggr(mv[:], st[:]), reads=[st], writes=[mv])
    S.ts('dve', rs[:, 0:1], mv[:, 1:2], EPS, ALU.add)
    S.act(rs[:, 0:1], rs[:, 0:1], AF.Sqrt)
    S.op('dve', lambda t: t.reciprocal(rs[:, 0:1], rs[:, 0:1]), reads=[rs], writes=[rs])
    S.ts('dve', zt[:], zt[:], mv[:, 0:1], ALU.subtract, rs[:, 0:1], ALU.mult)
    S.tt('pool', zt[:], zt[:], lng_t[:], ALU.mult)
    S.tt('pool', xt[:], zt[:], lnb_t[:], ALU.add)
    S.dma(q, X_rows, xt[:])


def alloc_ln_bufs(ph, k=""):
    xt = ph.alloc("lx" + k, [128, D], F32)
    zt = ph.alloc("lz" + k, [128, D], F32)
    st = ph.alloc("lst" + k, [128, 24], F32)
    mv = ph.alloc("lmv" + k, [128, 2], F32)
    rs = ph.alloc("lrs" + k, [128, 2], F32)
    return (xt, zt, st, mv, rs)

HD = 128
HQ = 16
HKV = 4
ATT_SCALE = HD ** -0.5


def phase_proj_tm(g, i, X, MOD, m_scale, m_shift, w_dram, ncols, P, tiles_per_b):
    S, nb = g.S, g.nb
    with Phase(g) as ph:
        wsb = ph.alloc("pw", [128, 16, ncols], BF16)
        load_w_bf16(g, wsb, w_dram, ncols)
        hT = [ph.alloc("phT%d" % k, [128, 16, 128], BF16) for k in range(2)]
        xt = [ph.alloc("pxt%d" % k, [128, D], F32) for k in range(2)]
        pt = [ph.alloc("ppt%d" % k, [128, ncols], F32) for k in range(2)]
        mods = {}
        for r in range(nb + 1):
            mods[r] = load_mod_cols(g, ph, MOD, r, m_scale, m_shift)
        n = 0
        for b in range(nb):
            for t in tiles_per_b:
                sc, sh = mods[mod_row(b, t, nb)]
                x_ = xt[n % 2]
                h_ = hT[n % 2]
                p_ = pt[n % 2]
                S.dma('sp', x_[:], X[b, t * 128:(t + 1) * 128, :])
                build_hT_tile(g, x_, h_, sc, sh, psbase=0)
                for c in range(ncols // 512):
                    ps = g.ps[4 + (c % 4)]
                    for kc in range(16):
                        S.mm(ps[:], h_[:, kc, :], wsb[:, kc, c * 512:(c + 1) * 512], start=(kc == 0), stop=(kc == 15))
                    if c % 2 == 0:
                        S.act(p_[:, c * 512:(c + 1) * 512], ps[:], AF.Copy)
                    else:
                        S.copy('dve', p_[:, c * 512:(c + 1) * 512], ps[:])
                S.dma('sp', P[b, t * 128:(t + 1) * 128, 0:ncols], p_[:])
                n += 1


def rms_heads(g, tmp, x3, nh, gb, sq, red):
    S = g.S
    S.tt('pool', sq[:, 0:nh, :], x3, x3, ALU.mult)
    S.op('dve', lambda t: t.tensor_reduce(red[:, 0:nh], sq[:, 0:nh, :], AX.X, ALU.add), reads=[sq], writes=[red])
    S.ts('dve', red[:, 0:nh], red[:, 0:nh], 1.0 / HD, ALU.mult, EPS, ALU.add)
    S.act(red[:, 0:nh], red[:, 0:nh], AF.Sqrt)
    S.op('dve', lambda t: t.reciprocal(red[:, 0:nh], red[:, 0:nh]), reads=[red], writes=[red])
    S.tt('dve', x3, x3, red[:, 0:nh].unsqueeze(2).broadcast_to([128, nh, HD]), ALU.mult)
    S.tt('pool', x3, x3, gb[:].unsqueeze(1).broadcast_to([128, nh, HD]), ALU.mult)


def rope_heads(g, x3, out3, nh, cs, sn, t1, t2):
    S = g.S
    xv = x3[:, 0:nh * HD].rearrange("p (h a s f) -> p h a s f", h=nh, a=2, s=2, f=32)
    ov = out3[:, 0:nh * HD].rearrange("p (h a s f) -> p h a s f", h=nh, a=2, s=2, f=32)
    csb = cs[:].rearrange("p (a f) -> p a f", a=2)
    snb = sn[:].rearrange("p (a f) -> p a f", a=2)
    t1v = t1[:, 0:nh * 64].rearrange("p (h a f) -> p h a f", h=nh, a=2, f=32)
    t2v = t2[:, 0:nh * 64].rearrange("p (h a f) -> p h a f", h=nh, a=2, f=32)
    x1 = xv[:, :, :, 0, :]
    x2 = xv[:, :, :, 1, :]
    csb4 = csb.unsqueeze(1).broadcast_to([128, nh, 2, 32])
    snb4 = snb.unsqueeze(1).broadcast_to([128, nh, 2, 32])
    S.tt('dve', t1v, x1, csb4, ALU.mult)
    S.tt('pool', t2v, x2, snb4, ALU.mult)
    S.tt('dve', ov[:, :, :, 0, :], t1v, t2v, ALU.subtract)
    S.tt('pool', t1v, x2, csb4, ALU.mult)
    S.tt('dve', t2v, x1, snb4, ALU.mult)
    S.tt('pool', ov[:, :, :, 1, :], t1v, t2v, ALU.add)


def phase_attn_core(g, i, X, MOD, P, wout_d, qg_d, kg_d, lng_d, lnb_d, cos_d, sin_d, last):
    S, nb, c = g.S, g.nb, g.c
    with Phase(g) as ph:
        wout = ph.alloc("awo", [128, 16, D], BF16)
        load_w_bf16(g, wout, wout_d, D)
        qgb = load_row_bcast(g, ph, "qgb", qg_d, HD)
        kgb = load_row_bcast(g, ph, "kgb", kg_d, HD)
        lng = load_row_bcast(g, ph, "lng", lng_d)
        lnb = load_row_bcast(g, ph, "lnb", lnb_d)
        gate_c = load_row_bcast(g, ph, "gatec", MOD[nb, 2 * D:3 * D])
        gate_l = ph.alloc("gatel", [128, D], F32)
        KT = ph.alloc("KT", [128, HKV, T], BF16)
        Vs = ph.alloc("Vs", [128, NT, HKV * HD], BF16)
        kv = ph.alloc("kv", [128, 2 * HKV * HD], F32)
        kr = ph.alloc("kr", [128, HKV * HD], F32)
        qx = ph.alloc("qx", [128, D], F32)
        qr = ph.alloc("qr", [128, D], F32)
        sq = ph.alloc("sq", [128, HQ, HD], F32)
        red = ph.alloc("red", [128, HQ], F32)
        t1 = ph.alloc("rt1", [128, HQ * 64], F32)
        t2 = ph.alloc("rt2", [128, HQ * 64], F32)
        cs = ph.alloc("cs", [128, 64], F32)
        sn = ph.alloc("sn", [128, 64], F32)
        QT = ph.alloc("QT", [128, HQ, 128], BF16)
        OT = ph.alloc("OT", [128, HQ, 128], BF16)
        PT = [ph.alloc("PT%d" % k, [128, 512], BF16) for k in range(3)]
        rec = ph.alloc("rec", [128, 512], F32)
        lnbufs = alloc_ln_bufs(ph)
        for b in range(nb):
            S.dma('sp', gate_l[:], MOD[b, 2 * D:3 * D][None, :].broadcast_to([128, D]))
            for t in range(NT):
                S.dma('sp', kv[:], P[b, t * 128:(t + 1) * 128, HQ * HD:HQ * HD + 2 * HKV * HD])
                k3 = kv[:, 0:HKV * HD].rearrange("p (h d) -> p h d", h=HKV)
                rms_heads(g, None, k3, HKV, kgb, sq, red)
                ksrc = kv
                if t >= 2:
                    S.dma('sp', cs[:], cos_d[(t - 2) * 128:(t - 1) * 128, :])
                    S.dma('sp', sn[:], sin_d[(t - 2) * 128:(t - 1) * 128, :])
                    rope_heads(g, kv, kr, HKV, cs, sn, t1, t2)
                    ksrc = kr
                ps = g.ps[t % 2]
                for h in range(HKV):
                    S.transpose(ps[:, h * 128:(h + 1) * 128], ksrc[:, h * HD:(h + 1) * HD], c['ident'][:])
                S.act(KT[:, :, t * 128:(t + 1) * 128], ps[:].rearrange("p (h d) -> p h d", h=HKV), AF.Copy)
                S.copy('dve', Vs[:, t, :], kv[:, HKV * HD:2 * HKV * HD])
            qtiles = list(range(2, NT)) if last else list(range(NT))
            for t in qtiles:
                is_ctx = t < 2
                S.dma('sp', qx[:], P[b, t * 128:(t + 1) * 128, 0:HQ * HD])
                q3 = qx[:].rearrange("p (h d) -> p h d", h=HQ)
                rms_heads(g, None, q3, HQ, qgb, sq, red)
                qsrc = qx
                if not is_ctx:
                    S.dma('sp', cs[:], cos_d[(t - 2) * 128:(t - 1) * 128, :])
                    S.dma('sp', sn[:], sin_d[(t - 2) * 128:(t - 1) * 128, :])
                    rope_heads(g, qx, qr, HQ, cs, sn, t1, t2)
                    qsrc = qr
                for gb in range(4):
                    ps = g.ps[gb % 2]
                    for j in range(4):
                        h = gb * 4 + j
                        S.transpose(ps[:, j * 128:(j + 1) * 128], qsrc[:, h * HD:(h + 1) * HD], c['ident'][:])
                    S.act(QT[:, gb * 4:(gb + 1) * 4, :], ps[:].rearrange("p (h d) -> p h d", h=4), AF.Copy)
                stiles = [0, 1] if is_ctx else list(range(NT))
                for kvh in range(HKV):
                    o_ps = g.ps[2]
                    d_ps = g.ps[3]
                    for si, s in enumerate(stiles):
                        s_ps = g.ps[4 + (si % 3)]
                        S.mm(s_ps[:], KT[:, kvh, s * 128:(s + 1) * 128], QT[:, kvh * 4:(kvh + 1) * 4, :])
                        p_ = PT[si % 3]
                        S.act(p_[:], s_ps[:], AF.Exp, scale=ATT_SCALE)
                        S.mm(o_ps[:], Vs[:, s, kvh * HD:(kvh + 1) * HD], p_[:], start=(si == 0), stop=(si == len(stiles) - 1))
                        S.mm(d_ps[:], c['ones_b'][:], p_[:], start=(si == 0), stop=(si == len(stiles) - 1))
                    S.op('dve', lambda tt_: tt_.reciprocal(rec[:], d_ps[:]), reads=[d_ps], writes=[rec])
                    S.tt('dve', OT[:, kvh * 4:(kvh + 1) * 4, :], o_ps[:].rearrange("p (h d) -> p h d", h=4),
                         rec[:].rearrange("p (h d) -> p h d", h=4), ALU.mult)
                ych = []
                for dc in range(4):
                    ps = g.ps[4 + dc] if False else g.ps[(dc % 2) + 0] if False else None
                banks = [0, 1, 7, 3]
                for dc in range(4):
                    ps = g.ps[banks[dc]]
                    for h in range(HQ):
                        S.mm(ps[:], OT[:, h, :], wout[:, h, dc * 512:(dc + 1) * 512], start=(h == 0), stop=(h == HQ - 1))
                    ych.append(ps[:])
                resid_ln_tile(g, ph, lnbufs, ych, X[b, t * 128:(t + 1) * 128, :],
                              gate_c if is_ctx else gate_l, lng, lnb)

NE = 16
DE = 1024
TG = 4
NQ = 4
FQ = DE // NQ
NFC = FQ // 128


PIECE = 16 * 2 * FQ + NFC * D


def phase_moe_precast(g, wg_d, wu_d, wd_d, Wb, experts=range(NE)):
    S = g.S
    with Phase(g) as ph:
        st = [ph.alloc("pcw%d" % k, [128, PIECE], BF16) for k in range(3)]
        n = 0
        for e in experts:
            wgv = wg_d[e].rearrange("(kc p) f -> p kc f", p=128)
            wuv = wu_d[e].rearrange("(kc p) f -> p kc f", p=128)
            wdv = wd_d[e].rearrange("(fc p) d -> p fc d", p=128)
            for hf in range(NQ):
                s_ = st[n % 3]
                n += 1
                wb = s_[:, 0:16 * 2 * FQ].rearrange("p (kc two f) -> p kc two f", kc=16, two=2)
                wdb = s_[:, 16 * 2 * FQ:PIECE].rearrange("p (fc d) -> p fc d", fc=NFC)
                for k4 in range(0, 16, 4):
                    S.dma('pool', wb[:, k4:k4 + 4, 0, :], wgv[:, k4:k4 + 4, hf * FQ:(hf + 1) * FQ], par=True)
                    S.dma('pool', wb[:, k4:k4 + 4, 1, :], wuv[:, k4:k4 + 4, hf * FQ:(hf + 1) * FQ], par=True)
                S.dma('pool', wdb[:, :, :], wdv[:, hf * NFC:(hf + 1) * NFC, :], par=True)
                S.dma('sp' if n % 2 == 0 else 'act', Wb[e * NQ + hf, :, :], s_[:])


def gating_tile(g, bufs, logits_ps, rbb, Gdst):
    S = g.S
    sc, sel, w = bufs
    S.act(sc[:], logits_ps, AF.Sigmoid)
    S.tt('dve', sel[:], sc[:], rbb[:], ALU.add)
    s4 = sel[:].rearrange("p (g e) -> p g e", g=4)
    x0, x1, x2, x3 = s4[:, :, 0], s4[:, :, 1], s4[:, :, 2], s4[:, :, 3]
    M1, m1, M2, m2 = w[:, 0:4], w[:, 4:8], w[:, 8:12], w[:, 12:16]
    A, Bm, Cm, s2, gs = w[:, 16:20], w[:, 20:24], w[:, 24:28], w[:, 28:32], w[:, 32:36]
    gmax, gmask, den = w[:, 36:37], w[:, 40:44], w[:, 44:45]
    S.tt('dve', M1, x0, x1, ALU.max)
    S.tt('dve', m1, x0, x1, ALU.min)
    S.tt('dve', M2, x2, x3, ALU.max)
    S.tt('dve', m2, x2, x3, ALU.min)
    S.tt('dve', A, M1, M2, ALU.max)
    S.tt('dve', Bm, M1, M2, ALU.min)
    S.tt('dve', Cm, m1, m2, ALU.max)
    S.tt('dve', s2, Bm, Cm, ALU.max)
    S.tt('dve', gs, A, s2, ALU.add)
    S.op('dve', lambda t: t.tensor_reduce(gmax, gs, AX.X, ALU.max), reads=[w], writes=[w])
    S.ts('dve', gmask, gs, gmax, ALU.is_ge)
    em = w[:, 48:64].rearrange("p (g e) -> p g e", g=4)
    S.tt('dve', em, s4, s2.unsqueeze(2).broadcast_to([128, 4, 4]), ALU.is_ge)
    S.tt('dve', em, em, gmask.unsqueeze(2).broadcast_to([128, 4, 4]), ALU.mult)
    S.tt('dve', w[:, 48:64], w[:, 48:64], sc[:], ALU.mult)
    S.op('dve', lambda t: t.tensor_reduce(den, w[:, 48:64], AX.X, ALU.add), reads=[w], writes=[w])
    S.op('dve', lambda t: t.reciprocal(den, den), reads=[w], writes=[w])
    S.ts('dve', Gdst, w[:, 48:64], den, ALU.mult)


def phase_moe(g, i, X, MOD, rw_d, rb_d, Wb, lng_d, lnb_d, tiles_per_b, experts=range(NE), dbg=0):
    S, nb, c = g.S, g.nb, g.c
    toks = [(b, t) for b in range(nb) for t in tiles_per_b]
    groups = [toks[k:k + TG] for k in range(0, len(toks), TG)]
    with Phase(g) as ph:
        rw32 = ph.alloc("rw32", [128, 16, NE], F32)
        S.dma('sp', rw32[:], rw_d.rearrange("(kc p) e -> p kc e", p=128))
        rbb = load_row_bcast(g, ph, "rbb", rb_d, NE)
        lng = load_row_bcast(g, ph, "mlng", lng_d)
        lnb = load_row_bcast(g, ph, "mlnb", lnb_d)
        gate_t = ph.alloc("mgate", [128, D], F32)
        mods = {}
        for r in range(nb + 1):
            mods[r] = load_mod_cols(g, ph, MOD, r, 4, 3)
        h2T = ph.alloc("h2T", [128, 16, TG * 128], BF16)
        h32 = ph.alloc("h32", [128, 16, 128], F32)
        Gt = ph.alloc("Gt", [128, TG, NE], F32)
        gb_ = (ph.alloc("gsc", [128, 16], F32), ph.alloc("gsel", [128, 16], F32), ph.alloc("gw", [128, 64], F32))
        acc = ph.alloc("acc", [128, TG, D], F32)
        actT = ph.alloc("actT", [128, 8, TG * 128], BF16)
        wst = [ph.alloc("wst%d" % k, [128, PIECE], BF16) for k in range(2)]
        sg = [ph.alloc("sg%d" % k, [128, TG * 128], F32) for k in range(2)]
        lnbufs = alloc_ln_bufs(ph)
        xt = lnbufs[0]
        nw = 0
        for grp in groups:
            ng = len(grp)
            ntok = ng * 128
            for ti, (b, t) in enumerate(grp):
                scm, shm = mods[mod_row(b, t, nb)]
                S.dma('sp', xt[:], X[b, t * 128:(t + 1) * 128, :])
                build_hT_tile(g, xt, h2T[:, :, ti * 128:(ti + 1) * 128], scm, shm, psbase=0, hT32=h32)
                lp = g.ps[4]
                for kc in range(16):
                    S.mm(lp[:, 0:NE], h32[:, kc, :], rw32[:, kc, :], start=(kc == 0), stop=(kc == 15))
                if dbg != 2:
                    gating_tile(g, gb_, lp[:, 0:NE], rbb, Gt[:, ti, :])
            first = True
            for e in experts:
                for hf in range(NQ):
                    w_ = wst[nw % 2]
                    wb = w_[:, 0:16 * 2 * FQ].rearrange("p (kc two f) -> p kc two f", kc=16, two=2)
                    wdb = w_[:, 16 * 2 * FQ:PIECE].rearrange("p (fc d) -> p fc d", fc=NFC)
                    nw += 1
                    S.dma('sp', w_[0:64, :], Wb[e * NQ + hf, 0:64, :], par=True)
                    S.dma('act', w_[64:128, :], Wb[e * NQ + hf, 64:128, :], par=True)
                    for fc in range(NFC):
                        g_ps = g.ps[(fc % 2) * 2]
                        u_ps = g.ps[(fc % 2) * 2 + 1]
                        for kc in range(16):
                            S.mm(g_ps[:, 0:ntok], wb[:, kc, 0, fc * 128:(fc + 1) * 128], h2T[:, kc, 0:ntok],
                                 start=(kc == 0), stop=(kc == 15))
                        for kc in range(16):
                            S.mm(u_ps[:, 0:ntok], wb[:, kc, 1, fc * 128:(fc + 1) * 128], h2T[:, kc, 0:ntok],
                                 start=(kc == 0), stop=(kc == 15))
                        s_ = sg[fc % 2]
                        S.act(s_[:, 0:ntok], g_ps[:, 0:ntok], AF.Silu)
                        S.tt('dve', actT[:, hf * NFC + fc, 0:ntok], s_[:, 0:ntok], u_ps[:, 0:ntok], ALU.mult)
                    for ti in range(ng):
                        for dc in range(4):
                            y_ps = g.ps[4 + dc]
                            for fc in range(NFC):
                                S.mm(y_ps[:], actT[:, hf * NFC + fc, ti * 128:(ti + 1) * 128],
                                     wdb[:, fc, dc * 512:(dc + 1) * 512], start=(fc == 0), stop=(fc == NFC - 1))
                            a_ = acc[:, ti, dc * 512:(dc + 1) * 512]
                            if first:
                                S.ts('dve', a_, y_ps[:], Gt[:, ti, e:e + 1], ALU.mult)
                            else:
                                S.stt(a_, y_ps[:], Gt[:, ti, e:e + 1], a_, ALU.mult, ALU.add)
                    first = False
            if dbg == 1:
                continue
            for ti, (b, t) in enumerate(grp):
                r = mod_row(b, t, nb)
                S.dma('sp', gate_t[:], MOD[r, 5 * D:6 * D][None, :].broadcast_to([128, D]))
                ych = [acc[:, ti, dc * 512:(dc + 1) * 512] for dc in range(4)]
                resid_ln_tile(g, ph, lnbufs, ych, X[b, t * 128:(t + 1) * 128, :], gate_t, lng, lnb)

PW = 12416
PROWS = 2312


def prow(t):
    return 2 + t * 128 if t < 2 else 262 + (t - 2) * 128


def phase_proj_blocks(g, X, MOD, m_scale, m_shift, w_dram, ntot, P, tiles_per_b, blk=3072):
    S, nb = g.S, g.nb
    for c0 in range(0, ntot, blk):
        ncols = min(blk, ntot - c0)
        with Phase(g) as ph:
            wsb = ph.alloc("pw", [128, 16, ncols], BF16)
            load_w_bf16(g, wsb, w_dram, ncols, col0=c0)
            hT = [ph.alloc("phT%d" % k, [128, 16, 128], BF16) for k in range(2)]
            xt = [ph.alloc("pxt%d" % k, [128, D], F32) for k in range(2)]
            pt = [ph.alloc("ppt%d" % k, [128, ncols], F32) for k in range(2)]
            mods = {}
            for r in range(nb + 1):
                mods[r] = load_mod_cols(g, ph, MOD, r, m_scale, m_shift)
            n = 0
            for b in range(nb):
                for t in tiles_per_b:
                    sc, sh = mods[mod_row(b, t, nb)]
                    x_ = xt[n % 2]
                    h_ = hT[n % 2]
                    p_ = pt[n % 2]
                    S.dma('sp', x_[:], X[b, t * 128:(t + 1) * 128, :])
                    build_hT_tile(g, x_, h_, sc, sh, psbase=0)
                    ci = 0
                    for cc in range(0, ncols, 512):
                        cw = min(512, ncols - cc)
                        ps = g.ps[4 + (ci % 4)]
                        for kc in range(16):
                            S.mm(ps[:, 0:cw], h_[:, kc, :], wsb[:, kc, cc:cc + cw], start=(kc == 0), stop=(kc == 15))
                        if ci % 2 == 0:
                            S.act(p_[:, cc:cc + cw], ps[:, 0:cw], AF.Copy)
                        else:
                            S.copy('dve', p_[:, cc:cc + cw], ps[:, 0:cw])
                        ci += 1
                    r0 = prow(t)
                    S.dma('sp', P[b, r0:r0 + 128, c0:c0 + ncols], p_[:, 0:ncols])
                    n += 1


def phase_readout(g, i, X, MOD, O, K, P, gate_col0, gate_func, ng_d, w_out_d, lng_d, lnb_d, last, Ypart):
    S, nb, c = g.S, g.nb, g.c
    nparts = K // 2048
    tiles = list(range(2, NT)) if last else list(range(NT))
    for part in range(nparts):
        with Phase(g) as ph:
            wsb = ph.alloc("rw", [128, 16, D], BF16)
            load_w_bf16(g, wsb, w_out_d[part * 2048:(part + 1) * 2048, :], D)
            ngb = load_row_bcast(g, ph, "ngb", ng_d, 128)
            oa = ph.alloc("roa", [128, D], F32)
            ob = ph.alloc("rob", [128, D], F32)
            sq = ph.alloc("rsq", [128, 16, 128], F32)
            red = ph.alloc("rred", [128, 16], F32)
            OT = ph.alloc("rOT", [128, 16, 128], BF16)
            one16 = ph.alloc("one16", [128, 16], F32)
            zero16 = ph.alloc("zero16", [128, 16], F32)
            S.memset('dve', one16[:], 1.0)
            S.memset('dve', zero16[:], 0.0)
            fin = part == nparts - 1
            if fin:
                lng = load_row_bcast(g, ph, "rlng", lng_d)
                lnb = load_row_bcast(g, ph, "rlnb", lnb_d)
                gate_c = load_row_bcast(g, ph, "rgc", MOD[nb, 2 * D:3 * D])
                gate_l = ph.alloc("rgl", [128, D], F32)
                lnbufs = alloc_ln_bufs(ph)
            if nparts > 1:
                ysb = ph.alloc("rys", [128, D], F32)
            for b in range(nb):
                if fin:
                    S.dma('sp', gate_l[:], MOD[b, 2 * D:3 * D][None, :].broadcast_to([128, D]))
                for t in tiles:
                    rows = slice(t * 128, (t + 1) * 128)
                    cols = slice(part * 2048, (part + 1) * 2048)
                    S.dma('sp', oa[:], O[0, b, rows, cols])
                    S.dma('sp', ob[:], O[1, b, rows, cols])
                    S.tt('dve', oa[:], oa[:], ob[:], ALU.add)
                    o3 = oa[:].rearrange("p (h d) -> p h d", h=16)
                    rms_heads(g, None, o3, 16, ngb, sq, red)
                    r0 = prow(t)
                    S.dma('sp', ob[:], P[b, r0:r0 + 128, gate_col0 + part * 2048:gate_col0 + (part + 1) * 2048])
                    S.act(ob[:], ob[:], gate_func)
                    S.tt('pool', oa[:], oa[:], ob[:], ALU.mult)
                    build_hT_tile(g, oa, OT, one16, zero16, psbase=0)
                    ych = []
                    for dc in range(4):
                        ps = g.ps[4 + dc]
                        for kc in range(16):
                            S.mm(ps[:], OT[:, kc, :], wsb[:, kc, dc * 512:(dc + 1) * 512], start=(kc == 0), stop=(kc == 15))
                        ych.append(ps[:])
                    if nparts > 1 and not fin:
                        for dc in range(4):
                            if dc % 2 == 0:
                                S.act(ysb[:, dc * 512:(dc + 1) * 512], ych[dc], AF.Copy)
                            else:
                                S.copy('dve', ysb[:, dc * 512:(dc + 1) * 512], ych[dc])
                        S.dma('sp', Ypart[b, rows, :], ysb[:])
                        continue
                    if nparts > 1:
                        S.dma('sp', ysb[:], Ypart[b, rows, :])
                        for dc in range(4):
                            S.tt('dve', ysb[:, dc * 512:(dc + 1) * 512], ysb[:, dc * 512:(dc + 1) * 512], ych[dc], ALU.add)
                        ych = [ysb[:, dc * 512:(dc + 1) * 512] for dc in range(4)]
                    resid_ln_tile(g, ph, lnbufs, ych, X[b, rows, :], gate_c if t < 2 else gate_l, lng, lnb)


def hg_consts():
    p = np.arange(128)
    same = (p[:, None] // 64) == (p[None, :] // 64)
    A_f = (same & (p[:, None] <= p[None, :])).astype(np.float32)
    B_f = (same & (p[:, None] > p[None, :])).astype(np.float32)
    hgA = np.stack([A_f, A_f.T]).astype(np.float32)
    hgB = np.stack([B_f, B_f.T]).astype(np.float32)
    ind = np.stack([(p < 64), (p >= 64)], axis=1).astype(np.float32)
    return {"c_hgA": hgA, "c_hgB": hgB, "c_ind": ind}


def phase_hg_scan(g, i, P, O, hg_lb):
    S, nb, c = g.S, g.nb, g.c
    with Phase(g) as ph:
        Am = [ph.alloc("hgA%d" % d, [128, 128], F32) for d in range(2)]
        Bm = [ph.alloc("hgB%d" % d, [128, 128], F32) for d in range(2)]
        hgA_d = dram_in(g, "c_hgA", [2, 128, 128])
        hgB_d = dram_in(g, "c_hgB", [2, 128, 128])
        ind_d = dram_in(g, "c_ind", [128, 2])
        for d in range(2):
            S.dma('sp', Am[d][:], hgA_d[d, :, :])
            S.dma('sp', Bm[d][:], hgB_d[d, :, :])
        ind = ph.alloc("hgind", [128, 2], F32)
        S.dma('sp', ind[:], ind_d[:, :])
        lb = [ph.alloc("hlb%d" % d, [128, D], F32) for d in range(2)]
        oml = [ph.alloc("homl%d" % d, [128, D], F32) for d in range(2)]
        W = [ph.alloc("hw%d" % k, [128, D], F32) for k in range(8)]
        qb, vb, fb, kk, kinv, kd0, kd1, exb = W
        kd = [kd0, kd1]
        for d in range(2):
            tmp, den, num = W[0], W[1], W[2]
            S.memset('dve', num[:], 0.0)
            for l in range(DEPTH):
                S.dma('sp', tmp[:], hg_lb[d, l, :][None, :].broadcast_to([128, D]))
                S.act(tmp[:], tmp[:], AF.Exp)
                if l == 0:
                    S.copy('dve', den[:], tmp[:])
                else:
                    S.tt('dve', den[:], den[:], tmp[:], ALU.add)
                    if l <= i:
                        S.tt('dve', num[:], num[:], tmp[:], ALU.add)
            S.op('dve', lambda t_, den=den: t_.reciprocal(den[:], den[:]), reads=[den], writes=[den])
            S.tt('dve', lb[d][:], num[:], den[:], ALU.mult)
            S.ts('dve', oml[d][:], lb[d][:], -1.0, ALU.mult, 1.0, ALU.add)
        Ssb = ph.alloc("hS", [128, 16, 128], F32)
        Fsb = ph.alloc("hF", [128, 32], F32)
        qT = ph.alloc("hqT", [128, 16, 128], F32)
        kT = ph.alloc("hkT", [128, 16, 128], F32)
        itr = ph.alloc("hitr", [128, 16, 128], F32)
        osb = [ph.alloc("hosb%d" % k, [64, D], F32) for k in range(2)]
        for b in range(nb):
            for d in range(2):
                order = list(range(NT)) if d == 0 else [1, 0] + list(range(NT - 1, 1, -1))
                S.memset('dve', Ssb[:], 0.0)
                for t in order:
                    r0 = prow(t)
                    S.dma('sp', qb[:], P[b, r0:r0 + 128, 0:2048])
                    S.dma('sp', vb[:], P[b, r0:r0 + 128, 2048:4096])
                    S.dma('sp', fb[:], P[b, r0:r0 + 128, 6144 + d * 2048:6144 + (d + 1) * 2048])
                    S.act(fb[:], fb[:], AF.Sigmoid)
                    S.tt('pool', fb[:], fb[:], oml[d][:], ALU.mult)
                    S.tt('dve', fb[:], fb[:], lb[d][:], ALU.add)
                    S.ts('dve', kk[:], fb[:], -1.0, ALU.mult, 1.0, ALU.add)
                    S.act(fb[:], fb[:], AF.Ln)
                    tp = g.ps[2]
                    for h in range(16):
                        S.mm(tp[:, h * 2:(h + 1) * 2], fb[:, h * 128:(h + 1) * 128], ind[:, :])
                    S.act(Fsb[:], tp[:, 0:32], AF.Exp)
                    for cc in range(4):
                        sl = slice(cc * 512, (cc + 1) * 512)
                        gp = g.ps[0]
                        sp_ = g.ps[1]
                        S.mm(gp[:], Am[d][:], fb[:, sl])
                        S.mm(sp_[:], Bm[d][:], fb[:, sl])
                        S.act(exb[:, sl], gp[:], AF.Exp)
                        S.tt('dve', qb[:, sl], qb[:, sl], exb[:, sl], ALU.mult)
                        S.act(exb[:, sl], gp[:], AF.Exp, scale=-1.0)
                        S.tt('pool', kinv[:, sl], kk[:, sl], exb[:, sl], ALU.mult)
                        S.act(exb[:, sl], sp_[:], AF.Exp)
                        S.stt(kd0[:, sl], exb[:, sl], ind[:, 0:1], kk[:, sl], ALU.mult, ALU.mult)
                        S.stt(kd1[:, sl], exb[:, sl], ind[:, 1:2], kk[:, sl], ALU.mult, ALU.mult)
                    for src, dst, bk in ((qb, qT, 3), (kinv, kT, 4)):
                        for gb in range(4):
                            ps = g.ps[bk]
                            for j in range(4):
                                h = gb * 4 + j
                                S.transpose(ps[:, j * 128:(j + 1) * 128], src[:, h * 128:(h + 1) * 128], c['ident'][:])
                            pv = ps[:].rearrange("p (h d) -> p h d", h=4)
                            if bk == 3:
                                S.act(dst[:, gb * 4:(gb + 1) * 4, :], pv, AF.Copy)
                            else:
                                S.copy('dve', dst[:, gb * 4:(gb + 1) * 4, :], pv)
                    for gb in range(4):
                        ps = g.ps[5 + (gb % 2)]
                        for j in range(4):
                            h = gb * 4 + j
                            S.mm(ps[:, j * 128:(j + 1) * 128], kT[:, h, :], qT[:, h, :])
                        S.tt('dve', itr[:, gb * 4:(gb + 1) * 4, :], ps[:].rearrange("p (h d) -> p h d", h=4),
                             Am[d][:].unsqueeze(1).broadcast_to([128, 4, 128]), ALU.mult)
                    for cidx in ((0, 1) if d == 0 else (1, 0)):
                        csl = slice(cidx * 64, (cidx + 1) * 64)
                        for gb in range(4):
                            ps = g.ps[5 + (gb % 2)]
                            for j in range(4):
                                h = gb * 4 + j
                                S.mm(ps[0:64, j * 128:(j + 1) * 128], itr[:, h, csl], vb[:, h * 128:(h + 1) * 128],
                                     start=True, stop=False)
                                S.mm(ps[0:64, j * 128:(j + 1) * 128], qT[:, h, csl], Ssb[:, h, :],
                                     start=False, stop=True)
                            if gb % 2 == 0:
                                S.act(osb[cidx][:, gb * 512:(gb + 1) * 512], ps[0:64, :], AF.Copy)
                            else:
                                S.copy('dve', osb[cidx][:, gb * 512:(gb + 1) * 512], ps[0:64, :])
                        for gb in range(4):
                            ps = g.ps[7 if gb % 2 == 0 else 0]
                            for j in range(4):
                                h = gb * 4 + j
                                S.mm(ps[:, j * 128:(j + 1) * 128], kd[cidx][:, h * 128:(h + 1) * 128],
                                     vb[:, h * 128:(h + 1) * 128])
                            for j in range(4):
                                h = gb * 4 + j
                                S.stt(Ssb[:, h, :], Ssb[:, h, :], Fsb[:, h * 2 + cidx:h * 2 + cidx + 1],
                                      ps[:, j * 128:(j + 1) * 128], ALU.mult, ALU.add)
                        S.dma('sp', O[d, b, t * 128 + cidx * 64:t * 128 + (cidx + 1) * 64, 0:2048], osb[cidx][:])


def phase_hgrn2(g, i, j, X, MOD, lng_d, lnb_d, last):
    w_in = dram_in(g, "hg_w_in%d" % j, [D, 10240])
    w_out = dram_in(g, "hg_w_out%d" % j, [D, D])
    hg_lb = dram_in(g, "hg_lb", [2, DEPTH, D])
    ng_d = dram_in(g, "hg_norm_g%d" % j, [128])
    phase_proj_blocks(g, X, MOD, 1, 0, w_in, 10240, g.P, list(range(NT)))
    phase_hg_scan(g, i, g.P, g.O, hg_lb)
    phase_readout(g, i, X, MOD, g.O, 2048, g.P, 4096, AF.Sigmoid, ng_d, w_out, lng_d, lnb_d, last, g.Ypart)

DNH = 32
NEGBIG = -30000.0


def dn_consts():
    p = np.arange(128)
    A_f = (p[:, None] <= p[None, :]).astype(np.float32)
    dnA = np.stack([A_f, A_f.T]).astype(np.float32)
    nms_f = np.where(p[None, :] < p[:, None], 0.0, NEGBIG).astype(np.float32)
    nms_b = np.where(p[None, :] > p[:, None], 0.0, NEGBIG).astype(np.float32)
    nmt_f = np.where(p[:, None] <= p[None, :], 0.0, NEGBIG).astype(np.float32)
    nmt_b = np.where(p[:, None] >= p[None, :], 0.0, NEGBIG).astype(np.float32)
    nm2 = np.stack([np.concatenate([nms_f, nmt_f], axis=1), np.concatenate([nms_b, nmt_b], axis=1)]).astype(np.float32)
    return {"c_dnA": dnA, "c_dnNM2": nm2}


def phase_dn_prep(g, j, P, QKV, GB, conv_d, alog_d, dtb_d):
    S, nb, c = g.S, g.nb, g.c
    with Phase(g) as ph:
        zt = ph.alloc("dz", [4, 8192], F32)
        S.memset('dve', zt[:], 0.0)
        for b in range(nb):
            S.dma('sp', P[b, 0:2, 0:8192], zt[0:2, :])
            S.dma('sp', P[b, 258:262, 0:8192], zt[0:4, :])
            S.dma('sp', P[b, 2310:2312, 0:8192], zt[0:2, :])
        wt = ph.alloc("cw", [128, 5, 2048], F32)
        xs = [ph.alloc("cx%d" % k, [128, 2048], F32) for k in range(3)]
        acc = [ph.alloc("cacc%d" % k, [128, 2048], F32) for k in range(2)]
        tmp = ph.alloc("ctmp", [128, 2048], F32)
        sq = ph.alloc("csq", [128, 16, 128], F32)
        red = ph.alloc("cred", [128, 16], F32)
        nega = ph.alloc("nega", [128, 64], F32)
        dtb = ph.alloc("dtb", [128, 64], F32)
        S.dma('sp', nega[:], alog_d.rearrange("a h -> (a h)")[None, :].broadcast_to([128, 64]))
        S.dma('sp', dtb[:], dtb_d.rearrange("a h -> (a h)")[None, :].broadcast_to([128, 64]))
        S.act(nega[:], nega[:], AF.Exp)
        S.ts('dve', nega[:], nega[:], -1.0, ALU.mult)
        ba = [ph.alloc("ba%d" % k, [128, 128], F32) for k in range(2)]
        n = 0
        for b in range(nb):
            for t in range(NT):
                r0 = prow(t)
                bt = ba[n % 2]
                n += 1
                S.dma('sp', bt[:], P[b, r0:r0 + 128, 12288:12416])
                S.act(bt[:, 0:64], bt[:, 0:64], AF.Sigmoid)
                S.tt('dve', bt[:, 64:128], bt[:, 64:128], dtb[:], ALU.add)
                S.act(bt[:, 64:128], bt[:, 64:128], AF.Exp)
                S.ts('dve', bt[:, 64:128], bt[:, 64:128], 1.0, ALU.add)
                S.act(bt[:, 64:128], bt[:, 64:128], AF.Ln)
                S.tt('dve', bt[:, 64:128], bt[:, 64:128], nega[:], ALU.mult)
                S.dma('sp', GB[b, t * 128:(t + 1) * 128, :], bt[:])
        n = 0
        for cb in range(4):
            for jt in range(5):
                S.dma('sp', wt[:, jt, :], conv_d[jt, cb * 2048:(cb + 1) * 2048][None, :].broadcast_to([128, 2048]))
            for b in range(nb):
                for t in range(NT):
                    r0 = prow(t)
                    a_ = acc[n % 2]
                    n += 1
                    for jt in range(5):
                        x_ = xs[jt % 3]
                        S.dma('sp' if jt % 2 == 0 else 'act', x_[:], P[b, r0 + jt - 2:r0 + jt - 2 + 128, cb * 2048:(cb + 1) * 2048])
                        if jt == 0:
                            S.tt('dve', a_[:], x_[:], wt[:, jt, :], ALU.mult)
                        else:
                            S.tt('pool', tmp[:], x_[:], wt[:, jt, :], ALU.mult)
                            S.tt('dve', a_[:], a_[:], tmp[:], ALU.add)
                    S.act(a_[:], a_[:], AF.Silu)
                    if cb < 2:
                        a3 = a_[:].rearrange("p (h d) -> p h d", h=16)
                        S.tt('pool', sq[:], a3, a3, ALU.mult)
                        S.op('dve', lambda t_: t_.tensor_reduce(red[:], sq[:], AX.X, ALU.add), reads=[sq], writes=[red])
                        S.ts('dve', red[:], red[:], EPS, ALU.add)
                        S.act(red[:], red[:], AF.Sqrt)
                        S.op('dve', lambda t_: t_.reciprocal(red[:], red[:]), reads=[red], writes=[red])
                        if cb == 0:
                            S.ts('dve', red[:], red[:], 128.0 ** -0.5, ALU.mult)
                        S.tt('dve', a3, a3, red[:].unsqueeze(2).broadcast_to([128, 16, 128]), ALU.mult)
                    S.dma('sp', QKV[b, t * 128:(t + 1) * 128, cb * 2048:(cb + 1) * 2048], a_[:])


def phase_dn_scan(g, QKV, GB, O):
    S, nb, c = g.S, g.nb, g.c
    with Phase(g) as ph:
        dnA_d = dram_in(g, "c_dnA", [2, 128, 128])
        nm2_d = dram_in(g, "c_dnNM2", [2, 128, 256])
        Am = [ph.alloc("dnA%d" % d, [128, 128], F32) for d in range(2)]
        NM2 = [ph.alloc("dnNM%d" % d, [128, 256], F32) for d in range(2)]
        for d in range(2):
            S.dma('sp', Am[d][:], dnA_d[d, :, :])
            S.dma('sp', NM2[d][:], nm2_d[d, :, :])
        ones = c['ones_f']
        negones = ph.alloc("negones", [128, 128], F32)
        S.memset('dve', negones[:], -1.0)
        ident = c['ident']
        qn = ph.alloc("dqn", [128, 2048], F32)
        kn = ph.alloc("dkn", [128, 2048], F32)
        vv = ph.alloc("dvv", [128, 4096], F32)
        gbt = ph.alloc("dgb", [128, 128], F32)
        ot = ph.alloc("dot", [128, 4096], F32)
        Sst = [ph.alloc("dS%d" % h, [128, 128], F32) for h in range(DNH)]
        hq = [ph.alloc("dhq%d" % k, [128, 4, 128], F32) for k in range(2)]
        cb = []
        for k in range(2):
            cb.append(dict(
                G1=ph.alloc("G1_%d" % k, [128, 128], F32),
                E2=ph.alloc("E2_%d" % k, [128, 256], F32),
                sm=ph.alloc("sm_%d" % k, [128, 2], F32),
                sx=ph.alloc("sx_%d" % k, [128, 3], F32),
                M=ph.alloc("M_%d" % k, [128, 128], F32),
                qk=ph.alloc("qk_%d" % k, [128, 128], F32),
                Q1=ph.alloc("Q1_%d" % k, [128, 128], F32),
                R=[ph.alloc("R%d_%d" % (r, k), [128, 256], F32) for r in range(2)],
                PQ=[ph.alloc("PQ%d_%d" % (r, k), [128, 256], F32) for r in range(2)],
                wT=ph.alloc("wT_%d" % k, [128, 128], F32),
                kd=ph.alloc("kd_%d" % k, [128, 128], F32),
                vn=ph.alloc("vn_%d" % k, [128, 128], F32),
                banks=(g.ps[3 * k], g.ps[3 * k + 1], g.ps[3 * k + 2]),
            ))

        def chain(d, h, hv, B, hqs):
            bx, by, bz = B['banks']
            kT, qT, KK, QKt = hqs[:, 0, :], hqs[:, 1, :], hqs[:, 2, :], hqs[:, 3, :]
            gcol = gbt[:, 64 + d * 32 + hv:64 + d * 32 + hv + 1]
            bcol = gbt[:, d * 32 + hv:d * 32 + hv + 1]
            G1, E2, sm, sx, M, qk, Q1 = B['G1'], B['E2'], B['sm'], B['sx'], B['M'], B['qk'], B['Q1']
            S.ts('dve', G1[:], Am[d][:], gcol, ALU.mult)
            yield
            S.mm(bx[:, 0:128], G1[:], ones[:], start=True, stop=False)
            S.mm(bx[:, 0:128], negones[:], G1[:], start=False, stop=True)
            S.mm(bx[:, 128:256], ones[:], G1[:], start=True, stop=False)
            S.mm(bx[:, 128:256], G1[:], negones[:], start=False, stop=True)
            S.mm(bx[:, 256:257], G1[:], ones[:, 0:1])
            S.mm(bx[:, 257:258], ones[:], gcol)
            yield
            S.tt('dve', E2[:], bx[:, 0:256], NM2[d][:], ALU.add)
            S.copy('dve', sm[:], bx[:, 256:258])
            yield
            S.act(E2[:], E2[:], AF.Exp)
            S.act(sx[:, 0:1], sm[:, 0:1], AF.Exp)
            S.act(sx[:, 1:2], sm[:, 0:1], AF.Exp, scale=-1.0, bias=sm[:, 1:2])
            S.act(sx[:, 2:3], sm[:, 1:2], AF.Exp)
            yield
            egc, ekd, gl = sx[:, 0:1], sx[:, 1:2], sx[:, 2:3]
            S.stt(M[:], KK, bcol, E2[:, 0:128], ALU.mult, ALU.mult)
            S.tt('pool', qk[:], QKt, E2[:, 128:256], ALU.mult)
            R = B['R'][0]
            S.ts('dve', R[:, 0:128], vv[:, hv * 128:(hv + 1) * 128], bcol, ALU.mult)
            S.ts('dve', R[:, 128:256], kn[:, h * 128:(h + 1) * 128], bcol, ALU.mult, egc, ALU.mult)
            S.ts('pool', B['kd'][:], kn[:, h * 128:(h + 1) * 128], ekd, ALU.mult)
            yield
            S.transpose(bx[:, 0:128], M[:], ident[:])
            yield
            S.copy('dve', Q1[:], bx[:, 0:128])
            yield
            Pk, Qk = M[:], Q1[:]
            ri = 0
            for lev in range(7):
                R = B['R'][ri]
                Rn = B['R'][1 - ri]
                S.mm(by[:, 0:256], Qk, R[:])
                if lev < 6:
                    S.mm(bz[:, 0:128], Pk, Qk)
                    if lev < 5:
                        S.mm(bz[:, 128:256], Qk, Pk)
                yield
                S.tt('dve', Rn[:], R[:], by[:, 0:256], ALU.subtract if lev == 0 else ALU.add)
                if lev < 6:
                    PQ = B['PQ'][lev % 2]
                    if lev < 5:
                        S.act(PQ[:], bz[:, 0:256], AF.Copy)
                    else:
                        S.act(PQ[:, 0:128], bz[:, 0:128], AF.Copy)
                    Qk, Pk = PQ[:, 0:128], PQ[:, 128:256]
                ri = 1 - ri
                yield
            R = B['R'][ri]
            u, w = R[:, 0:128], R[:, 128:256]
            S.transpose(bx[:, 0:128], w, ident[:])
            yield
            S.copy('dve', B['wT'][:], bx[:, 0:128])
            yield
            St = Sst[hv]
            S.mm(by[:, 0:128], B['wT'][:], St[:])
            S.mm(by[:, 128:256], qT, St[:])
            yield
            S.tt('dve', B['vn'][:], u, by[:, 0:128], ALU.subtract)
            yield
            S.mm(by[:, 256:384], qk[:], B['vn'][:])
            S.mm(bz[:, 0:128], B['kd'][:], B['vn'][:])
            yield
            S.ts('dve', ot[:, hv * 128:(hv + 1) * 128], by[:, 128:256], egc, ALU.mult)
            S.tt('dve', ot[:, hv * 128:(hv + 1) * 128], ot[:, hv * 128:(hv + 1) * 128], by[:, 256:384], ALU.add)
            S.stt(St[:], St[:], gl, bz[:, 0:128], ALU.mult, ALU.add)
            yield

        for b in range(nb):
            for d in range(2):
                order = list(range(NT)) if d == 0 else [1, 0] + list(range(NT - 1, 1, -1))
                for h in range(DNH):
                    S.memset('pool', Sst[h][:], 0.0)
                for t in order:
                    rows = slice(t * 128, (t + 1) * 128)
                    S.dma('sp', qn[:], QKV[b, rows, 0:2048])
                    S.dma('act', kn[:], QKV[b, rows, 2048:4096])
                    S.dma('sp', vv[:], QKV[b, rows, 4096:8192])
                    S.dma('act', gbt[:], GB[b, rows, :])
                    for h in range(16):
                        hqs = hq[h % 2]
                        pa = g.ps[6]
                        pb = g.ps[7]
                        S.transpose(pa[:, 0:128], kn[:, h * 128:(h + 1) * 128], ident[:])
                        S.transpose(pa[:, 128:256], qn[:, h * 128:(h + 1) * 128], ident[:])
                        S.act(hqs[:, 0:2, :], pa[:, 0:256].rearrange("p (a d) -> p a d", a=2), AF.Copy)
                        S.mm(pb[:, 0:128], hqs[:, 0, :], hqs[:, 0, :])
                        S.mm(pb[:, 128:256], hqs[:, 0, :], hqs[:, 1, :])
                        S.act(hqs[:, 2:4, :], pb[:, 0:256].rearrange("p (a d) -> p a d", a=2), AF.Copy)
                        gens = [chain(d, h, 2 * h + k, cb[k], hqs) for k in range(2)]
                        while gens:
                            for gen in list(gens):
                                try:
                                    next(gen)
                                except StopIteration:
                                    gens.remove(gen)
                    S.dma('sp', O[d, b, rows, :], ot[:])


def phase_deltanet(g, i, j, X, MOD, lng_d, lnb_d, last):
    w_in = dram_in(g, "dn_w_in%d" % j, [D, 12416])
    w_out = dram_in(g, "dn_w_out%d" % j, [4096, D])
    conv_d = dram_in(g, "dn_conv%d" % j, [5, 8192])
    alog_d = dram_in(g, "dn_a_log%d" % j, [2, 32])
    dtb_d = dram_in(g, "dn_dt_bias%d" % j, [2, 32])
    ng_d = dram_in(g, "dn_norm_g%d" % j, [128])
    phase_proj_blocks(g, X, MOD, 1, 0, w_in, 12416, g.P, list(range(NT)))
    phase_dn_prep(g, j, g.P, g.QKV, g.GB, conv_d, alog_d, dtb_d)
    phase_dn_scan(g, g.QKV, g.GB, g.O)
    phase_readout(g, i, X, MOD, g.O, 4096, g.P, 8192, AF.Silu, ng_d, w_out, lng_d, lnb_d, last, g.Ypart)

GRID_W = 64
ROPE_FREQS = 32
ROPE_BASE = 10000.0


def rope_tables():
    row = np.repeat(np.arange(L // GRID_W), GRID_W).astype(np.float32)
    col = (np.arange(L) % GRID_W).astype(np.float32)
    inv = (ROPE_BASE ** (-np.arange(ROPE_FREQS, dtype=np.float32) / ROPE_FREQS)).astype(np.float32)
    ang = np.stack([row[:, None] * inv, col[:, None] * inv], axis=1).astype(np.float32)
    return np.cos(ang).reshape(L, 64).astype(np.float32), np.sin(ang).reshape(L, 64).astype(np.float32)


def const_inputs():
    jj, ii = np.meshgrid(np.arange(128), np.arange(128), indexing='ij')
    cos, sin = rope_tables()
    return {
        "c_ident": np.eye(128, dtype=np.float32),
        "c_triu": (jj <= ii).astype(np.float32),
        "c_trius": (jj < ii).astype(np.float32),
        "c_cos": cos, "c_sin": sin,
        **hg_consts(), **dn_consts(),
    }


def build(nb, layers, moe_layers, debug=False, moe_experts=None, skip_mixer=False):
    nc = bass.Bass("TRN2", target_bir_lowering=False)
    g = init_globals(nc, nb)
    S = g.S
    load_consts(g)
    x_in = dram_in(g, "x_in", [nb, L, D])
    ctx_in = dram_in(g, "ctx_in", [nb, LC, D])
    c_all = dram_in(g, "c_all", [nb + 1, D])
    cos_d = dram_in(g, "c_cos", [L, 64])
    sin_d = dram_in(g, "c_sin", [L, 64])
    X = dram_scratch(g, "Xs", [nb, T, D])
    P = dram_scratch(g, "Ps", [nb, PROWS, PW])
    g.P = P
    g.O = dram_scratch(g, "Os", [2, nb, T, 4096])
    g.Ypart = dram_scratch(g, "Yp", [nb, T, D])
    g.QKV = dram_scratch(g, "QKVs", [nb, T, 8192])
    g.GB = dram_scratch(g, "GBs", [nb, T, 128])
    g.Wb = dram_scratch(g, "Wbs", [16 * NQ, 128, PIECE], BF16)
    out = nc.dram_tensor("out", [nb, L, D], F32, kind="ExternalOutput").ap()
    DRAM_NAMES.add("out")
    DRAM_NAMES.add("xdbg")
    for b in range(nb):
        S.dma('sp', X[b, 0:LC, :], ctx_in[b, :, :])
        for k in range(4):
            S.dma('sp', X[b, LC + k * 512:LC + (k + 1) * 512, :], x_in[b, k * 512:(k + 1) * 512, :])
    rw_d = dram_in(g, "router_w", [D, 16])
    rb_d = dram_in(g, "router_b", [16])
    for i in layers:
        last = i == DEPTH - 1
        MOD = dram_scratch(g, "MOD%d" % i, [nb + 1, NMOD * D])
        ada_w = dram_in(g, "ada_w%d" % i, [D, NMOD * D])
        ada_b = dram_in(g, "ada_b%d" % i, [NMOD * D])
        ln_g = dram_in(g, "ln_g%d" % i, [2, D])
        ln_b = dram_in(g, "ln_b%d" % i, [2, D])
        phase_mod(g, i, c_all, ada_w, ada_b, MOD)
        kind = i % 3
        j = i // 3
        if skip_mixer:
            pass
        elif kind == 0:
            w_in = dram_in(g, "attn_w_in%d" % j, [D, 3072])
            w_out = dram_in(g, "attn_w_out%d" % j, [D, D])
            qg = dram_in(g, "attn_q_g%d" % j, [128])
            kg = dram_in(g, "attn_k_g%d" % j, [128])
            phase_proj_tm(g, i, X, MOD, 1, 0, w_in, 3072, P, list(range(NT)))
            phase_attn_core(g, i, X, MOD, P, w_out, qg, kg, ln_g[0, :], ln_b[0, :], cos_d, sin_d, last)
        elif kind == 1:
            phase_deltanet(g, i, j, X, MOD, ln_g[0, :], ln_b[0, :], last)
        else:
            phase_hgrn2(g, i, j, X, MOD, ln_g[0, :], ln_b[0, :], last)
        if i in moe_layers:
            wg = wu = wd = None
            if moe_experts is None or len(moe_experts) > 0:
                wg = dram_in(g, "moe_w_gate%d" % i, [16, D, 1024])
                wu = dram_in(g, "moe_w_up%d" % i, [16, D, 1024])
                wd = dram_in(g, "moe_w_down%d" % i, [16, 1024, D])
            tiles = list(range(2, NT)) if last else list(range(NT))
            exps = moe_experts if moe_experts is not None else range(16)
            if wg is not None:
                phase_moe_precast(g, wg, wu, wd, g.Wb, experts=exps)
            phase_moe(g, i, X, MOD, rw_d, rb_d, g.Wb, ln_g[1, :], ln_b[1, :], tiles, experts=exps, dbg=G.moe_dbg)
    S.barrier()
    for b in range(nb):
        for k in range(4):
            S.dma('sp', out[b, k * 512:(k + 1) * 512, :], X[b, LC + k * 512:LC + (k + 1) * 512, :], is_output=True)
    if debug:
        xdbg = nc.dram_tensor("xdbg", [nb, T, D], F32, kind="ExternalOutput").ap()
        for b in range(nb):
            for k in range(6):
                S.dma('sp', xdbg[b, k * 384:(k + 1) * 384, :], X[b, k * 384:(k + 1) * 384, :], is_output=True)
    S.finish()
    return nc, g


def make_in_map(inp, g, b0, nb, consts, cache=None):
    if cache is None:
        cache = {}
    m = {}
    m["x_in"] = np.ascontiguousarray(inp["x"][b0:b0 + nb])
    m["ctx_in"] = np.ascontiguousarray(inp["ctx"][b0:b0 + nb])
    m["c_all"] = np.ascontiguousarray(np.concatenate([inp["c"][b0:b0 + nb], inp["c_ctx"][None, :]], axis=0))
    full = {}
    for name in g.din:
        if name in m:
            full[name] = m[name]
            continue
        if name in cache:
            full[name] = cache[name]
            continue
        if name in consts:
            v = consts[name]
        else:
            base = name.rstrip("0123456789")
            idx = int(name[len(base):]) if len(name) > len(base) else None
            if base in ("router_w", "router_b", "hg_lb"):
                v = np.ascontiguousarray(inp[base])
            else:
                v = np.ascontiguousarray(inp[base][idx])
        cache[name] = v
        full[name] = v
    return {k: full[k] for k in g.din}


NB_PER_CORE = 2
N_CORES = 8


def kernel(**inputs):
    inputs = {k: np.asarray(v) for k, v in inputs.items()}
    nc, g = build(NB_PER_CORE, [0, 1, 2, 3], [0, 1, 2, 3])
    consts = const_inputs()
    cache = {}
    in_maps = []
    for cid in range(N_CORES):
        m = make_in_map(inputs, g, cid * NB_PER_CORE, NB_PER_CORE, consts, cache)
        in_maps.append(m)
    res = run_bass_kernel_spmd(nc, in_maps, core_ids=list(range(N_CORES)))
    out = np.concatenate([np.asarray(r["out"]) for r in res.results], axis=0)
    return np.ascontiguousarray(out.astype(np.float32, copy=False))
```

```python
import numpy as np
import concourse.bass as bass
import concourse.mybir as mybir
from concourse.bass_utils import run_bass_kernel_spmd

F32 = mybir.dt.float32
BF16 = mybir.dt.bfloat16
I32 = mybir.dt.int32
AF = mybir.ActivationFunctionType
ALU = mybir.AluOpType
AX = mybir.AxisListType

ENGS = ['pe', 'act', 'dve', 'pool', 'sp']
NDS = 24
DRAM_NAMES = set()


class Sched:
    def __init__(self, nc):
        self.nc = nc
        self.eng = dict(pe=nc.tensor, act=nc.scalar, dve=nc.vector, pool=nc.gpsimd, sp=nc.sync)
        self.sems = {}
        self.cnt = {}
        for e in ENGS:
            self.sems[e] = nc.alloc_semaphore("sem_" + e)
            self.cnt[e] = 0
        self.dslots = {}
        for q in ['sp', 'pool', 'act']:
            self.dslots[q] = []
            for i in range(NDS):
                sid = "d_%s_%d" % (q, i)
                self.sems[sid] = nc.alloc_semaphore(sid)
                self.cnt[sid] = 0
                self.dslots[q].append(sid)
        self.dnext = {q: 0 for q in self.dslots}
        self.seen = {e: {} for e in ENGS}
        self.res = {}
        self.n_inst = 0
        self.n_wait = 0
        self.out_events = []

    def _need(self, e, need, sid, val):
        if val <= 0:
            return
        if sid == e and e == 'pe':
            return
        if self.seen[e].get(sid, 0) >= val:
            return
        if need.get(sid, 0) < val:
            need[sid] = val

    def _collect(self, e, reads, writes, par=False):
        need = {}
        for k in reads:
            r = self.res.get(k)
            if r is None:
                continue
            for sid, val in r['w'].items():
                self._need(e, need, sid, val)
            if k.startswith('psb'):
                for sid, val in r['r'].items():
                    if sid != e:
                        self._need(e, need, sid, val)
        for k in writes:
            r = self.res.get(k)
            if r is None:
                continue
            if not par:
                for sid, val in r['w'].items():
                    self._need(e, need, sid, val)
            for sid, val in r['r'].items():
                self._need(e, need, sid, val)
        return need

    def _emit_waits(self, e, need):
        eng = self.eng[e]
        for sid, val in need.items():
            mult = 16 if sid.startswith('d_') else 1
            eng.wait_ge(self.sems[sid], val * mult)
            self.seen[e][sid] = val
            self.n_wait += 1

    def _record(self, ev, reads, writes, par=False):
        sid, val = ev
        for k in reads:
            r = self.res.setdefault(k, {'w': {}, 'r': {}})
            if r['r'].get(sid, 0) < val:
                r['r'][sid] = val
        for k in writes:
            if par:
                r = self.res.setdefault(k, {'w': {}, 'r': {}})
                if r['w'].get(sid, 0) < val:
                    r['w'][sid] = val
            else:
                self.res[k] = {'w': {sid: val}, 'r': {}}

    @staticmethod
    def keys_of(aps):
        ks = []
        for a in aps:
            if a is None:
                continue
            if isinstance(a, str) or isinstance(a, tuple):
                ks.append(a)
            elif hasattr(a, 'tensor'):
                ks.append(a.tensor.name)
            elif hasattr(a, 'name'):
                ks.append(a.name)
        return ks

    def op(self, e, fn, reads=(), writes=()):
        reads = self.keys_of(reads)
        writes = self.keys_of(writes)
        need = self._collect(e, reads, writes)
        self._emit_waits(e, need)
        inst = fn(self.eng[e])
        self.cnt[e] += 1
        inst.then_inc(self.sems[e], 1)
        self._record((e, self.cnt[e]), reads, writes)
        self.n_inst += 1
        return inst

    def dma(self, q, out, in_, reads=None, writes=None, is_output=False, par=None, **kw):
        reads = self.keys_of([in_] if reads is None else reads)
        writes = self.keys_of([out] if writes is None else writes)
        if par is None:
            par = all(k in DRAM_NAMES for k in writes)
        slot = self.dslots[q][self.dnext[q]]
        self.dnext[q] = (self.dnext[q] + 1) % NDS
        need = self._collect(q, reads, writes, par=par)
        self._need(q, need, slot, self.cnt[slot])
        self._emit_waits(q, need)
        inst = self.eng[q].dma_start(out=out, in_=in_, **kw)
        self.cnt[slot] += 1
        inst.then_inc(self.sems[slot], 16)
        ev = (slot, self.cnt[slot])
        self._record(ev, reads, writes, par=par)
        if is_output:
            self.out_events.append(ev)
        self.n_inst += 1
        return inst

    def barrier(self):
        for e in ENGS:
            need = {}
            for sid, c in self.cnt.items():
                if c > 0:
                    self._need(e, need, sid, c)
            self._emit_waits(e, need)

    def finish(self):
        need = {}
        for sid, c in self.cnt.items():
            if c > 0 and self.seen['sp'].get(sid, 0) < c:
                need[sid] = c
        self._emit_waits('sp', need)

    def mm(self, out, lhsT, rhs, start=True, stop=True, **kw):
        return self.op('pe', lambda t: t.matmul(out, lhsT, rhs, start=start, stop=stop, **kw),
                       reads=[lhsT, rhs], writes=[out])

    def transpose(self, out, in_, ident):
        return self.op('pe', lambda t: t.transpose(out, in_, ident), reads=[in_, ident], writes=[out])

    def act(self, out, in_, func, scale=None, bias=None, accum_out=None, e='act'):
        kw = {}
        rd = [in_]
        if scale is not None:
            kw['scale'] = scale
            if not isinstance(scale, (int, float)):
                rd.append(scale)
        if bias is not None:
            kw['bias'] = bias
            if not isinstance(bias, (int, float)):
                rd.append(bias)
        wr = [out]
        if accum_out is not None:
            kw['accum_out'] = accum_out
            wr.append(accum_out)
        return self.op('act', lambda t: t.activation(out, in_, func, **kw), reads=rd, writes=wr)

    def tt(self, e, out, in0, in1, op):
        return self.op(e, lambda t: t.tensor_tensor(out, in0, in1, op), reads=[in0, in1], writes=[out])

    def ts(self, e, out, in0, s1, op0, s2=None, op1=None, accum_out=None):
        rd = [in0]
        if not isinstance(s1, (int, float)):
            rd.append(s1)
        if s2 is not None and not isinstance(s2, (int, float)):
            rd.append(s2)
        kw = {}
        wr = [out]
        if accum_out is not None:
            kw['accum_out'] = accum_out
            wr.append(accum_out)
        if op1 is None:
            return self.op(e, lambda t: t.tensor_scalar(out, in0, s1, None, op0, **kw), reads=rd, writes=wr)
        return self.op(e, lambda t: t.tensor_scalar(out, in0, s1, s2, op0, op1, **kw), reads=rd, writes=wr)

    def stt(self, out, in0, scalar, in1, op0, op1, e='dve'):
        rd = [in0, in1]
        if not isinstance(scalar, (int, float)):
            rd.append(scalar)
        return self.op(e, lambda t: t.scalar_tensor_tensor(out, in0, scalar, in1, op0, op1),
                       reads=rd, writes=[out])

    def copy(self, e, out, in_):
        if e == 'act':
            return self.act(out, in_, AF.Copy)
        return self.op(e, lambda t: t.tensor_copy(out, in_), reads=[in_], writes=[out])

    def memset(self, e, ap, val):
        return self.op(e, lambda t: t.memset(ap, val), reads=[], writes=[ap])
import math
import numpy as np

D = 2048
LC = 256
L = 2048
T = LC + L
NT = T // 128
DEPTH = 4
ALPHA = (2.0 * DEPTH) ** 0.25
EPS = 1e-6
NMOD = 6


class G:
    moe_dbg = 0


def uname(g, base):
    g.uid += 1
    return "%s_%d" % (base, g.uid)


def sb(g, base, shape, dt):
    return g.nc.alloc_sbuf_tensor(uname(g, base), list(shape), dt)


class Phase:
    def __init__(self, g):
        self.g = g

    def __enter__(self):
        g = self.g
        g.S.barrier()
        self.stack = []
        g.phase_stack.append(self)
        return self

    def alloc(self, base, shape, dt):
        g = self.g
        cm = g.nc.sbuf_tensor(uname(g, base), list(shape), dt)
        t = cm.__enter__()
        self.stack.append(cm)
        return t

    def __exit__(self, *a):
        g = self.g
        g.S.barrier()
        for cm in reversed(self.stack):
            cm.__exit__(None, None, None)
        g.phase_stack.pop()
        return False


def init_globals(nc, nb):
    g = G()
    g.nc = nc
    g.S = Sched(nc)
    g.uid = 0
    g.nb = nb
    g.phase_stack = []
    g.ps = [nc.alloc_psum_tensor("psb%d" % i, [128, 512], F32) for i in range(8)]
    g.din = {}
    return g


def dram_in(g, name, shape, dt=F32):
    if name not in g.din:
        g.din[name] = g.nc.dram_tensor(name, list(shape), dt, kind="ExternalInput").ap()
    return g.din[name]


def dram_scratch(g, name, shape, dt=F32):
    DRAM_NAMES.add(name)
    return g.nc.dram_tensor(name, list(shape), dt, kind="Internal").ap()


def load_consts(g):
    nc, S = g.nc, g.S
    c = {}
    ident_d = dram_in(g, "c_ident", [128, 128])
    c['ident'] = nc.alloc_sbuf_tensor("ident", [128, 128], F32)
    S.dma('sp', c['ident'][:], ident_d[:, :])
    tri_d = dram_in(g, "c_triu", [128, 128])
    c['triu'] = nc.alloc_sbuf_tensor("triu", [128, 128], F32)
    S.dma('sp', c['triu'][:], tri_d[:, :])
    tris_d = dram_in(g, "c_trius", [128, 128])
    c['trius'] = nc.alloc_sbuf_tensor("trius", [128, 128], F32)
    S.dma('sp', c['trius'][:], tris_d[:, :])
    c['ones_b'] = nc.alloc_sbuf_tensor("ones_b", [128, 128], BF16)
    S.memset('dve', c['ones_b'][:], 1.0)
    c['ones_f'] = nc.alloc_sbuf_tensor("ones_f", [128, 128], F32)
    S.memset('dve', c['ones_f'][:], 1.0)
    g.c = c
    return c


def phase_mod(g, i, c_all, ada_w, ada_b, MOD):
    nc, S = g.nc, g.S
    R = g.nb + 1
    NW = NMOD * D
    with Phase(g) as ph:
        cT = ph.alloc("cT", [128, 16, R], F32)
        sT = ph.alloc("sT", [128, 16, R], BF16)
        for r in range(R):
            S.dma('sp', cT[:, :, r], c_all[r, :].rearrange("(kc p) -> p kc", p=128),
                  allow_slow_non_contiguous=True)
        S.act(sT[:], cT[:], AF.Silu)
        bias = ph.alloc("mbias", [R, NW], F32)
        S.dma('sp', bias[:], ada_b[None, :].broadcast_to([R, NW]))
        modt = ph.alloc("modt", [R, NW], F32)
        wb = [ph.alloc("mw%d" % k, [128, 16, 512], BF16) for k in range(2)]
        wv = ada_w.rearrange("(kc p) n -> p kc n", p=128)
        for n in range(NW // 512):
            w = wb[n % 2]
            for kc in range(16):
                S.dma('pool', w[:, kc, :], wv[:, kc, n * 512:(n + 1) * 512], par=True)
            ps = g.ps[n % 2]
            for kc in range(16):
                S.mm(ps[0:R, :], sT[:, kc, :], w[:, kc, :], start=(kc == 0), stop=(kc == 15))
            S.tt('dve', modt[:, n * 512:(n + 1) * 512], ps[0:R, :], bias[:, n * 512:(n + 1) * 512], ALU.add)
        for m in (1, 4):
            S.ts('dve', modt[:, m * D:(m + 1) * D], modt[:, m * D:(m + 1) * D], 1.0, ALU.add)
        S.dma('sp', MOD[:, :], modt[:])


def load_mod_cols(g, ph, MOD, r, m_scale, m_shift):
    S = g.S
    sc = ph.alloc("msc", [128, 16], F32)
    sh = ph.alloc("msh", [128, 16], F32)
    S.dma('sp', sc[:], MOD[r, m_scale * D:(m_scale + 1) * D].rearrange("(kc p) -> p kc", p=128),
          allow_slow_non_contiguous=True)
    S.dma('sp', sh[:], MOD[r, m_shift * D:(m_shift + 1) * D].rearrange("(kc p) -> p kc", p=128),
          allow_slow_non_contiguous=True)
    return sc, sh


def load_row_bcast(g, ph, name, row_ap, n=D):
    t = ph.alloc(name, [128, n], F32)
    g.S.dma('sp', t[:], row_ap[None, :].broadcast_to([128, n]))
    return t


def build_hT_tile(g, xt, hT_dst, sc, sh, psbase=0, hT32=None):
    S, c = g.S, g.c
    for gb in range(4):
        ps = g.ps[psbase + gb]
        for j in range(4):
            kc = gb * 4 + j
            S.transpose(ps[:, j * 128:(j + 1) * 128], xt[:, kc * 128:(kc + 1) * 128], c['ident'][:])
        for j in range(4):
            kc = gb * 4 + j
            src = ps[:, j * 128:(j + 1) * 128]
            dsts = [hT_dst] + ([hT32] if hT32 is not None else [])
            for dst in dsts:
                if gb % 2 == 0:
                    S.act(dst[:, kc, :], src, AF.Identity, scale=sc[:, kc:kc + 1], bias=sh[:, kc:kc + 1])
                else:
                    S.ts('dve', dst[:, kc, :], src, sc[:, kc:kc + 1], ALU.mult, sh[:, kc:kc + 1], ALU.add)


def load_w_bf16(g, wsb, w_dram, ncols, col0=0, chunk=1024):
    S = g.S
    K = w_dram.shape[0]
    wv = w_dram.rearrange("(kc p) n -> p kc n", p=128)
    for kc in range(K // 128):
        for c0 in range(0, ncols, chunk):
            cw = min(chunk, ncols - c0)
            S.dma('pool', wsb[:, kc, c0:c0 + cw], wv[:, kc, col0 + c0:col0 + c0 + cw], par=True)


def mod_row(b, t, nb):
    return nb if t < 2 else b


def resid_ln_tile(g, ph, bufs, ychunks, X_rows, gate_t, lng_t, lnb_t, q='sp'):
    S, nc = g.S, g.nc
    xt, zt, st, mv, rs = bufs
    S.dma(q, xt[:], X_rows)
    for dc in range(4):
        sl = slice(dc * 512, (dc + 1) * 512)
        S.tt('dve', zt[:, sl], ychunks[dc], gate_t[:, sl], ALU.mult)
    S.stt(zt[:], xt[:], ALPHA, zt[:], ALU.mult, ALU.add, e='dve')
    for dc in range(4):
        S.op('dve', lambda t, dc=dc: t.bn_stats(st[:, dc * 6:(dc + 1) * 6], zt[:, dc * 512:(dc + 1) * 512]),
             reads=[zt], writes=[st])
    S.op('dve', lambda t: t.bn_aggr(mv[:], st[:]), reads=[st], writes=[mv])
    S.ts('dve', rs[:, 0:1], mv[:, 1:2], EPS, ALU.add)
    S.act(rs[:, 0:1], rs[:, 0:1], AF.Sqrt)
    S.op('dve', lambda t: t.reciprocal(rs[:, 0:1], rs[:, 0:1]), reads=[rs], writes=[rs])
    S.ts('dve', zt[:], zt[:], mv[:, 0:1], ALU.subtract, rs[:, 0:1], ALU.mult)
    S.tt('pool', zt[:], zt[:], lng_t[:], ALU.mult)
    S.tt('pool', xt[:], zt[:], lnb_t[:], ALU.add)
    S.dma(q, X_rows, xt[:])


def alloc_ln_bufs(ph, k=""):
    xt = ph.alloc("lx" + k, [128, D], F32)
    zt = ph.alloc("lz" + k, [128, D], F32)
    st = ph.alloc("lst" + k, [128, 24], F32)
    mv = ph.alloc("lmv" + k, [128, 2], F32)
    rs = ph.alloc("lrs" + k, [128, 2], F32)
    return (xt, zt, st, mv, rs)

HD = 128
HQ = 16
HKV = 4
ATT_SCALE = HD ** -0.5


def phase_proj_tm(g, i, X, MOD, m_scale, m_shift, w_dram, ncols, P, tiles_per_b):
    S, nb = g.S, g.nb
    with Phase(g) as ph:
        wsb = ph.alloc("pw", [128, 16, ncols], BF16)
        load_w_bf16(g, wsb, w_dram, ncols)
        hT = [ph.alloc("phT%d" % k, [128, 16, 128], BF16) for k in range(2)]
        xt = [ph.alloc("pxt%d" % k, [128, D], F32) for k in range(2)]
        pt = [ph.alloc("ppt%d" % k, [128, ncols], F32) for k in range(2)]
        mods = {}
        for r in range(nb + 1):
            mods[r] = load_mod_cols(g, ph, MOD, r, m_scale, m_shift)
        n = 0
        for b in range(nb):
            for t in tiles_per_b:
                sc, sh = mods[mod_row(b, t, nb)]
                x_ = xt[n % 2]
                h_ = hT[n % 2]
                p_ = pt[n % 2]
                S.dma('sp', x_[:], X[b, t * 128:(t + 1) * 128, :])
                build_hT_tile(g, x_, h_, sc, sh, psbase=0)
                for c in range(ncols // 512):
                    ps = g.ps[4 + (c % 4)]
                    for kc in range(16):
                        S.mm(ps[:], h_[:, kc, :], wsb[:, kc, c * 512:(c + 1) * 512], start=(kc == 0), stop=(kc == 15))
                    if c % 2 == 0:
                        S.act(p_[:, c * 512:(c + 1) * 512], ps[:], AF.Copy)
                    else:
                        S.copy('dve', p_[:, c * 512:(c + 1) * 512], ps[:])
                S.dma('sp', P[b, t * 128:(t + 1) * 128, 0:ncols], p_[:])
                n += 1


def rms_heads(g, tmp, x3, nh, gb, sq, red):
    S = g.S
    S.tt('pool', sq[:, 0:nh, :], x3, x3, ALU.mult)
    S.op('dve', lambda t: t.tensor_reduce(red[:, 0:nh], sq[:, 0:nh, :], AX.X, ALU.add), reads=[sq], writes=[red])
    S.ts('dve', red[:, 0:nh], red[:, 0:nh], 1.0 / HD, ALU.mult, EPS, ALU.add)
    S.act(red[:, 0:nh], red[:, 0:nh], AF.Sqrt)
    S.op('dve', lambda t: t.reciprocal(red[:, 0:nh], red[:, 0:nh]), reads=[red], writes=[red])
    S.tt('dve', x3, x3, red[:, 0:nh].unsqueeze(2).broadcast_to([128, nh, HD]), ALU.mult)
    S.tt('pool', x3, x3, gb[:].unsqueeze(1).broadcast_to([128, nh, HD]), ALU.mult)


def rope_heads(g, x3, out3, nh, cs, sn, t1, t2):
    S = g.S
    xv = x3[:, 0:nh * HD].rearrange("p (h a s f) -> p h a s f", h=nh, a=2, s=2, f=32)
    ov = out3[:, 0:nh * HD].rearrange("p (h a s f) -> p h a s f", h=nh, a=2, s=2, f=32)
    csb = cs[:].rearrange("p (a f) -> p a f", a=2)
    snb = sn[:].rearrange("p (a f) -> p a f", a=2)
    t1v = t1[:, 0:nh * 64].rearrange("p (h a f) -> p h a f", h=nh, a=2, f=32)
    t2v = t2[:, 0:nh * 64].rearrange("p (h a f) -> p h a f", h=nh, a=2, f=32)
    x1 = xv[:, :, :, 0, :]
    x2 = xv[:, :, :, 1, :]
    csb4 = csb.unsqueeze(1).broadcast_to([128, nh, 2, 32])
    snb4 = snb.unsqueeze(1).broadcast_to([128, nh, 2, 32])
    S.tt('dve', t1v, x1, csb4, ALU.mult)
    S.tt('pool', t2v, x2, snb4, ALU.mult)
    S.tt('dve', ov[:, :, :, 0, :], t1v, t2v, ALU.subtract)
    S.tt('pool', t1v, x2, csb4, ALU.mult)
    S.tt('dve', t2v, x1, snb4, ALU.mult)
    S.tt('pool', ov[:, :, :, 1, :], t1v, t2v, ALU.add)


def phase_attn_core(g, i, X, MOD, P, wout_d, qg_d, kg_d, lng_d, lnb_d, cos_d, sin_d, last):
    S, nb, c = g.S, g.nb, g.c
    with Phase(g) as ph:
        wout = ph.alloc("awo", [128, 16, D], BF16)
        load_w_bf16(g, wout, wout_d, D)
        qgb = load_row_bcast(g, ph, "qgb", qg_d, HD)
        kgb = load_row_bcast(g, ph, "kgb", kg_d, HD)
        lng = load_row_bcast(g, ph, "lng", lng_d)
        lnb = load_row_bcast(g, ph, "lnb", lnb_d)
        gate_c = load_row_bcast(g, ph, "gatec", MOD[nb, 2 * D:3 * D])
        gate_l = ph.alloc("gatel", [128, D], F32)
        KT = ph.alloc("KT", [128, HKV, T], BF16)
        Vs = ph.alloc("Vs", [128, NT, HKV * HD], BF16)
        kv = ph.alloc("kv", [128, 2 * HKV * HD], F32)
        kr = ph.alloc("kr", [128, HKV * HD], F32)
        qx = ph.alloc("qx", [128, D], F32)
        qr = ph.alloc("qr", [128, D], F32)
        sq = ph.alloc("sq", [128, HQ, HD], F32)
        red = ph.alloc("red", [128, HQ], F32)
        t1 = ph.alloc("rt1", [128, HQ * 64], F32)
        t2 = ph.alloc("rt2", [128, HQ * 64], F32)
        cs = ph.alloc("cs", [128, 64], F32)
        sn = ph.alloc("sn", [128, 64], F32)
        QT = ph.alloc("QT", [128, HQ, 128], BF16)
        OT = ph.alloc("OT", [128, HQ, 128], BF16)
        PT = [ph.alloc("PT%d" % k, [128, 512], BF16) for k in range(3)]
        rec = ph.alloc("rec", [128, 512], F32)
        lnbufs = alloc_ln_bufs(ph)
        for b in range(nb):
            S.dma('sp', gate_l[:], MOD[b, 2 * D:3 * D][None, :].broadcast_to([128, D]))
            for t in range(NT):
                S.dma('sp', kv[:], P[b, t * 128:(t + 1) * 128, HQ * HD:HQ * HD + 2 * HKV * HD])
                k3 = kv[:, 0:HKV * HD].rearrange("p (h d) -> p h d", h=HKV)
                rms_heads(g, None, k3, HKV, kgb, sq, red)
                ksrc = kv
                if t >= 2:
                    S.dma('sp', cs[:], cos_d[(t - 2) * 128:(t - 1) * 128, :])
                    S.dma('sp', sn[:], sin_d[(t - 2) * 128:(t - 1) * 128, :])
                    rope_heads(g, kv, kr, HKV, cs, sn, t1, t2)
                    ksrc = kr
                ps = g.ps[t % 2]
                for h in range(HKV):
                    S.transpose(ps[:, h * 128:(h + 1) * 128], ksrc[:, h * HD:(h + 1) * HD], c['ident'][:])
                S.act(KT[:, :, t * 128:(t + 1) * 128], ps[:].rearrange("p (h d) -> p h d", h=HKV), AF.Copy)
                S.copy('dve', Vs[:, t, :], kv[:, HKV * HD:2 * HKV * HD])
            qtiles = list(range(2, NT)) if last else list(range(NT))
            for t in qtiles:
                is_ctx = t < 2
                S.dma('sp', qx[:], P[b, t * 128:(t + 1) * 128, 0:HQ * HD])
                q3 = qx[:].rearrange("p (h d) -> p h d", h=HQ)
                rms_heads(g, None, q3, HQ, qgb, sq, red)
                qsrc = qx
                if not is_ctx:
                    S.dma('sp', cs[:], cos_d[(t - 2) * 128:(t - 1) * 128, :])
                    S.dma('sp', sn[:], sin_d[(t - 2) * 128:(t - 1) * 128, :])
                    rope_heads(g, qx, qr, HQ, cs, sn, t1, t2)
                    qsrc = qr
                for gb in range(4):
                    ps = g.ps[gb % 2]
                    for j in range(4):
                        h = gb * 4 + j
                        S.transpose(ps[:, j * 128:(j + 1) * 128], qsrc[:, h * HD:(h + 1) * HD], c['ident'][:])
                    S.act(QT[:, gb * 4:(gb + 1) * 4, :], ps[:].rearrange("p (h d) -> p h d", h=4), AF.Copy)
                stiles = [0, 1] if is_ctx else list(range(NT))
                for kvh in range(HKV):
                    o_ps = g.ps[2]
                    d_ps = g.ps[3]
                    for si, s in enumerate(stiles):
                        s_ps = g.ps[4 + (si % 3)]
                        S.mm(s_ps[:], KT[:, kvh, s * 128:(s + 1) * 128], QT[:, kvh * 4:(kvh + 1) * 4, :])
                        p_ = PT[si % 3]
                        S.act(p_[:], s_ps[:], AF.Exp, scale=ATT_SCALE)
                        S.mm(o_ps[:], Vs[:, s, kvh * HD:(kvh + 1) * HD], p_[:], start=(si == 0), stop=(si == len(stiles) - 1))
                        S.mm(d_ps[:], c['ones_b'][:], p_[:], start=(si == 0), stop=(si == len(stiles) - 1))
                    S.op('dve', lambda tt_: tt_.reciprocal(rec[:], d_ps[:]), reads=[d_ps], writes=[rec])
                    S.tt('dve', OT[:, kvh * 4:(kvh + 1) * 4, :], o_ps[:].rearrange("p (h d) -> p h d", h=4),
                         rec[:].rearrange("p (h d) -> p h d", h=4), ALU.mult)
                ych = []
                for dc in range(4):
                    ps = g.ps[4 + dc] if False else g.ps[(dc % 2) + 0] if False else None
                banks = [0, 1, 7, 3]
                for dc in range(4):
                    ps = g.ps[banks[dc]]
                    for h in range(HQ):
                        S.mm(ps[:], OT[:, h, :], wout[:, h, dc * 512:(dc + 1) * 512], start=(h == 0), stop=(h == HQ - 1))
                    ych.append(ps[:])
                resid_ln_tile(g, ph, lnbufs, ych, X[b, t * 128:(t + 1) * 128, :],
                              gate_c if is_ctx else gate_l, lng, lnb)

NE = 16
DE = 1024
TG = 4
NQ = 4
FQ = DE // NQ
NFC = FQ // 128


PIECE = 16 * 2 * FQ + NFC * D


def phase_moe_precast(g, wg_d, wu_d, wd_d, Wb, experts=range(NE)):
    S = g.S
    with Phase(g) as ph:
        st = [ph.alloc("pcw%d" % k, [128, PIECE], BF16) for k in range(3)]
        n = 0
        for e in experts:
            wgv = wg_d[e].rearrange("(kc p) f -> p kc f", p=128)
            wuv = wu_d[e].rearrange("(kc p) f -> p kc f", p=128)
            wdv = wd_d[e].rearrange("(fc p) d -> p fc d", p=128)
            for hf in range(NQ):
                s_ = st[n % 3]
                n += 1
                wb = s_[:, 0:16 * 2 * FQ].rearrange("p (kc two f) -> p kc two f", kc=16, two=2)
                wdb = s_[:, 16 * 2 * FQ:PIECE].rearrange("p (fc d) -> p fc d", fc=NFC)
                for k4 in range(0, 16, 4):
                    S.dma('pool', wb[:, k4:k4 + 4, 0, :], wgv[:, k4:k4 + 4, hf * FQ:(hf + 1) * FQ], par=True)
                    S.dma('pool', wb[:, k4:k4 + 4, 1, :], wuv[:, k4:k4 + 4, hf * FQ:(hf + 1) * FQ], par=True)
                S.dma('pool', wdb[:, :, :], wdv[:, hf * NFC:(hf + 1) * NFC, :], par=True)
                S.dma('sp' if n % 2 == 0 else 'act', Wb[e * NQ + hf, :, :], s_[:])


def gating_tile(g, bufs, logits_ps, rbb, Gdst):
    S = g.S
    sc, sel, w = bufs
    S.act(sc[:], logits_ps, AF.Sigmoid)
    S.tt('dve', sel[:], sc[:], rbb[:], ALU.add)
    s4 = sel[:].rearrange("p (g e) -> p g e", g=4)
    x0, x1, x2, x3 = s4[:, :, 0], s4[:, :, 1], s4[:, :, 2], s4[:, :, 3]
    M1, m1, M2, m2 = w[:, 0:4], w[:, 4:8], w[:, 8:12], w[:, 12:16]
    A, Bm, Cm, s2, gs = w[:, 16:20], w[:, 20:24], w[:, 24:28], w[:, 28:32], w[:, 32:36]
    gmax, gmask, den = w[:, 36:37], w[:, 40:44], w[:, 44:45]
    S.tt('dve', M1, x0, x1, ALU.max)
    S.tt('dve', m1, x0, x1, ALU.min)
    S.tt('dve', M2, x2, x3, ALU.max)
    S.tt('dve', m2, x2, x3, ALU.min)
    S.tt('dve', A, M1, M2, ALU.max)
    S.tt('dve', Bm, M1, M2, ALU.min)
    S.tt('dve', Cm, m1, m2, ALU.max)
    S.tt('dve', s2, Bm, Cm, ALU.max)
    S.tt('dve', gs, A, s2, ALU.add)
    S.op('dve', lambda t: t.tensor_reduce(gmax, gs, AX.X, ALU.max), reads=[w], writes=[w])
    S.ts('dve', gmask, gs, gmax, ALU.is_ge)
    em = w[:, 48:64].rearrange("p (g e) -> p g e", g=4)
    S.tt('dve', em, s4, s2.unsqueeze(2).broadcast_to([128, 4, 4]), ALU.is_ge)
    S.tt('dve', em, em, gmask.unsqueeze(2).broadcast_to([128, 4, 4]), ALU.mult)
    S.tt('dve', w[:, 48:64], w[:, 48:64], sc[:], ALU.mult)
    S.op('dve', lambda t: t.tensor_reduce(den, w[:, 48:64], AX.X, ALU.add), reads=[w], writes=[w])
    S.op('dve', lambda t: t.reciprocal(den, den), reads=[w], writes=[w])
    S.ts('dve', Gdst, w[:, 48:64], den, ALU.mult)


def phase_moe(g, i, X, MOD, rw_d, rb_d, Wb, lng_d, lnb_d, tiles_per_b, experts=range(NE), dbg=0):
    S, nb, c = g.S, g.nb, g.c
    toks = [(b, t) for b in range(nb) for t in tiles_per_b]
    groups = [toks[k:k + TG] for k in range(0, len(toks), TG)]
    with Phase(g) as ph:
        rw32 = ph.alloc("rw32", [128, 16, NE], F32)
        S.dma('sp', rw32[:], rw_d.rearrange("(kc p) e -> p kc e", p=128))
        rbb = load_row_bcast(g, ph, "rbb", rb_d, NE)
        lng = load_row_bcast(g, ph, "mlng", lng_d)
        lnb = load_row_bcast(g, ph, "mlnb", lnb_d)
        gate_t = ph.alloc("mgate", [128, D], F32)
        mods = {}
        for r in range(nb + 1):
            mods[r] = load_mod_cols(g, ph, MOD, r, 4, 3)
        h2T = ph.alloc("h2T", [128, 16, TG * 128], BF16)
        h32 = ph.alloc("h32", [128, 16, 128], F32)
        Gt = ph.alloc("Gt", [128, TG, NE], F32)
        gb_ = (ph.alloc("gsc", [128, 16], F32), ph.alloc("gsel", [128, 16], F32), ph.alloc("gw", [128, 64], F32))
        acc = ph.alloc("acc", [128, TG, D], F32)
        actT = ph.alloc("actT", [128, 8, TG * 128], BF16)
        wst = [ph.alloc("wst%d" % k, [128, PIECE], BF16) for k in range(2)]
        sg = [ph.alloc("sg%d" % k, [128, TG * 128], F32) for k in range(2)]
        lnbufs = alloc_ln_bufs(ph)
        xt = lnbufs[0]
        nw = 0
        for grp in groups:
            ng = len(grp)
            ntok = ng * 128
            for ti, (b, t) in enumerate(grp):
                scm, shm = mods[mod_row(b, t, nb)]
                S.dma('sp', xt[:], X[b, t * 128:(t + 1) * 128, :])
                build_hT_tile(g, xt, h2T[:, :, ti * 128:(ti + 1) * 128], scm, shm, psbase=0, hT32=h32)
                lp = g.ps[4]
                for kc in range(16):
                    S.mm(lp[:, 0:NE], h32[:, kc, :], rw32[:, kc, :], start=(kc == 0), stop=(kc == 15))
                if dbg != 2:
                    gating_tile(g, gb_, lp[:, 0:NE], rbb, Gt[:, ti, :])
            first = True
            for e in experts:
                for hf in range(NQ):
                    w_ = wst[nw % 2]
                    wb = w_[:, 0:16 * 2 * FQ].rearrange("p (kc two f) -> p kc two f", kc=16, two=2)
                    wdb = w_[:, 16 * 2 * FQ:PIECE].rearrange("p (fc d) -> p fc d", fc=NFC)
                    nw += 1
                    S.dma('sp', w_[0:64, :], Wb[e * NQ + hf, 0:64, :], par=True)
                    S.dma('act', w_[64:128, :], Wb[e * NQ + hf, 64:128, :], par=True)
                    for fc in range(NFC):
                        g_ps = g.ps[(fc % 2) * 2]
                        u_ps = g.ps[(fc % 2) * 2 + 1]
                        for kc in range(16):
                            S.mm(g_ps[:, 0:ntok], wb[:, kc, 0, fc * 128:(fc + 1) * 128], h2T[:, kc, 0:ntok],
                                 start=(kc == 0), stop=(kc == 15))
                        for kc in range(16):
                            S.mm(u_ps[:, 0:ntok], wb[:, kc, 1, fc * 128:(fc + 1) * 128], h2T[:, kc, 0:ntok],
                                 start=(kc == 0), stop=(kc == 15))
                        s_ = sg[fc % 2]
                        S.act(s_[:, 0:ntok], g_ps[:, 0:ntok], AF.Silu)
                        S.tt('dve', actT[:, hf * NFC + fc, 0:ntok], s_[:, 0:ntok], u_ps[:, 0:ntok], ALU.mult)
                    for ti in range(ng):
                        for dc in range(4):
                            y_ps = g.ps[4 + dc]
                            for fc in range(NFC):
                                S.mm(y_ps[:], actT[:, hf * NFC + fc, ti * 128:(ti + 1) * 128],
                                     wdb[:, fc, dc * 512:(dc + 1) * 512], start=(fc == 0), stop=(fc == NFC - 1))
                            a_ = acc[:, ti, dc * 512:(dc + 1) * 512]
                            if first:
                                S.ts('dve', a_, y_ps[:], Gt[:, ti, e:e + 1], ALU.mult)
                            else:
                                S.stt(a_, y_ps[:], Gt[:, ti, e:e + 1], a_, ALU.mult, ALU.add)
                    first = False
            if dbg == 1:
                continue
            for ti, (b, t) in enumerate(grp):
                r = mod_row(b, t, nb)
                S.dma('sp', gate_t[:], MOD[r, 5 * D:6 * D][None, :].broadcast_to([128, D]))
                ych = [acc[:, ti, dc * 512:(dc + 1) * 512] for dc in range(4)]
                resid_ln_tile(g, ph, lnbufs, ych, X[b, t * 128:(t + 1) * 128, :], gate_t, lng, lnb)

PW = 12416
PROWS = 2312


def prow(t):
    return 2 + t * 128 if t < 2 else 262 + (t - 2) * 128


def phase_proj_blocks(g, X, MOD, m_scale, m_shift, w_dram, ntot, P, tiles_per_b, blk=3072):
    S, nb = g.S, g.nb
    for c0 in range(0, ntot, blk):
        ncols = min(blk, ntot - c0)
        with Phase(g) as ph:
            wsb = ph.alloc("pw", [128, 16, ncols], BF16)
            load_w_bf16(g, wsb, w_dram, ncols, col0=c0)
            hT = [ph.alloc("phT%d" % k, [128, 16, 128], BF16) for k in range(2)]
            xt = [ph.alloc("pxt%d" % k, [128, D], F32) for k in range(2)]
            pt = [ph.alloc("ppt%d" % k, [128, ncols], F32) for k in range(2)]
            mods = {}
            for r in range(nb + 1):
                mods[r] = load_mod_cols(g, ph, MOD, r, m_scale, m_shift)
            n = 0
            for b in range(nb):
                for t in tiles_per_b:
                    sc, sh = mods[mod_row(b, t, nb)]
                    x_ = xt[n % 2]
                    h_ = hT[n % 2]
                    p_ = pt[n % 2]
                    S.dma('sp', x_[:], X[b, t * 128:(t + 1) * 128, :])
                    build_hT_tile(g, x_, h_, sc, sh, psbase=0)
                    ci = 0
                    for cc in range(0, ncols, 512):
                        cw = min(512, ncols - cc)
                        ps = g.ps[4 + (ci % 4)]
                        for kc in range(16):
                            S.mm(ps[:, 0:cw], h_[:, kc, :], wsb[:, kc, cc:cc + cw], start=(kc == 0), stop=(kc == 15))
                        if ci % 2 == 0:
                            S.act(p_[:, cc:cc + cw], ps[:, 0:cw], AF.Copy)
                        else:
                            S.copy('dve', p_[:, cc:cc + cw], ps[:, 0:cw])
                        ci += 1
                    r0 = prow(t)
                    S.dma('sp', P[b, r0:r0 + 128, c0:c0 + ncols], p_[:, 0:ncols])
                    n += 1


def phase_readout(g, i, X, MOD, O, K, P, gate_col0, gate_func, ng_d, w_out_d, lng_d, lnb_d, last, Ypart):
    S, nb, c = g.S, g.nb, g.c
    nparts = K // 2048
    tiles = list(range(2, NT)) if last else list(range(NT))
    for part in range(nparts):
        with Phase(g) as ph:
            wsb = ph.alloc("rw", [128, 16, D], BF16)
            load_w_bf16(g, wsb, w_out_d[part * 2048:(part + 1) * 2048, :], D)
            ngb = load_row_bcast(g, ph, "ngb", ng_d, 128)
            oa = ph.alloc("roa", [128, D], F32)
            ob = ph.alloc("rob", [128, D], F32)
            sq = ph.alloc("rsq", [128, 16, 128], F32)
            red = ph.alloc("rred", [128, 16], F32)
            OT = ph.alloc("rOT", [128, 16, 128], BF16)
            one16 = ph.alloc("one16", [128, 16], F32)
            zero16 = ph.alloc("zero16", [128, 16], F32)
            S.memset('dve', one16[:], 1.0)
            S.memset('dve', zero16[:], 0.0)
            fin = part == nparts - 1
            if fin:
                lng = load_row_bcast(g, ph, "rlng", lng_d)
                lnb = load_row_bcast(g, ph, "rlnb", lnb_d)
                gate_c = load_row_bcast(g, ph, "rgc", MOD[nb, 2 * D:3 * D])
                gate_l = ph.alloc("rgl", [128, D], F32)
                lnbufs = alloc_ln_bufs(ph)
            if nparts > 1:
                ysb = ph.alloc("rys", [128, D], F32)
            for b in range(nb):
                if fin:
                    S.dma('sp', gate_l[:], MOD[b, 2 * D:3 * D][None, :].broadcast_to([128, D]))
                for t in tiles:
                    rows = slice(t * 128, (t + 1) * 128)
                    cols = slice(part * 2048, (part + 1) * 2048)
                    S.dma('sp', oa[:], O[0, b, rows, cols])
                    S.dma('sp', ob[:], O[1, b, rows, cols])
                    S.tt('dve', oa[:], oa[:], ob[:], ALU.add)
                    o3 = oa[:].rearrange("p (h d) -> p h d", h=16)
                    rms_heads(g, None, o3, 16, ngb, sq, red)
                    r0 = prow(t)
                    S.dma('sp', ob[:], P[b, r0:r0 + 128, gate_col0 + part * 2048:gate_col0 + (part + 1) * 2048])
                    S.act(ob[:], ob[:], gate_func)
                    S.tt('pool', oa[:], oa[:], ob[:], ALU.mult)
                    build_hT_tile(g, oa, OT, one16, zero16, psbase=0)
                    ych = []
                    for dc in range(4):
                        ps = g.ps[4 + dc]
                        for kc in range(16):
                            S.mm(ps[:], OT[:, kc, :], wsb[:, kc, dc * 512:(dc + 1) * 512], start=(kc == 0), stop=(kc == 15))
                        ych.append(ps[:])
                    if nparts > 1 and not fin:
                        for dc in range(4):
                            if dc % 2 == 0:
                                S.act(ysb[:, dc * 512:(dc + 1) * 512], ych[dc], AF.Copy)
                            else:
                                S.copy('dve', ysb[:, dc * 512:(dc + 1) * 512], ych[dc])
                        S.dma('sp', Ypart[b, rows, :], ysb[:])
                        continue
                    if nparts > 1:
                        S.dma('sp', ysb[:], Ypart[b, rows, :])
                        for dc in range(4):
                            S.tt('dve', ysb[:, dc * 512:(dc + 1) * 512], ysb[:, dc * 512:(dc + 1) * 512], ych[dc], ALU.add)
                        ych = [ysb[:, dc * 512:(dc + 1) * 512] for dc in range(4)]
                    resid_ln_tile(g, ph, lnbufs, ych, X[b, rows, :], gate_c if t < 2 else gate_l, lng, lnb)


def hg_consts():
    p = np.arange(128)
    same = (p[:, None] // 64) == (p[None, :] // 64)
    A_f = (same & (p[:, None] <= p[None, :])).astype(np.float32)
    B_f = (same & (p[:, None] > p[None, :])).astype(np.float32)
    hgA = np.stack([A_f, A_f.T]).astype(np.float32)
    hgB = np.stack([B_f, B_f.T]).astype(np.float32)
    ind = np.stack([(p < 64), (p >= 64)], axis=1).astype(np.float32)
    return {"c_hgA": hgA, "c_hgB": hgB, "c_ind": ind}


def phase_hg_scan(g, i, P, O, hg_lb):
    S, nb, c = g.S, g.nb, g.c
    with Phase(g) as ph:
        Am = [ph.alloc("hgA%d" % d, [128, 128], F32) for d in range(2)]
        Bm = [ph.alloc("hgB%d" % d, [128, 128], F32) for d in range(2)]
        hgA_d = dram_in(g, "c_hgA", [2, 128, 128])
        hgB_d = dram_in(g, "c_hgB", [2, 128, 128])
        ind_d = dram_in(g, "c_ind", [128, 2])
        for d in range(2):
            S.dma('sp', Am[d][:], hgA_d[d, :, :])
            S.dma('sp', Bm[d][:], hgB_d[d, :, :])
        ind = ph.alloc("hgind", [128, 2], F32)
        S.dma('sp', ind[:], ind_d[:, :])
        lb = [ph.alloc("hlb%d" % d, [128, D], F32) for d in range(2)]
        oml = [ph.alloc("homl%d" % d, [128, D], F32) for d in range(2)]
        W = [ph.alloc("hw%d" % k, [128, D], F32) for k in range(8)]
        qb, vb, fb, kk, kinv, kd0, kd1, exb = W
        kd = [kd0, kd1]
        for d in range(2):
            tmp, den, num = W[0], W[1], W[2]
            S.memset('dve', num[:], 0.0)
            for l in range(DEPTH):
                S.dma('sp', tmp[:], hg_lb[d, l, :][None, :].broadcast_to([128, D]))
                S.act(tmp[:], tmp[:], AF.Exp)
                if l == 0:
                    S.copy('dve', den[:], tmp[:])
                else:
                    S.tt('dve', den[:], den[:], tmp[:], ALU.add)
                    if l <= i:
                        S.tt('dve', num[:], num[:], tmp[:], ALU.add)
            S.op('dve', lambda t_, den=den: t_.reciprocal(den[:], den[:]), reads=[den], writes=[den])
            S.tt('dve', lb[d][:], num[:], den[:], ALU.mult)
            S.ts('dve', oml[d][:], lb[d][:], -1.0, ALU.mult, 1.0, ALU.add)
        Ssb = ph.alloc("hS", [128, 16, 128], F32)
        Fsb = ph.alloc("hF", [128, 32], F32)
        qT = ph.alloc("hqT", [128, 16, 128], F32)
        kT = ph.alloc("hkT", [128, 16, 128], F32)
        itr = ph.alloc("hitr", [128, 16, 128], F32)
        osb = [ph.alloc("hosb%d" % k, [64, D], F32) for k in range(2)]
        for b in range(nb):
            for d in range(2):
                order = list(range(NT)) if d == 0 else [1, 0] + list(range(NT - 1, 1, -1))
                S.memset('dve', Ssb[:], 0.0)
                for t in order:
                    r0 = prow(t)
                    S.dma('sp', qb[:], P[b, r0:r0 + 128, 0:2048])
                    S.dma('sp', vb[:], P[b, r0:r0 + 128, 2048:4096])
                    S.dma('sp', fb[:], P[b, r0:r0 + 128, 6144 + d * 2048:6144 + (d + 1) * 2048])
                    S.act(fb[:], fb[:], AF.Sigmoid)
                    S.tt('pool', fb[:], fb[:], oml[d][:], ALU.mult)
                    S.tt('dve', fb[:], fb[:], lb[d][:], ALU.add)
                    S.ts('dve', kk[:], fb[:], -1.0, ALU.mult, 1.0, ALU.add)
                    S.act(fb[:], fb[:], AF.Ln)
                    tp = g.ps[2]
                    for h in range(16):
                        S.mm(tp[:, h * 2:(h + 1) * 2], fb[:, h * 128:(h + 1) * 128], ind[:, :])
                    S.act(Fsb[:], tp[:, 0:32], AF.Exp)
                    for cc in range(4):
                        sl = slice(cc * 512, (cc + 1) * 512)
                        gp = g.ps[0]
                        sp_ = g.ps[1]
                        S.mm(gp[:], Am[d][:], fb[:, sl])
                        S.mm(sp_[:], Bm[d][:], fb[:, sl])
                        S.act(exb[:, sl], gp[:], AF.Exp)
                        S.tt('dve', qb[:, sl], qb[:, sl], exb[:, sl], ALU.mult)
                        S.act(exb[:, sl], gp[:], AF.Exp, scale=-1.0)
                        S.tt('pool', kinv[:, sl], kk[:, sl], exb[:, sl], ALU.mult)
                        S.act(exb[:, sl], sp_[:], AF.Exp)
                        S.stt(kd0[:, sl], exb[:, sl], ind[:, 0:1], kk[:, sl], ALU.mult, ALU.mult)
                        S.stt(kd1[:, sl], exb[:, sl], ind[:, 1:2], kk[:, sl], ALU.mult, ALU.mult)
                    for src, dst, bk in ((qb, qT, 3), (kinv, kT, 4)):
                        for gb in range(4):
                            ps = g.ps[bk]
                            for j in range(4):
                                h = gb * 4 + j
                                S.transpose(ps[:, j * 128:(j + 1) * 128], src[:, h * 128:(h + 1) * 128], c['ident'][:])
                            pv = ps[:].rearrange("p (h d) -> p h d", h=4)
                            if bk == 3:
                                S.act(dst[:, gb * 4:(gb + 1) * 4, :], pv, AF.Copy)
                            else:
                                S.copy('dve', dst[:, gb * 4:(gb + 1) * 4, :], pv)
                    for gb in range(4):
                        ps = g.ps[5 + (gb % 2)]
                        for j in range(4):
                            h = gb * 4 + j
                            S.mm(ps[:, j * 128:(j + 1) * 128], kT[:, h, :], qT[:, h, :])
                        S.tt('dve', itr[:, gb * 4:(gb + 1) * 4, :], ps[:].rearrange("p (h d) -> p h d", h=4),
                             Am[d][:].unsqueeze(1).broadcast_to([128, 4, 128]), ALU.mult)
                    for cidx in ((0, 1) if d == 0 else (1, 0)):
                        csl = slice(cidx * 64, (cidx + 1) * 64)
                        for gb in range(4):
                            ps = g.ps[5 + (gb % 2)]
                            for j in range(4):
                                h = gb * 4 + j
                                S.mm(ps[0:64, j * 128:(j + 1) * 128], itr[:, h, csl], vb[:, h * 128:(h + 1) * 128],
                                     start=True, stop=False)
                                S.mm(ps[0:64, j * 128:(j + 1) * 128], qT[:, h, csl], Ssb[:, h, :],
                                     start=False, stop=True)
                            if gb % 2 == 0:
                                S.act(osb[cidx][:, gb * 512:(gb + 1) * 512], ps[0:64, :], AF.Copy)
                            else:
                                S.copy('dve', osb[cidx][:, gb * 512:(gb + 1) * 512], ps[0:64, :])
                        for gb in range(4):
                            ps = g.ps[7 if gb % 2 == 0 else 0]
                            for j in range(4):
                                h = gb * 4 + j
                                S.mm(ps[:, j * 128:(j + 1) * 128], kd[cidx][:, h * 128:(h + 1) * 128],
                                     vb[:, h * 128:(h + 1) * 128])
                            for j in range(4):
                                h = gb * 4 + j
                                S.stt(Ssb[:, h, :], Ssb[:, h, :], Fsb[:, h * 2 + cidx:h * 2 + cidx + 1],
                                      ps[:, j * 128:(j + 1) * 128], ALU.mult, ALU.add)
                        S.dma('sp', O[d, b, t * 128 + cidx * 64:t * 128 + (cidx + 1) * 64, 0:2048], osb[cidx][:])


def phase_hgrn2(g, i, j, X, MOD, lng_d, lnb_d, last):
    w_in = dram_in(g, "hg_w_in%d" % j, [D, 10240])
    w_out = dram_in(g, "hg_w_out%d" % j, [D, D])
    hg_lb = dram_in(g, "hg_lb", [2, DEPTH, D])
    ng_d = dram_in(g, "hg_norm_g%d" % j, [128])
    phase_proj_blocks(g, X, MOD, 1, 0, w_in, 10240, g.P, list(range(NT)))
    phase_hg_scan(g, i, g.P, g.O, hg_lb)
    phase_readout(g, i, X, MOD, g.O, 2048, g.P, 4096, AF.Sigmoid, ng_d, w_out, lng_d, lnb_d, last, g.Ypart)

DNH = 32
NEGBIG = -30000.0


def dn_consts():
    p = np.arange(128)
    A_f = (p[:, None] <= p[None, :]).astype(np.float32)
    dnA = np.stack([A_f, A_f.T]).astype(np.float32)
    nms_f = np.where(p[None, :] < p[:, None], 0.0, NEGBIG).astype(np.float32)
    nms_b = np.where(p[None, :] > p[:, None], 0.0, NEGBIG).astype(np.float32)
    nmt_f = np.where(p[:, None] <= p[None, :], 0.0, NEGBIG).astype(np.float32)
    nmt_b = np.where(p[:, None] >= p[None, :], 0.0, NEGBIG).astype(np.float32)
    nm2 = np.stack([np.concatenate([nms_f, nmt_f], axis=1), np.concatenate([nms_b, nmt_b], axis=1)]).astype(np.float32)
    return {"c_dnA": dnA, "c_dnNM2": nm2}


def phase_dn_prep(g, j, P, QKV, GB, conv_d, alog_d, dtb_d):
    S, nb, c = g.S, g.nb, g.c
    with Phase(g) as ph:
        zt = ph.alloc("dz", [4, 8192], F32)
        S.memset('dve', zt[:], 0.0)
        for b in range(nb):
            S.dma('sp', P[b, 0:2, 0:8192], zt[0:2, :])
            S.dma('sp', P[b, 258:262, 0:8192], zt[0:4, :])
            S.dma('sp', P[b, 2310:2312, 0:8192], zt[0:2, :])
        wt = ph.alloc("cw", [128, 5, 2048], F32)
        xs = [ph.alloc("cx%d" % k, [128, 2048], F32) for k in range(3)]
        acc = [ph.alloc("cacc%d" % k, [128, 2048], F32) for k in range(2)]
        tmp = ph.alloc("ctmp", [128, 2048], F32)
        sq = ph.alloc("csq", [128, 16, 128], F32)
        red = ph.alloc("cred", [128, 16], F32)
        nega = ph.alloc("nega", [128, 64], F32)
        dtb = ph.alloc("dtb", [128, 64], F32)
        S.dma('sp', nega[:], alog_d.rearrange("a h -> (a h)")[None, :].broadcast_to([128, 64]))
        S.dma('sp', dtb[:], dtb_d.rearrange("a h -> (a h)")[None, :].broadcast_to([128, 64]))
        S.act(nega[:], nega[:], AF.Exp)
        S.ts('dve', nega[:], nega[:], -1.0, ALU.mult)
        ba = [ph.alloc("ba%d" % k, [128, 128], F32) for k in range(2)]
        n = 0
        for b in range(nb):
            for t in range(NT):
                r0 = prow(t)
                bt = ba[n % 2]
                n += 1
                S.dma('sp', bt[:], P[b, r0:r0 + 128, 12288:12416])
                S.act(bt[:, 0:64], bt[:, 0:64], AF.Sigmoid)
                S.tt('dve', bt[:, 64:128], bt[:, 64:128], dtb[:], ALU.add)
                S.act(bt[:, 64:128], bt[:, 64:128], AF.Exp)
                S.ts('dve', bt[:, 64:128], bt[:, 64:128], 1.0, ALU.add)
                S.act(bt[:, 64:128], bt[:, 64:128], AF.Ln)
                S.tt('dve', bt[:, 64:128], bt[:, 64:128], nega[:], ALU.mult)
                S.dma('sp', GB[b, t * 128:(t + 1) * 128, :], bt[:])
        n = 0
        for cb in range(4):
            for jt in range(5):
                S.dma('sp', wt[:, jt, :], conv_d[jt, cb * 2048:(cb + 1) * 2048][None, :].broadcast_to([128, 2048]))
            for b in range(nb):
                for t in range(NT):
                    r0 = prow(t)
                    a_ = acc[n % 2]
                    n += 1
                    for jt in range(5):
                        x_ = xs[jt % 3]
                        S.dma('sp' if jt % 2 == 0 else 'act', x_[:], P[b, r0 + jt - 2:r0 + jt - 2 + 128, cb * 2048:(cb + 1) * 2048])
                        if jt == 0:
                            S.tt('dve', a_[:], x_[:], wt[:, jt, :], ALU.mult)
                        else:
                            S.tt('pool', tmp[:], x_[:], wt[:, jt, :], ALU.mult)
                            S.tt('dve', a_[:], a_[:], tmp[:], ALU.add)
                    S.act(a_[:], a_[:], AF.Silu)
                    if cb < 2:
                        a3 = a_[:].rearrange("p (h d) -> p h d", h=16)
                        S.tt('pool', sq[:], a3, a3, ALU.mult)
                        S.op('dve', lambda t_: t_.tensor_reduce(red[:], sq[:], AX.X, ALU.add), reads=[sq], writes=[red])
                        S.ts('dve', red[:], red[:], EPS, ALU.add)
                        S.act(red[:], red[:], AF.Sqrt)
                        S.op('dve', lambda t_: t_.reciprocal(red[:], red[:]), reads=[red], writes=[red])
                        if cb == 0:
                            S.ts('dve', red[:], red[:], 128.0 ** -0.5, ALU.mult)
                        S.tt('dve', a3, a3, red[:].unsqueeze(2).broadcast_to([128, 16, 128]), ALU.mult)
                    S.dma('sp', QKV[b, t * 128:(t + 1) * 128, cb * 2048:(cb + 1) * 2048], a_[:])


def phase_dn_scan(g, QKV, GB, O):
    S, nb, c = g.S, g.nb, g.c
    with Phase(g) as ph:
        dnA_d = dram_in(g, "c_dnA", [2, 128, 128])
        nm2_d = dram_in(g, "c_dnNM2", [2, 128, 256])
        Am = [ph.alloc("dnA%d" % d, [128, 128], F32) for d in range(2)]
        NM2 = [ph.alloc("dnNM%d" % d, [128, 256], F32) for d in range(2)]
        for d in range(2):
            S.dma('sp', Am[d][:], dnA_d[d, :, :])
            S.dma('sp', NM2[d][:], nm2_d[d, :, :])
        ones = c['ones_f']
        negones = ph.alloc("negones", [128, 128], F32)
        S.memset('dve', negones[:], -1.0)
        ident = c['ident']
        qn = ph.alloc("dqn", [128, 2048], F32)
        kn = ph.alloc("dkn", [128, 2048], F32)
        vv = ph.alloc("dvv", [128, 4096], F32)
        gbt = ph.alloc("dgb", [128, 128], F32)
        ot = ph.alloc("dot", [128, 4096], F32)
        Sst = [ph.alloc("dS%d" % h, [128, 128], F32) for h in range(DNH)]
        hq = [ph.alloc("dhq%d" % k, [128, 4, 128], F32) for k in range(2)]
        cb = []
        for k in range(4):
            cb.append(dict(
                G1=ph.alloc("G1_%d" % k, [128, 128], F32),
                E2=ph.alloc("E2_%d" % k, [128, 256], F32),
                sm=ph.alloc("sm_%d" % k, [128, 2], F32),
                sx=ph.alloc("sx_%d" % k, [128, 3], F32),
                M=ph.alloc("M_%d" % k, [128, 128], F32),
                qk=ph.alloc("qk_%d" % k, [128, 128], F32),
                Q1=ph.alloc("Q1_%d" % k, [128, 128], F32),
                R=[ph.alloc("R%d_%d" % (r, k), [128, 256], F32) for r in range(2)],
                PQ=[ph.alloc("PQ%d_%d" % (r, k), [128, 256], F32) for r in range(2)],
                wT=ph.alloc("wT_%d" % k, [128, 128], F32),
                kd=ph.alloc("kd_%d" % k, [128, 128], F32),
                vn=ph.alloc("vn_%d" % k, [128, 128], F32),
                banks=(g.ps[2 * k], g.ps[2 * k + 1]),
            ))

        def chain(d, h, hv, B, hqs):
            bx, by = B['banks']
            kT, qT, KK, QKt = hqs[:, 0, :], hqs[:, 1, :], hqs[:, 2, :], hqs[:, 3, :]
            gcol = gbt[:, 64 + d * 32 + hv:64 + d * 32 + hv + 1]
            bcol = gbt[:, d * 32 + hv:d * 32 + hv + 1]
            G1, E2, sm, sx, M, qk, Q1 = B['G1'], B['E2'], B['sm'], B['sx'], B['M'], B['qk'], B['Q1']
            S.ts('dve', G1[:], Am[d][:], gcol, ALU.mult)
            yield
            S.mm(bx[:, 0:128], G1[:], ones[:], start=True, stop=False)
            S.mm(bx[:, 0:128], negones[:], G1[:], start=False, stop=True)
            S.mm(bx[:, 128:256], ones[:], G1[:], start=True, stop=False)
            S.mm(bx[:, 128:256], G1[:], negones[:], start=False, stop=True)
            S.mm(bx[:, 256:257], G1[:], ones[:, 0:1])
            S.mm(bx[:, 257:258], ones[:], gcol)
            yield
            S.tt('dve', E2[:], bx[:, 0:256], NM2[d][:], ALU.add)
            S.copy('dve', sm[:], bx[:, 256:258])
            yield
            S.act(E2[:], E2[:], AF.Exp)
            S.act(sx[:, 0:1], sm[:, 0:1], AF.Exp)
            S.act(sx[:, 1:2], sm[:, 0:1], AF.Exp, scale=-1.0, bias=sm[:, 1:2])
            S.act(sx[:, 2:3], sm[:, 1:2], AF.Exp)
            yield
            egc, ekd, gl = sx[:, 0:1], sx[:, 1:2], sx[:, 2:3]
            S.stt(M[:], KK, bcol, E2[:, 0:128], ALU.mult, ALU.mult)
            S.tt('pool', qk[:], QKt, E2[:, 128:256], ALU.mult)
            R = B['R'][0]
            S.ts('dve', R[:, 0:128], vv[:, hv * 128:(hv + 1) * 128], bcol, ALU.mult)
            S.ts('dve', R[:, 128:256], kn[:, h * 128:(h + 1) * 128], bcol, ALU.mult, egc, ALU.mult)
            S.ts('pool', B['kd'][:], kn[:, h * 128:(h + 1) * 128], ekd, ALU.mult)
            yield
            S.transpose(bx[:, 0:128], M[:], ident[:])
            yield
            S.copy('dve', Q1[:], bx[:, 0:128])
            yield
            Pk, Qk = M[:], Q1[:]
            ri = 0
            for lev in range(7):
                R = B['R'][ri]
                Rn = B['R'][1 - ri]
                S.mm(by[:, 0:256], Qk, R[:])
                if lev < 6:
                    S.mm(by[:, 256:384], Pk, Qk)
                    if lev < 5:
                        S.mm(by[:, 384:512], Qk, Pk)
                yield
                S.tt('dve', Rn[:], R[:], by[:, 0:256], ALU.subtract if lev == 0 else ALU.add)
                if lev < 6:
                    PQ = B['PQ'][lev % 2]
                    if lev < 5:
                        S.act(PQ[:], by[:, 256:512], AF.Copy)
                    else:
                        S.act(PQ[:, 0:128], by[:, 256:384], AF.Copy)
                    Qk, Pk = PQ[:, 0:128], PQ[:, 128:256]
                ri = 1 - ri
                yield
            R = B['R'][ri]
            u, w = R[:, 0:128], R[:, 128:256]
            S.transpose(bx[:, 0:128], w, ident[:])
            yield
            S.copy('dve', B['wT'][:], bx[:, 0:128])
            yield
            St = Sst[hv]
            S.mm(by[:, 0:128], B['wT'][:], St[:])
            S.mm(by[:, 128:256], qT, St[:])
            yield
            S.tt('dve', B['vn'][:], u, by[:, 0:128], ALU.subtract)
            yield
            S.mm(by[:, 256:384], qk[:], B['vn'][:])
            S.mm(by[:, 384:512], B['kd'][:], B['vn'][:])
            yield
            S.ts('dve', ot[:, hv * 128:(hv + 1) * 128], by[:, 128:256], egc, ALU.mult)
            S.tt('dve', ot[:, hv * 128:(hv + 1) * 128], ot[:, hv * 128:(hv + 1) * 128], by[:, 256:384], ALU.add)
            S.stt(St[:], St[:], gl, by[:, 384:512], ALU.mult, ALU.add)
            yield

        for b in range(nb):
            for d in range(2):
                order = list(range(NT)) if d == 0 else [1, 0] + list(range(NT - 1, 1, -1))
                for h in range(DNH):
                    S.memset('pool', Sst[h][:], 0.0)
                for t in order:
                    rows = slice(t * 128, (t + 1) * 128)
                    S.dma('sp', qn[:], QKV[b, rows, 0:2048])
                    S.dma('act', kn[:], QKV[b, rows, 2048:4096])
                    S.dma('sp', vv[:], QKV[b, rows, 4096:8192])
                    S.dma('act', gbt[:], GB[b, rows, :])
                    for hh in range(0, 16, 2):
                        gens = []
                        for k2 in range(2):
                            h = hh + k2
                            hqs = hq[k2]
                            pa = cb[2 * k2]['banks'][0]
                            pb = cb[2 * k2 + 1]['banks'][0]
                            S.transpose(pa[:, 0:128], kn[:, h * 128:(h + 1) * 128], ident[:])
                            S.transpose(pa[:, 128:256], qn[:, h * 128:(h + 1) * 128], ident[:])
                            S.act(hqs[:, 0:2, :], pa[:, 0:256].rearrange("p (a d) -> p a d", a=2), AF.Copy)
                            S.mm(pb[:, 0:128], hqs[:, 0, :], hqs[:, 0, :])
                            S.mm(pb[:, 128:256], hqs[:, 0, :], hqs[:, 1, :])
                            S.act(hqs[:, 2:4, :], pb[:, 0:256].rearrange("p (a d) -> p a d", a=2), AF.Copy)
                            gens += [chain(d, h, 2 * h + k, cb[2 * k2 + k], hqs) for k in range(2)]
                        while gens:
                            for gen in list(gens):
                                try:
                                    next(gen)
                                except StopIteration:
                                    gens.remove(gen)
                    S.dma('sp', O[d, b, rows, :], ot[:])


def phase_deltanet(g, i, j, X, MOD, lng_d, lnb_d, last):
    w_in = dram_in(g, "dn_w_in%d" % j, [D, 12416])
    w_out = dram_in(g, "dn_w_out%d" % j, [4096, D])
    conv_d = dram_in(g, "dn_conv%d" % j, [5, 8192])
    alog_d = dram_in(g, "dn_a_log%d" % j, [2, 32])
    dtb_d = dram_in(g, "dn_dt_bias%d" % j, [2, 32])
    ng_d = dram_in(g, "dn_norm_g%d" % j, [128])
    phase_proj_blocks(g, X, MOD, 1, 0, w_in, 12416, g.P, list(range(NT)))
    phase_dn_prep(g, j, g.P, g.QKV, g.GB, conv_d, alog_d, dtb_d)
    phase_dn_scan(g, g.QKV, g.GB, g.O)
    phase_readout(g, i, X, MOD, g.O, 4096, g.P, 8192, AF.Silu, ng_d, w_out, lng_d, lnb_d, last, g.Ypart)

GRID_W = 64
ROPE_FREQS = 32
ROPE_BASE = 10000.0


def rope_tables():
    row = np.repeat(np.arange(L // GRID_W), GRID_W).astype(np.float32)
    col = (np.arange(L) % GRID_W).astype(np.float32)
    inv = (ROPE_BASE ** (-np.arange(ROPE_FREQS, dtype=np.float32) / ROPE_FREQS)).astype(np.float32)
    ang = np.stack([row[:, None] * inv, col[:, None] * inv], axis=1).astype(np.float32)
    return np.cos(ang).reshape(L, 64).astype(np.float32), np.sin(ang).reshape(L, 64).astype(np.float32)


def const_inputs():
    jj, ii = np.meshgrid(np.arange(128), np.arange(128), indexing='ij')
    cos, sin = rope_tables()
    return {
        "c_ident": np.eye(128, dtype=np.float32),
        "c_triu": (jj <= ii).astype(np.float32),
        "c_trius": (jj < ii).astype(np.float32),
        "c_cos": cos, "c_sin": sin,
        **hg_consts(), **dn_consts(),
    }


def build(nb, layers, moe_layers, debug=False, moe_experts=None, skip_mixer=False):
    nc = bass.Bass("TRN2", target_bir_lowering=False)
    g = init_globals(nc, nb)
    S = g.S
    load_consts(g)
    x_in = dram_in(g, "x_in", [nb, L, D])
    ctx_in = dram_in(g, "ctx_in", [nb, LC, D])
    c_all = dram_in(g, "c_all", [nb + 1, D])
    cos_d = dram_in(g, "c_cos", [L, 64])
    sin_d = dram_in(g, "c_sin", [L, 64])
    X = dram_scratch(g, "Xs", [nb, T, D])
    P = dram_scratch(g, "Ps", [nb, PROWS, PW])
    g.P = P
    g.O = dram_scratch(g, "Os", [2, nb, T, 4096])
    g.Ypart = dram_scratch(g, "Yp", [nb, T, D])
    g.QKV = dram_scratch(g, "QKVs", [nb, T, 8192])
    g.GB = dram_scratch(g, "GBs", [nb, T, 128])
    g.Wb = dram_scratch(g, "Wbs", [16 * NQ, 128, PIECE], BF16)
    out = nc.dram_tensor("out", [nb, L, D], F32, kind="ExternalOutput").ap()
    DRAM_NAMES.add("out")
    DRAM_NAMES.add("xdbg")
    for b in range(nb):
        S.dma('sp', X[b, 0:LC, :], ctx_in[b, :, :])
        for k in range(4):
            S.dma('sp', X[b, LC + k * 512:LC + (k + 1) * 512, :], x_in[b, k * 512:(k + 1) * 512, :])
    rw_d = dram_in(g, "router_w", [D, 16])
    rb_d = dram_in(g, "router_b", [16])
    for i in layers:
        last = i == DEPTH - 1
        MOD = dram_scratch(g, "MOD%d" % i, [nb + 1, NMOD * D])
        ada_w = dram_in(g, "ada_w%d" % i, [D, NMOD * D])
        ada_b = dram_in(g, "ada_b%d" % i, [NMOD * D])
        ln_g = dram_in(g, "ln_g%d" % i, [2, D])
        ln_b = dram_in(g, "ln_b%d" % i, [2, D])
        phase_mod(g, i, c_all, ada_w, ada_b, MOD)
        kind = i % 3
        j = i // 3
        if skip_mixer:
            pass
        elif kind == 0:
            w_in = dram_in(g, "attn_w_in%d" % j, [D, 3072])
            w_out = dram_in(g, "attn_w_out%d" % j, [D, D])
            qg = dram_in(g, "attn_q_g%d" % j, [128])
            kg = dram_in(g, "attn_k_g%d" % j, [128])
            phase_proj_tm(g, i, X, MOD, 1, 0, w_in, 3072, P, list(range(NT)))
            phase_attn_core(g, i, X, MOD, P, w_out, qg, kg, ln_g[0, :], ln_b[0, :], cos_d, sin_d, last)
        elif kind == 1:
            phase_deltanet(g, i, j, X, MOD, ln_g[0, :], ln_b[0, :], last)
        else:
            phase_hgrn2(g, i, j, X, MOD, ln_g[0, :], ln_b[0, :], last)
        if i in moe_layers:
            wg = wu = wd = None
            if moe_experts is None or len(moe_experts) > 0:
                wg = dram_in(g, "moe_w_gate%d" % i, [16, D, 1024])
                wu = dram_in(g, "moe_w_up%d" % i, [16, D, 1024])
                wd = dram_in(g, "moe_w_down%d" % i, [16, 1024, D])
            tiles = list(range(2, NT)) if last else list(range(NT))
            exps = moe_experts if moe_experts is not None else range(16)
            if wg is not None:
                phase_moe_precast(g, wg, wu, wd, g.Wb, experts=exps)
            phase_moe(g, i, X, MOD, rw_d, rb_d, g.Wb, ln_g[1, :], ln_b[1, :], tiles, experts=exps, dbg=G.moe_dbg)
    S.barrier()
    for b in range(nb):
        for k in range(4):
            S.dma('sp', out[b, k * 512:(k + 1) * 512, :], X[b, LC + k * 512:LC + (k + 1) * 512, :], is_output=True)
    if debug:
        xdbg = nc.dram_tensor("xdbg", [nb, T, D], F32, kind="ExternalOutput").ap()
        for b in range(nb):
            for k in range(6):
                S.dma('sp', xdbg[b, k * 384:(k + 1) * 384, :], X[b, k * 384:(k + 1) * 384, :], is_output=True)
    S.finish()
    return nc, g


def make_in_map(inp, g, b0, nb, consts, cache=None):
    if cache is None:
        cache = {}
    m = {}
    m["x_in"] = np.ascontiguousarray(inp["x"][b0:b0 + nb])
    m["ctx_in"] = np.ascontiguousarray(inp["ctx"][b0:b0 + nb])
    m["c_all"] = np.ascontiguousarray(np.concatenate([inp["c"][b0:b0 + nb], inp["c_ctx"][None, :]], axis=0))
    full = {}
    for name in g.din:
        if name in m:
            full[name] = m[name]
            continue
        if name in cache:
            full[name] = cache[name]
            continue
        if name in consts:
            v = consts[name]
        else:
            base = name.rstrip("0123456789")
            idx = int(name[len(base):]) if len(name) > len(base) else None
            if base in ("router_w", "router_b", "hg_lb"):
                v = np.ascontiguousarray(inp[base])
            else:
                v = np.ascontiguousarray(inp[base][idx])
        cache[name] = v
        full[name] = v
    return {k: full[k] for k in g.din}


NB_PER_CORE = 2
N_CORES = 8


def kernel(**inputs):
    inputs = {k: np.asarray(v) for k, v in inputs.items()}
    nc, g = build(NB_PER_CORE, [0, 1, 2, 3], [0, 1, 2, 3])
    consts = const_inputs()
    cache = {}
    in_maps = []
    for cid in range(N_CORES):
        m = make_in_map(inputs, g, cid * NB_PER_CORE, NB_PER_CORE, consts, cache)
        in_maps.append(m)
    res = run_bass_kernel_spmd(nc, in_maps, core_ids=list(range(N_CORES)))
    out = np.concatenate([np.asarray(r["out"]) for r in res.results], axis=0)
    return np.ascontiguousarray(out.astype(np.float32, copy=False))
```

```python
import numpy as np
import concourse.bass as bass
import concourse.mybir as mybir
from concourse.bass_utils import run_bass_kernel_spmd

F32 = mybir.dt.float32
BF16 = mybir.dt.bfloat16
I32 = mybir.dt.int32
AF = mybir.ActivationFunctionType
ALU = mybir.AluOpType
AX = mybir.AxisListType

ENGS = ['pe', 'act', 'dve', 'pool', 'sp']
NDS = 24
DRAM_NAMES = set()


class Sched:
    def __init__(self, nc):
        self.nc = nc
        self.eng = dict(pe=nc.tensor, act=nc.scalar, dve=nc.vector, pool=nc.gpsimd, sp=nc.sync)
        self.sems = {}
        self.cnt = {}
        for e in ENGS:
            self.sems[e] = nc.alloc_semaphore("sem_" + e)
            self.cnt[e] = 0
        self.dslots = {}
        for q in ['sp', 'pool', 'act']:
            self.dslots[q] = []
            for i in range(NDS):
                sid = "d_%s_%d" % (q, i)
                self.sems[sid] = nc.alloc_semaphore(sid)
                self.cnt[sid] = 0
                self.dslots[q].append(sid)
        self.dnext = {q: 0 for q in self.dslots}
        self.seen = {e: {} for e in ENGS}
        self.res = {}
        self.n_inst = 0
        self.n_wait = 0
        self.out_events = []

    def _need(self, e, need, sid, val):
        if val <= 0:
            return
        if sid == e and e == 'pe':
            return
        if self.seen[e].get(sid, 0) >= val:
            return
        if need.get(sid, 0) < val:
            need[sid] = val

    def _collect(self, e, reads, writes, par=False):
        need = {}
        for k in reads:
            r = self.res.get(k)
            if r is None:
                continue
            for sid, val in r['w'].items():
                self._need(e, need, sid, val)
            if k.startswith('psb'):
                for sid, val in r['r'].items():
                    if sid != e:
                        self._need(e, need, sid, val)
        for k in writes:
            r = self.res.get(k)
            if r is None:
                continue
            if not par:
                for sid, val in r['w'].items():
                    self._need(e, need, sid, val)
            for sid, val in r['r'].items():
                self._need(e, need, sid, val)
        return need

    def _emit_waits(self, e, need):
        eng = self.eng[e]
        for sid, val in need.items():
            mult = 16 if sid.startswith('d_') else 1
            eng.wait_ge(self.sems[sid], val * mult)
            self.seen[e][sid] = val
            self.n_wait += 1

    def _record(self, ev, reads, writes, par=False):
        sid, val = ev
        for k in reads:
            r = self.res.setdefault(k, {'w': {}, 'r': {}})
            if r['r'].get(sid, 0) < val:
                r['r'][sid] = val
        for k in writes:
            if par:
                r = self.res.setdefault(k, {'w': {}, 'r': {}})
                if r['w'].get(sid, 0) < val:
                    r['w'][sid] = val
            else:
                self.res[k] = {'w': {sid: val}, 'r': {}}

    @staticmethod
    def keys_of(aps):
        ks = []
        for a in aps:
            if a is None:
                continue
            if isinstance(a, str) or isinstance(a, tuple):
                ks.append(a)
            elif hasattr(a, 'tensor'):
                ks.append(a.tensor.name)
            elif hasattr(a, 'name'):
                ks.append(a.name)
        return ks

    def op(self, e, fn, reads=(), writes=(), signal=True):
        reads = self.keys_of(reads)
        writes = self.keys_of(writes)
        need = self._collect(e, reads, writes)
        self._emit_waits(e, need)
        inst = fn(self.eng[e])
        if signal:
            self.cnt[e] += 1
            inst.then_inc(self.sems[e], 1)
            ev = (e, self.cnt[e])
        else:
            ev = (e, self.cnt[e] + 1)
        self._record(ev, reads, writes)
        self.n_inst += 1
        return inst

    def dma(self, q, out, in_, reads=None, writes=None, is_output=False, par=None, **kw):
        reads = self.keys_of([in_] if reads is None else reads)
        writes = self.keys_of([out] if writes is None else writes)
        if par is None:
            par = all(k in DRAM_NAMES for k in writes)
        slot = self.dslots[q][self.dnext[q]]
        self.dnext[q] = (self.dnext[q] + 1) % NDS
        need = self._collect(q, reads, writes, par=par)
        self._need(q, need, slot, self.cnt[slot])
        self._emit_waits(q, need)
        inst = self.eng[q].dma_start(out=out, in_=in_, **kw)
        self.cnt[slot] += 1
        inst.then_inc(self.sems[slot], 16)
        ev = (slot, self.cnt[slot])
        self._record(ev, reads, writes, par=par)
        if is_output:
            self.out_events.append(ev)
        self.n_inst += 1
        return inst

    def barrier(self):
        for e in ENGS:
            need = {}
            for sid, c in self.cnt.items():
                if c > 0:
                    self._need(e, need, sid, c)
            self._emit_waits(e, need)

    def finish(self):
        need = {}
        for sid, c in self.cnt.items():
            if c > 0 and self.seen['sp'].get(sid, 0) < c:
                need[sid] = c
        self._emit_waits('sp', need)

    def mm(self, out, lhsT, rhs, start=True, stop=True, quiet=False, **kw):
        return self.op('pe', lambda t: t.matmul(out, lhsT, rhs, start=start, stop=stop, **kw),
                       reads=[lhsT, rhs], writes=[out], signal=(bool(stop) or not quiet))

    def transpose(self, out, in_, ident):
        return self.op('pe', lambda t: t.transpose(out, in_, ident), reads=[in_, ident], writes=[out])

    def act(self, out, in_, func, scale=None, bias=None, accum_out=None, e='act'):
        kw = {}
        rd = [in_]
        if scale is not None:
            kw['scale'] = scale
            if not isinstance(scale, (int, float)):
                rd.append(scale)
        if bias is not None:
            kw['bias'] = bias
            if not isinstance(bias, (int, float)):
                rd.append(bias)
        wr = [out]
        if accum_out is not None:
            kw['accum_out'] = accum_out
            wr.append(accum_out)
        return self.op('act', lambda t: t.activation(out, in_, func, **kw), reads=rd, writes=wr)

    def tt(self, e, out, in0, in1, op):
        return self.op(e, lambda t: t.tensor_tensor(out, in0, in1, op), reads=[in0, in1], writes=[out])

    def ts(self, e, out, in0, s1, op0, s2=None, op1=None, accum_out=None):
        rd = [in0]
        if not isinstance(s1, (int, float)):
            rd.append(s1)
        if s2 is not None and not isinstance(s2, (int, float)):
            rd.append(s2)
        kw = {}
        wr = [out]
        if accum_out is not None:
            kw['accum_out'] = accum_out
            wr.append(accum_out)
        if op1 is None:
            return self.op(e, lambda t: t.tensor_scalar(out, in0, s1, None, op0, **kw), reads=rd, writes=wr)
        return self.op(e, lambda t: t.tensor_scalar(out, in0, s1, s2, op0, op1, **kw), reads=rd, writes=wr)

    def stt(self, out, in0, scalar, in1, op0, op1, e='dve'):
        rd = [in0, in1]
        if not isinstance(scalar, (int, float)):
            rd.append(scalar)
        return self.op(e, lambda t: t.scalar_tensor_tensor(out, in0, scalar, in1, op0, op1),
                       reads=rd, writes=[out])

    def copy(self, e, out, in_):
        if e == 'act':
            return self.act(out, in_, AF.Copy)
        return self.op(e, lambda t: t.tensor_copy(out, in_), reads=[in_], writes=[out])

    def memset(self, e, ap, val):
        return self.op(e, lambda t: t.memset(ap, val), reads=[], writes=[ap])
import math
import numpy as np

D = 2048
LC = 256
L = 2048
T = LC + L
NT = T // 128
DEPTH = 4
ALPHA = (2.0 * DEPTH) ** 0.25
EPS = 1e-6
NMOD = 6


class G:
    moe_dbg = 0


def uname(g, base):
    g.uid += 1
    return "%s_%d" % (base, g.uid)


def sb(g, base, shape, dt):
    return g.nc.alloc_sbuf_tensor(uname(g, base), list(shape), dt)


class Phase:
    def __init__(self, g):
        self.g = g

    def __enter__(self):
        g = self.g
        g.S.barrier()
        self.stack = []
        g.phase_stack.append(self)
        return self

    def alloc(self, base, shape, dt):
        g = self.g
        cm = g.nc.sbuf_tensor(uname(g, base), list(shape), dt)
        t = cm.__enter__()
        self.stack.append(cm)
        return t

    def __exit__(self, *a):
        g = self.g
        g.S.barrier()
        for cm in reversed(self.stack):
            cm.__exit__(None, None, None)
        g.phase_stack.pop()
        return False


def init_globals(nc, nb):
    g = G()
    g.nc = nc
    g.S = Sched(nc)
    g.uid = 0
    g.nb = nb
    g.phase_stack = []
    g.ps = [nc.alloc_psum_tensor("psb%d" % i, [128, 512], F32) for i in range(8)]
    g.din = {}
    return g


def dram_in(g, name, shape, dt=F32):
    if name not in g.din:
        g.din[name] = g.nc.dram_tensor(name, list(shape), dt, kind="ExternalInput").ap()
    return g.din[name]


def dram_scratch(g, name, shape, dt=F32):
    DRAM_NAMES.add(name)
    return g.nc.dram_tensor(name, list(shape), dt, kind="Internal").ap()


def load_consts(g):
    nc, S = g.nc, g.S
    c = {}
    ident_d = dram_in(g, "c_ident", [128, 128])
    c['ident'] = nc.alloc_sbuf_tensor("ident", [128, 128], F32)
    S.dma('sp', c['ident'][:], ident_d[:, :])
    tri_d = dram_in(g, "c_triu", [128, 128])
    c['triu'] = nc.alloc_sbuf_tensor("triu", [128, 128], F32)
    S.dma('sp', c['triu'][:], tri_d[:, :])
    tris_d = dram_in(g, "c_trius", [128, 128])
    c['trius'] = nc.alloc_sbuf_tensor("trius", [128, 128], F32)
    S.dma('sp', c['trius'][:], tris_d[:, :])
    c['ones_b'] = nc.alloc_sbuf_tensor("ones_b", [128, 128], BF16)
    S.memset('dve', c['ones_b'][:], 1.0)
    c['ones_f'] = nc.alloc_sbuf_tensor("ones_f", [128, 128], F32)
    S.memset('dve', c['ones_f'][:], 1.0)
    g.c = c
    return c


def phase_mod(g, i, c_all, ada_w, ada_b, MOD):
    nc, S = g.nc, g.S
    R = g.nb + 1
    NW = NMOD * D
    with Phase(g) as ph:
        cT = ph.alloc("cT", [128, 16, R], F32)
        sT = ph.alloc("sT", [128, 16, R], BF16)
        for r in range(R):
            S.dma('sp', cT[:, :, r], c_all[r, :].rearrange("(kc p) -> p kc", p=128),
                  allow_slow_non_contiguous=True)
        S.act(sT[:], cT[:], AF.Silu)
        bias = ph.alloc("mbias", [R, NW], F32)
        S.dma('sp', bias[:], ada_b[None, :].broadcast_to([R, NW]))
        modt = ph.alloc("modt", [R, NW], F32)
        wb = [ph.alloc("mw%d" % k, [128, 16, 512], BF16) for k in range(2)]
        wv = ada_w.rearrange("(kc p) n -> p kc n", p=128)
        for n in range(NW // 512):
            w = wb[n % 2]
            for kc in range(16):
                S.dma('pool', w[:, kc, :], wv[:, kc, n * 512:(n + 1) * 512], par=True)
            ps = g.ps[n % 2]
            for kc in range(16):
                S.mm(ps[0:R, :], sT[:, kc, :], w[:, kc, :], start=(kc == 0), stop=(kc == 15), quiet=True)
            S.tt('dve', modt[:, n * 512:(n + 1) * 512], ps[0:R, :], bias[:, n * 512:(n + 1) * 512], ALU.add)
        for m in (1, 4):
            S.ts('dve', modt[:, m * D:(m + 1) * D], modt[:, m * D:(m + 1) * D], 1.0, ALU.add)
        S.dma('sp', MOD[:, :], modt[:])


def load_mod_cols(g, ph, MOD, r, m_scale, m_shift):
    S = g.S
    sc = ph.alloc("msc", [128, 16], F32)
    sh = ph.alloc("msh", [128, 16], F32)
    S.dma('sp', sc[:], MOD[r, m_scale * D:(m_scale + 1) * D].rearrange("(kc p) -> p kc", p=128),
          allow_slow_non_contiguous=True)
    S.dma('sp', sh[:], MOD[r, m_shift * D:(m_shift + 1) * D].rearrange("(kc p) -> p kc", p=128),
          allow_slow_non_contiguous=True)
    return sc, sh


def load_row_bcast(g, ph, name, row_ap, n=D):
    t = ph.alloc(name, [128, n], F32)
    g.S.dma('sp', t[:], row_ap[None, :].broadcast_to([128, n]))
    return t


def build_hT_tile(g, xt, hT_dst, sc, sh, psbase=0, hT32=None):
    S, c = g.S, g.c
    for gb in range(4):
        ps = g.ps[psbase + gb]
        for j in range(4):
            kc = gb * 4 + j
            S.transpose(ps[:, j * 128:(j + 1) * 128], xt[:, kc * 128:(kc + 1) * 128], c['ident'][:])
        for j in range(4):
            kc = gb * 4 + j
            src = ps[:, j * 128:(j + 1) * 128]
            dsts = [hT_dst] + ([hT32] if hT32 is not None else [])
            for dst in dsts:
                if gb % 2 == 0:
                    S.act(dst[:, kc, :], src, AF.Identity, scale=sc[:, kc:kc + 1], bias=sh[:, kc:kc + 1])
                else:
                    S.ts('dve', dst[:, kc, :], src, sc[:, kc:kc + 1], ALU.mult, sh[:, kc:kc + 1], ALU.add)


def load_w_bf16(g, wsb, w_dram, ncols, col0=0, chunk=1024):
    S = g.S
    K = w_dram.shape[0]
    wv = w_dram.rearrange("(kc p) n -> p kc n", p=128)
    for kc in range(K // 128):
        for c0 in range(0, ncols, chunk):
            cw = min(chunk, ncols - c0)
            S.dma('pool', wsb[:, kc, c0:c0 + cw], wv[:, kc, col0 + c0:col0 + c0 + cw], par=True)


def mod_row(b, t, nb):
    return nb if t < 2 else b


def resid_ln_tile(g, ph, bufs, ychunks, X_rows, gate_t, lng_t, lnb_t, q='sp'):
    S, nc = g.S, g.nc
    xt, zt, st, mv, rs = bufs
    S.dma(q, xt[:], X_rows)
    for dc in range(4):
        sl = slice(dc * 512, (dc + 1) * 512)
        S.tt('dve', zt[:, sl], ychunks[dc], gate_t[:, sl], ALU.mult)
    S.stt(zt[:], xt[:], ALPHA, zt[:], ALU.mult, ALU.add, e='dve')
    for dc in range(4):
        S.op('dve', lambda t, dc=dc: t.bn_stats(st[:, dc * 6:(dc + 1) * 6], zt[:, dc * 512:(dc + 1) * 512]),
             reads=[zt], writes=[st])
    S.op('dve', lambda t: t.bn_aggr(mv[:], st[:]), reads=[st], writes=[mv])
    S.ts('dve', rs[:, 0:1], mv[:, 1:2], EPS, ALU.add)
    S.act(rs[:, 0:1], rs[:, 0:1], AF.Sqrt)
    S.op('dve', lambda t: t.reciprocal(rs[:, 0:1], rs[:, 0:1]), reads=[rs], writes=[rs])
    S.ts('dve', zt[:], zt[:], mv[:, 0:1], ALU.subtract, rs[:, 0:1], ALU.mult)
    S.tt('pool', zt[:], zt[:], lng_t[:], ALU.mult)
    S.tt('pool', xt[:], zt[:], lnb_t[:], ALU.add)
    S.dma(q, X_rows, xt[:])


def alloc_ln_bufs(ph, k=""):
    xt = ph.alloc("lx" + k, [128, D], F32)
    zt = ph.alloc("lz" + k, [128, D], F32)
    st = ph.alloc("lst" + k, [128, 24], F32)
    mv = ph.alloc("lmv" + k, [128, 2], F32)
    rs = ph.alloc("lrs" + k, [128, 2], F32)
    return (xt, zt, st, mv, rs)

HD = 128
HQ = 16
HKV = 4
ATT_SCALE = HD ** -0.5


def phase_proj_tm(g, i, X, MOD, m_scale, m_shift, w_dram, ncols, P, tiles_per_b):
    S, nb = g.S, g.nb
    with Phase(g) as ph:
        wsb = ph.alloc("pw", [128, 16, ncols], BF16)
        load_w_bf16(g, wsb, w_dram, ncols)
        hT = [ph.alloc("phT%d" % k, [128, 16, 128], BF16) for k in range(2)]
        xt = [ph.alloc("pxt%d" % k, [128, D], F32) for k in range(2)]
        pt = [ph.alloc("ppt%d" % k, [128, ncols], F32) for k in range(2)]
        mods = {}
        for r in range(nb + 1):
            mods[r] = load_mod_cols(g, ph, MOD, r, m_scale, m_shift)
        n = 0
        for b in range(nb):
            for t in tiles_per_b:
                sc, sh = mods[mod_row(b, t, nb)]
                x_ = xt[n % 2]
                h_ = hT[n % 2]
                p_ = pt[n % 2]
                S.dma('sp', x_[:], X[b, t * 128:(t + 1) * 128, :])
                build_hT_tile(g, x_, h_, sc, sh, psbase=0)
                for c in range(ncols // 512):
                    ps = g.ps[4 + (c % 4)]
                    for kc in range(16):
                        S.mm(ps[:], h_[:, kc, :], wsb[:, kc, c * 512:(c + 1) * 512], start=(kc == 0), stop=(kc == 15), quiet=True)
                    if c % 2 == 0:
                        S.act(p_[:, c * 512:(c + 1) * 512], ps[:], AF.Copy)
                    else:
                        S.copy('dve', p_[:, c * 512:(c + 1) * 512], ps[:])
                S.dma('sp', P[b, t * 128:(t + 1) * 128, 0:ncols], p_[:])
                n += 1


def rms_heads(g, tmp, x3, nh, gb, sq, red):
    S = g.S
    S.tt('pool', sq[:, 0:nh, :], x3, x3, ALU.mult)
    S.op('dve', lambda t: t.tensor_reduce(red[:, 0:nh], sq[:, 0:nh, :], AX.X, ALU.add), reads=[sq], writes=[red])
    S.ts('dve', red[:, 0:nh], red[:, 0:nh], 1.0 / HD, ALU.mult, EPS, ALU.add)
    S.act(red[:, 0:nh], red[:, 0:nh], AF.Sqrt)
    S.op('dve', lambda t: t.reciprocal(red[:, 0:nh], red[:, 0:nh]), reads=[red], writes=[red])
    S.tt('dve', x3, x3, red[:, 0:nh].unsqueeze(2).broadcast_to([128, nh, HD]), ALU.mult)
    S.tt('pool', x3, x3, gb[:].unsqueeze(1).broadcast_to([128, nh, HD]), ALU.mult)


def rope_heads(g, x3, out3, nh, cs, sn, t1, t2):
    S = g.S
    xv = x3[:, 0:nh * HD].rearrange("p (h a s f) -> p h a s f", h=nh, a=2, s=2, f=32)
    ov = out3[:, 0:nh * HD].rearrange("p (h a s f) -> p h a s f", h=nh, a=2, s=2, f=32)
    csb = cs[:].rearrange("p (a f) -> p a f", a=2)
    snb = sn[:].rearrange("p (a f) -> p a f", a=2)
    t1v = t1[:, 0:nh * 64].rearrange("p (h a f) -> p h a f", h=nh, a=2, f=32)
    t2v = t2[:, 0:nh * 64].rearrange("p (h a f) -> p h a f", h=nh, a=2, f=32)
    x1 = xv[:, :, :, 0, :]
    x2 = xv[:, :, :, 1, :]
    csb4 = csb.unsqueeze(1).broadcast_to([128, nh, 2, 32])
    snb4 = snb.unsqueeze(1).broadcast_to([128, nh, 2, 32])
    S.tt('dve', t1v, x1, csb4, ALU.mult)
    S.tt('pool', t2v, x2, snb4, ALU.mult)
    S.tt('dve', ov[:, :, :, 0, :], t1v, t2v, ALU.subtract)
    S.tt('pool', t1v, x2, csb4, ALU.mult)
    S.tt('dve', t2v, x1, snb4, ALU.mult)
    S.tt('pool', ov[:, :, :, 1, :], t1v, t2v, ALU.add)


def phase_attn_core(g, i, X, MOD, P, wout_d, qg_d, kg_d, lng_d, lnb_d, cos_d, sin_d, last):
    S, nb, c = g.S, g.nb, g.c
    with Phase(g) as ph:
        wout = ph.alloc("awo", [128, 16, D], BF16)
        load_w_bf16(g, wout, wout_d, D)
        qgb = load_row_bcast(g, ph, "qgb", qg_d, HD)
        kgb = load_row_bcast(g, ph, "kgb", kg_d, HD)
        lng = load_row_bcast(g, ph, "lng", lng_d)
        lnb = load_row_bcast(g, ph, "lnb", lnb_d)
        gate_c = load_row_bcast(g, ph, "gatec", MOD[nb, 2 * D:3 * D])
        gate_l = ph.alloc("gatel", [128, D], F32)
        KT = ph.alloc("KT", [128, HKV, T], BF16)
        Vs = ph.alloc("Vs", [128, NT, HKV * HD], BF16)
        kv = ph.alloc("kv", [128, 2 * HKV * HD], F32)
        kr = ph.alloc("kr", [128, HKV * HD], F32)
        qx = ph.alloc("qx", [128, D], F32)
        qr = ph.alloc("qr", [128, D], F32)
        sq = ph.alloc("sq", [128, HQ, HD], F32)
        red = ph.alloc("red", [128, HQ], F32)
        t1 = ph.alloc("rt1", [128, HQ * 64], F32)
        t2 = ph.alloc("rt2", [128, HQ * 64], F32)
        cs = ph.alloc("cs", [128, 64], F32)
        sn = ph.alloc("sn", [128, 64], F32)
        QT = ph.alloc("QT", [128, HQ, 128], BF16)
        OT = ph.alloc("OT", [128, HQ, 128], BF16)
        PT = [ph.alloc("PT%d" % k, [128, 512], BF16) for k in range(3)]
        rec = ph.alloc("rec", [128, 512], F32)
        lnbufs = alloc_ln_bufs(ph)
        for b in range(nb):
            S.dma('sp', gate_l[:], MOD[b, 2 * D:3 * D][None, :].broadcast_to([128, D]))
            for t in range(NT):
                S.dma('sp', kv[:], P[b, t * 128:(t + 1) * 128, HQ * HD:HQ * HD + 2 * HKV * HD])
                k3 = kv[:, 0:HKV * HD].rearrange("p (h d) -> p h d", h=HKV)
                rms_heads(g, None, k3, HKV, kgb, sq, red)
                ksrc = kv
                if t >= 2:
                    S.dma('sp', cs[:], cos_d[(t - 2) * 128:(t - 1) * 128, :])
                    S.dma('sp', sn[:], sin_d[(t - 2) * 128:(t - 1) * 128, :])
                    rope_heads(g, kv, kr, HKV, cs, sn, t1, t2)
                    ksrc = kr
                ps = g.ps[t % 2]
                for h in range(HKV):
                    S.transpose(ps[:, h * 128:(h + 1) * 128], ksrc[:, h * HD:(h + 1) * HD], c['ident'][:])
                S.act(KT[:, :, t * 128:(t + 1) * 128], ps[:].rearrange("p (h d) -> p h d", h=HKV), AF.Copy)
                S.copy('dve', Vs[:, t, :], kv[:, HKV * HD:2 * HKV * HD])
            qtiles = list(range(2, NT)) if last else list(range(NT))
            for t in qtiles:
                is_ctx = t < 2
                S.dma('sp', qx[:], P[b, t * 128:(t + 1) * 128, 0:HQ * HD])
                q3 = qx[:].rearrange("p (h d) -> p h d", h=HQ)
                rms_heads(g, None, q3, HQ, qgb, sq, red)
                qsrc = qx
                if not is_ctx:
                    S.dma('sp', cs[:], cos_d[(t - 2) * 128:(t - 1) * 128, :])
                    S.dma('sp', sn[:], sin_d[(t - 2) * 128:(t - 1) * 128, :])
                    rope_heads(g, qx, qr, HQ, cs, sn, t1, t2)
                    qsrc = qr
                for gb in range(4):
                    ps = g.ps[gb % 2]
                    for j in range(4):
                        h = gb * 4 + j
                        S.transpose(ps[:, j * 128:(j + 1) * 128], qsrc[:, h * HD:(h + 1) * HD], c['ident'][:])
                    S.act(QT[:, gb * 4:(gb + 1) * 4, :], ps[:].rearrange("p (h d) -> p h d", h=4), AF.Copy)
                stiles = [0, 1] if is_ctx else list(range(NT))
                for kvh in range(HKV):
                    o_ps = g.ps[2]
                    d_ps = g.ps[3]
                    for si, s in enumerate(stiles):
                        s_ps = g.ps[4 + (si % 3)]
                        S.mm(s_ps[:], KT[:, kvh, s * 128:(s + 1) * 128], QT[:, kvh * 4:(kvh + 1) * 4, :])
                        p_ = PT[si % 3]
                        S.act(p_[:], s_ps[:], AF.Exp, scale=ATT_SCALE)
                        S.mm(o_ps[:], Vs[:, s, kvh * HD:(kvh + 1) * HD], p_[:], start=(si == 0), stop=(si == len(stiles) - 1))
                        S.mm(d_ps[:], c['ones_b'][:], p_[:], start=(si == 0), stop=(si == len(stiles) - 1))
                    S.op('dve', lambda tt_: tt_.reciprocal(rec[:], d_ps[:]), reads=[d_ps], writes=[rec])
                    S.tt('dve', OT[:, kvh * 4:(kvh + 1) * 4, :], o_ps[:].rearrange("p (h d) -> p h d", h=4),
                         rec[:].rearrange("p (h d) -> p h d", h=4), ALU.mult)
                ych = []
                for dc in range(4):
                    ps = g.ps[4 + dc] if False else g.ps[(dc % 2) + 0] if False else None
                banks = [0, 1, 7, 3]
                for dc in range(4):
                    ps = g.ps[banks[dc]]
                    for h in range(HQ):
                        S.mm(ps[:], OT[:, h, :], wout[:, h, dc * 512:(dc + 1) * 512], start=(h == 0), stop=(h == HQ - 1), quiet=True)
                    ych.append(ps[:])
                resid_ln_tile(g, ph, lnbufs, ych, X[b, t * 128:(t + 1) * 128, :],
                              gate_c if is_ctx else gate_l, lng, lnb)

NE = 16
DE = 1024
TG = 4
NQ = 4
FQ = DE // NQ
NFC = FQ // 128


PIECE = 16 * 2 * FQ + NFC * D


def phase_moe_precast(g, wg_d, wu_d, wd_d, Wb, experts=range(NE)):
    S = g.S
    with Phase(g) as ph:
        st = [ph.alloc("pcw%d" % k, [128, PIECE], BF16) for k in range(3)]
        n = 0
        for e in experts:
            wgv = wg_d[e].rearrange("(kc p) f -> p kc f", p=128)
            wuv = wu_d[e].rearrange("(kc p) f -> p kc f", p=128)
            wdv = wd_d[e].rearrange("(fc p) d -> p fc d", p=128)
            for hf in range(NQ):
                s_ = st[n % 3]
                n += 1
                wb = s_[:, 0:16 * 2 * FQ].rearrange("p (kc two f) -> p kc two f", kc=16, two=2)
                wdb = s_[:, 16 * 2 * FQ:PIECE].rearrange("p (fc d) -> p fc d", fc=NFC)
                for k4 in range(0, 16, 4):
                    S.dma('pool', wb[:, k4:k4 + 4, 0, :], wgv[:, k4:k4 + 4, hf * FQ:(hf + 1) * FQ], par=True)
                    S.dma('pool', wb[:, k4:k4 + 4, 1, :], wuv[:, k4:k4 + 4, hf * FQ:(hf + 1) * FQ], par=True)
                S.dma('pool', wdb[:, :, :], wdv[:, hf * NFC:(hf + 1) * NFC, :], par=True)
                S.dma('sp' if n % 2 == 0 else 'act', Wb[e * NQ + hf, :, :], s_[:])


def gating_tile(g, bufs, logits_ps, rbb, Gdst):
    S = g.S
    sc, sel, w = bufs
    S.act(sc[:], logits_ps, AF.Sigmoid)
    S.tt('dve', sel[:], sc[:], rbb[:], ALU.add)
    s4 = sel[:].rearrange("p (g e) -> p g e", g=4)
    x0, x1, x2, x3 = s4[:, :, 0], s4[:, :, 1], s4[:, :, 2], s4[:, :, 3]
    M1, m1, M2, m2 = w[:, 0:4], w[:, 4:8], w[:, 8:12], w[:, 12:16]
    A, Bm, Cm, s2, gs = w[:, 16:20], w[:, 20:24], w[:, 24:28], w[:, 28:32], w[:, 32:36]
    gmax, gmask, den = w[:, 36:37], w[:, 40:44], w[:, 44:45]
    S.tt('dve', M1, x0, x1, ALU.max)
    S.tt('dve', m1, x0, x1, ALU.min)
    S.tt('dve', M2, x2, x3, ALU.max)
    S.tt('dve', m2, x2, x3, ALU.min)
    S.tt('dve', A, M1, M2, ALU.max)
    S.tt('dve', Bm, M1, M2, ALU.min)
    S.tt('dve', Cm, m1, m2, ALU.max)
    S.tt('dve', s2, Bm, Cm, ALU.max)
    S.tt('dve', gs, A, s2, ALU.add)
    S.op('dve', lambda t: t.tensor_reduce(gmax, gs, AX.X, ALU.max), reads=[w], writes=[w])
    S.ts('dve', gmask, gs, gmax, ALU.is_ge)
    em = w[:, 48:64].rearrange("p (g e) -> p g e", g=4)
    S.tt('dve', em, s4, s2.unsqueeze(2).broadcast_to([128, 4, 4]), ALU.is_ge)
    S.tt('dve', em, em, gmask.unsqueeze(2).broadcast_to([128, 4, 4]), ALU.mult)
    S.tt('dve', w[:, 48:64], w[:, 48:64], sc[:], ALU.mult)
    S.op('dve', lambda t: t.tensor_reduce(den, w[:, 48:64], AX.X, ALU.add), reads=[w], writes=[w])
    S.op('dve', lambda t: t.reciprocal(den, den), reads=[w], writes=[w])
    S.ts('dve', Gdst, w[:, 48:64], den, ALU.mult)


def phase_moe(g, i, X, MOD, rw_d, rb_d, Wb, lng_d, lnb_d, tiles_per_b, experts=range(NE), dbg=0):
    S, nb, c = g.S, g.nb, g.c
    toks = [(b, t) for b in range(nb) for t in tiles_per_b]
    groups = [toks[k:k + TG] for k in range(0, len(toks), TG)]
    with Phase(g) as ph:
        rw32 = ph.alloc("rw32", [128, 16, NE], F32)
        S.dma('sp', rw32[:], rw_d.rearrange("(kc p) e -> p kc e", p=128))
        rbb = load_row_bcast(g, ph, "rbb", rb_d, NE)
        lng = load_row_bcast(g, ph, "mlng", lng_d)
        lnb = load_row_bcast(g, ph, "mlnb", lnb_d)
        gate_t = ph.alloc("mgate", [128, D], F32)
        mods = {}
        for r in range(nb + 1):
            mods[r] = load_mod_cols(g, ph, MOD, r, 4, 3)
        h2T = ph.alloc("h2T", [128, 16, TG * 128], BF16)
        h32 = ph.alloc("h32", [128, 16, 128], F32)
        Gt = ph.alloc("Gt", [128, TG, NE], F32)
        gb_ = (ph.alloc("gsc", [128, 16], F32), ph.alloc("gsel", [128, 16], F32), ph.alloc("gw", [128, 64], F32))
        acc = ph.alloc("acc", [128, TG, D], F32)
        actT = ph.alloc("actT", [128, 8, TG * 128], BF16)
        wst = [ph.alloc("wst%d" % k, [128, PIECE], BF16) for k in range(2)]
        sg = [ph.alloc("sg%d" % k, [128, TG * 128], F32) for k in range(2)]
        lnbufs = alloc_ln_bufs(ph)
        xt = lnbufs[0]
        nw = 0
        for grp in groups:
            ng = len(grp)
            ntok = ng * 128
            for ti, (b, t) in enumerate(grp):
                scm, shm = mods[mod_row(b, t, nb)]
                S.dma('sp', xt[:], X[b, t * 128:(t + 1) * 128, :])
                build_hT_tile(g, xt, h2T[:, :, ti * 128:(ti + 1) * 128], scm, shm, psbase=0, hT32=h32)
                lp = g.ps[4]
                for kc in range(16):
                    S.mm(lp[:, 0:NE], h32[:, kc, :], rw32[:, kc, :], start=(kc == 0), stop=(kc == 15), quiet=True)
                if dbg != 2:
                    gating_tile(g, gb_, lp[:, 0:NE], rbb, Gt[:, ti, :])
            first = True
            for e in experts:
                for hf in range(NQ):
                    w_ = wst[nw % 2]
                    wb = w_[:, 0:16 * 2 * FQ].rearrange("p (kc two f) -> p kc two f", kc=16, two=2)
                    wdb = w_[:, 16 * 2 * FQ:PIECE].rearrange("p (fc d) -> p fc d", fc=NFC)
                    nw += 1
                    S.dma('sp', w_[0:64, :], Wb[e * NQ + hf, 0:64, :], par=True)
                    S.dma('act', w_[64:128, :], Wb[e * NQ + hf, 64:128, :], par=True)
                    for fc in range(NFC):
                        g_ps = g.ps[(fc % 2) * 2]
                        u_ps = g.ps[(fc % 2) * 2 + 1]
                        for kc in range(16):
                            S.mm(g_ps[:, 0:ntok], wb[:, kc, 0, fc * 128:(fc + 1) * 128], h2T[:, kc, 0:ntok],
                                 start=(kc == 0), stop=(kc == 15), quiet=True)
                        for kc in range(16):
                            S.mm(u_ps[:, 0:ntok], wb[:, kc, 1, fc * 128:(fc + 1) * 128], h2T[:, kc, 0:ntok],
                                 start=(kc == 0), stop=(kc == 15), quiet=True)
                        s_ = sg[fc % 2]
                        S.act(s_[:, 0:ntok], g_ps[:, 0:ntok], AF.Silu)
                        S.tt('dve', actT[:, hf * NFC + fc, 0:ntok], s_[:, 0:ntok], u_ps[:, 0:ntok], ALU.mult)
                    for ti in range(ng):
                        for dc in range(4):
                            y_ps = g.ps[4 + dc]
                            for fc in range(NFC):
                                S.mm(y_ps[:], actT[:, hf * NFC + fc, ti * 128:(ti + 1) * 128],
                                     wdb[:, fc, dc * 512:(dc + 1) * 512], start=(fc == 0), stop=(fc == NFC - 1), quiet=True)
                            a_ = acc[:, ti, dc * 512:(dc + 1) * 512]
                            if first:
                                S.ts('dve', a_, y_ps[:], Gt[:, ti, e:e + 1], ALU.mult)
                            else:
                                S.stt(a_, y_ps[:], Gt[:, ti, e:e + 1], a_, ALU.mult, ALU.add)
                    first = False
            if dbg == 1:
                continue
            for ti, (b, t) in enumerate(grp):
                r = mod_row(b, t, nb)
                S.dma('sp', gate_t[:], MOD[r, 5 * D:6 * D][None, :].broadcast_to([128, D]))
                ych = [acc[:, ti, dc * 512:(dc + 1) * 512] for dc in range(4)]
                resid_ln_tile(g, ph, lnbufs, ych, X[b, t * 128:(t + 1) * 128, :], gate_t, lng, lnb)

PW = 12416
PROWS = 2312


def prow(t):
    return 2 + t * 128 if t < 2 else 262 + (t - 2) * 128


def phase_proj_blocks(g, X, MOD, m_scale, m_shift, w_dram, ntot, P, tiles_per_b, blk=3072):
    S, nb = g.S, g.nb
    for c0 in range(0, ntot, blk):
        ncols = min(blk, ntot - c0)
        with Phase(g) as ph:
            wsb = ph.alloc("pw", [128, 16, ncols], BF16)
            load_w_bf16(g, wsb, w_dram, ncols, col0=c0)
            hT = [ph.alloc("phT%d" % k, [128, 16, 128], BF16) for k in range(2)]
            xt = [ph.alloc("pxt%d" % k, [128, D], F32) for k in range(2)]
            pt = [ph.alloc("ppt%d" % k, [128, ncols], F32) for k in range(2)]
            mods = {}
            for r in range(nb + 1):
                mods[r] = load_mod_cols(g, ph, MOD, r, m_scale, m_shift)
            n = 0
            for b in range(nb):
                for t in tiles_per_b:
                    sc, sh = mods[mod_row(b, t, nb)]
                    x_ = xt[n % 2]
                    h_ = hT[n % 2]
                    p_ = pt[n % 2]
                    S.dma('sp', x_[:], X[b, t * 128:(t + 1) * 128, :])
                    build_hT_tile(g, x_, h_, sc, sh, psbase=0)
                    ci = 0
                    for cc in range(0, ncols, 512):
                        cw = min(512, ncols - cc)
                        ps = g.ps[4 + (ci % 4)]
                        for kc in range(16):
                            S.mm(ps[:, 0:cw], h_[:, kc, :], wsb[:, kc, cc:cc + cw], start=(kc == 0), stop=(kc == 15), quiet=True)
                        if ci % 2 == 0:
                            S.act(p_[:, cc:cc + cw], ps[:, 0:cw], AF.Copy)
                        else:
                            S.copy('dve', p_[:, cc:cc + cw], ps[:, 0:cw])
                        ci += 1
                    r0 = prow(t)
                    S.dma('sp', P[b, r0:r0 + 128, c0:c0 + ncols], p_[:, 0:ncols])
                    n += 1


def phase_readout(g, i, X, MOD, O, K, P, gate_col0, gate_func, ng_d, w_out_d, lng_d, lnb_d, last, Ypart):
    S, nb, c = g.S, g.nb, g.c
    nparts = K // 2048
    tiles = list(range(2, NT)) if last else list(range(NT))
    for part in range(nparts):
        with Phase(g) as ph:
            wsb = ph.alloc("rw", [128, 16, D], BF16)
            load_w_bf16(g, wsb, w_out_d[part * 2048:(part + 1) * 2048, :], D)
            ngb = load_row_bcast(g, ph, "ngb", ng_d, 128)
            oa = ph.alloc("roa", [128, D], F32)
            ob = ph.alloc("rob", [128, D], F32)
            sq = ph.alloc("rsq", [128, 16, 128], F32)
            red = ph.alloc("rred", [128, 16], F32)
            OT = ph.alloc("rOT", [128, 16, 128], BF16)
            one16 = ph.alloc("one16", [128, 16], F32)
            zero16 = ph.alloc("zero16", [128, 16], F32)
            S.memset('dve', one16[:], 1.0)
            S.memset('dve', zero16[:], 0.0)
            fin = part == nparts - 1
            if fin:
                lng = load_row_bcast(g, ph, "rlng", lng_d)
                lnb = load_row_bcast(g, ph, "rlnb", lnb_d)
                gate_c = load_row_bcast(g, ph, "rgc", MOD[nb, 2 * D:3 * D])
                gate_l = ph.alloc("rgl", [128, D], F32)
                lnbufs = alloc_ln_bufs(ph)
            if nparts > 1:
                ysb = ph.alloc("rys", [128, D], F32)
            for b in range(nb):
                if fin:
                    S.dma('sp', gate_l[:], MOD[b, 2 * D:3 * D][None, :].broadcast_to([128, D]))
                for t in tiles:
                    rows = slice(t * 128, (t + 1) * 128)
                    cols = slice(part * 2048, (part + 1) * 2048)
                    S.dma('sp', oa[:], O[0, b, rows, cols])
                    S.dma('sp', ob[:], O[1, b, rows, cols])
                    S.tt('dve', oa[:], oa[:], ob[:], ALU.add)
                    o3 = oa[:].rearrange("p (h d) -> p h d", h=16)
                    rms_heads(g, None, o3, 16, ngb, sq, red)
                    r0 = prow(t)
                    S.dma('sp', ob[:], P[b, r0:r0 + 128, gate_col0 + part * 2048:gate_col0 + (part + 1) * 2048])
                    S.act(ob[:], ob[:], gate_func)
                    S.tt('pool', oa[:], oa[:], ob[:], ALU.mult)
                    build_hT_tile(g, oa, OT, one16, zero16, psbase=0)
                    ych = []
                    for dc in range(4):
                        ps = g.ps[4 + dc]
                        for kc in range(16):
                            S.mm(ps[:], OT[:, kc, :], wsb[:, kc, dc * 512:(dc + 1) * 512], start=(kc == 0), stop=(kc == 15), quiet=True)
                        ych.append(ps[:])
                    if nparts > 1 and not fin:
                        for dc in range(4):
                            if dc % 2 == 0:
                                S.act(ysb[:, dc * 512:(dc + 1) * 512], ych[dc], AF.Copy)
                            else:
                                S.copy('dve', ysb[:, dc * 512:(dc + 1) * 512], ych[dc])
                        S.dma('sp', Ypart[b, rows, :], ysb[:])
                        continue
                    if nparts > 1:
                        S.dma('sp', ysb[:], Ypart[b, rows, :])
                        for dc in range(4):
                            S.tt('dve', ysb[:, dc * 512:(dc + 1) * 512], ysb[:, dc * 512:(dc + 1) * 512], ych[dc], ALU.add)
                        ych = [ysb[:, dc * 512:(dc + 1) * 512] for dc in range(4)]
                    resid_ln_tile(g, ph, lnbufs, ych, X[b, rows, :], gate_c if t < 2 else gate_l, lng, lnb)


def hg_consts():
    p = np.arange(128)
    same = (p[:, None] // 64) == (p[None, :] // 64)
    A_f = (same & (p[:, None] <= p[None, :])).astype(np.float32)
    B_f = (same & (p[:, None] > p[None, :])).astype(np.float32)
    hgA = np.stack([A_f, A_f.T]).astype(np.float32)
    hgB = np.stack([B_f, B_f.T]).astype(np.float32)
    ind = np.stack([(p < 64), (p >= 64)], axis=1).astype(np.float32)
    return {"c_hgA": hgA, "c_hgB": hgB, "c_ind": ind}


def phase_hg_scan(g, i, P, O, hg_lb):
    S, nb, c = g.S, g.nb, g.c
    with Phase(g) as ph:
        Am = [ph.alloc("hgA%d" % d, [128, 128], F32) for d in range(2)]
        Bm = [ph.alloc("hgB%d" % d, [128, 128], F32) for d in range(2)]
        hgA_d = dram_in(g, "c_hgA", [2, 128, 128])
        hgB_d = dram_in(g, "c_hgB", [2, 128, 128])
        ind_d = dram_in(g, "c_ind", [128, 2])
        for d in range(2):
            S.dma('sp', Am[d][:], hgA_d[d, :, :])
            S.dma('sp', Bm[d][:], hgB_d[d, :, :])
        ind = ph.alloc("hgind", [128, 2], F32)
        S.dma('sp', ind[:], ind_d[:, :])
        lb = [ph.alloc("hlb%d" % d, [128, D], F32) for d in range(2)]
        oml = [ph.alloc("homl%d" % d, [128, D], F32) for d in range(2)]
        W = [ph.alloc("hw%d" % k, [128, D], F32) for k in range(8)]
        qb, vb, fb, kk, kinv, kd0, kd1, exb = W
        kd = [kd0, kd1]
        for d in range(2):
            tmp, den, num = W[0], W[1], W[2]
            S.memset('dve', num[:], 0.0)
            for l in range(DEPTH):
                S.dma('sp', tmp[:], hg_lb[d, l, :][None, :].broadcast_to([128, D]))
                S.act(tmp[:], tmp[:], AF.Exp)
                if l == 0:
                    S.copy('dve', den[:], tmp[:])
                else:
                    S.tt('dve', den[:], den[:], tmp[:], ALU.add)
                    if l <= i:
                        S.tt('dve', num[:], num[:], tmp[:], ALU.add)
            S.op('dve', lambda t_, den=den: t_.reciprocal(den[:], den[:]), reads=[den], writes=[den])
            S.tt('dve', lb[d][:], num[:], den[:], ALU.mult)
            S.ts('dve', oml[d][:], lb[d][:], -1.0, ALU.mult, 1.0, ALU.add)
        Ssb = ph.alloc("hS", [128, 16, 128], F32)
        Fsb = ph.alloc("hF", [128, 32], F32)
        qT = ph.alloc("hqT", [128, 16, 128], F32)
        kT = ph.alloc("hkT", [128, 16, 128], F32)
        itr = ph.alloc("hitr", [128, 16, 128], F32)
        osb = [ph.alloc("hosb%d" % k, [64, D], F32) for k in range(2)]
        for b in range(nb):
            for d in range(2):
                order = list(range(NT)) if d == 0 else [1, 0] + list(range(NT - 1, 1, -1))
                S.memset('dve', Ssb[:], 0.0)
                for t in order:
                    r0 = prow(t)
                    S.dma('sp', qb[:], P[b, r0:r0 + 128, 0:2048])
                    S.dma('sp', vb[:], P[b, r0:r0 + 128, 2048:4096])
                    S.dma('sp', fb[:], P[b, r0:r0 + 128, 6144 + d * 2048:6144 + (d + 1) * 2048])
                    S.act(fb[:], fb[:], AF.Sigmoid)
                    S.tt('pool', fb[:], fb[:], oml[d][:], ALU.mult)
                    S.tt('dve', fb[:], fb[:], lb[d][:], ALU.add)
                    S.ts('dve', kk[:], fb[:], -1.0, ALU.mult, 1.0, ALU.add)
                    S.act(fb[:], fb[:], AF.Ln)
                    tp = g.ps[2]
                    for h in range(16):
                        S.mm(tp[:, h * 2:(h + 1) * 2], fb[:, h * 128:(h + 1) * 128], ind[:, :])
                    S.act(Fsb[:], tp[:, 0:32], AF.Exp)
                    for cc in range(4):
                        sl = slice(cc * 512, (cc + 1) * 512)
                        gp = g.ps[0]
                        sp_ = g.ps[1]
                        S.mm(gp[:], Am[d][:], fb[:, sl])
                        S.mm(sp_[:], Bm[d][:], fb[:, sl])
                        S.act(exb[:, sl], gp[:], AF.Exp)
                        S.tt('dve', qb[:, sl], qb[:, sl], exb[:, sl], ALU.mult)
                        S.act(exb[:, sl], gp[:], AF.Exp, scale=-1.0)
                        S.tt('pool', kinv[:, sl], kk[:, sl], exb[:, sl], ALU.mult)
                        S.act(exb[:, sl], sp_[:], AF.Exp)
                        S.stt(kd0[:, sl], exb[:, sl], ind[:, 0:1], kk[:, sl], ALU.mult, ALU.mult)
                        S.stt(kd1[:, sl], exb[:, sl], ind[:, 1:2], kk[:, sl], ALU.mult, ALU.mult)
                    for src, dst, bk in ((qb, qT, 3), (kinv, kT, 4)):
                        for gb in range(4):
                            ps = g.ps[bk]
                            for j in range(4):
                                h = gb * 4 + j
                                S.transpose(ps[:, j * 128:(j + 1) * 128], src[:, h * 128:(h + 1) * 128], c['ident'][:])
                            pv = ps[:].rearrange("p (h d) -> p h d", h=4)
                            if bk == 3:
                                S.act(dst[:, gb * 4:(gb + 1) * 4, :], pv, AF.Copy)
                            else:
                                S.copy('dve', dst[:, gb * 4:(gb + 1) * 4, :], pv)
                    for gb in range(4):
                        ps = g.ps[5 + (gb % 2)]
                        for j in range(4):
                            h = gb * 4 + j
                            S.mm(ps[:, j * 128:(j + 1) * 128], kT[:, h, :], qT[:, h, :])
                        S.tt('dve', itr[:, gb * 4:(gb + 1) * 4, :], ps[:].rearrange("p (h d) -> p h d", h=4),
                             Am[d][:].unsqueeze(1).broadcast_to([128, 4, 128]), ALU.mult)
                    for cidx in ((0, 1) if d == 0 else (1, 0)):
                        csl = slice(cidx * 64, (cidx + 1) * 64)
                        for gb in range(4):
                            ps = g.ps[5 + (gb % 2)]
                            for j in range(4):
                                h = gb * 4 + j
                                S.mm(ps[0:64, j * 128:(j + 1) * 128], itr[:, h, csl], vb[:, h * 128:(h + 1) * 128],
                                     start=True, stop=False)
                                S.mm(ps[0:64, j * 128:(j + 1) * 128], qT[:, h, csl], Ssb[:, h, :],
                                     start=False, stop=True)
                            if gb % 2 == 0:
                                S.act(osb[cidx][:, gb * 512:(gb + 1) * 512], ps[0:64, :], AF.Copy)
                            else:
                                S.copy('dve', osb[cidx][:, gb * 512:(gb + 1) * 512], ps[0:64, :])
                        for gb in range(4):
                            ps = g.ps[7 if gb % 2 == 0 else 0]
                            for j in range(4):
                                h = gb * 4 + j
                                S.mm(ps[:, j * 128:(j + 1) * 128], kd[cidx][:, h * 128:(h + 1) * 128],
                                     vb[:, h * 128:(h + 1) * 128])
                            for j in range(4):
                                h = gb * 4 + j
                                S.stt(Ssb[:, h, :], Ssb[:, h, :], Fsb[:, h * 2 + cidx:h * 2 + cidx + 1],
                                      ps[:, j * 128:(j + 1) * 128], ALU.mult, ALU.add)
                        S.dma('sp', O[d, b, t * 128 + cidx * 64:t * 128 + (cidx + 1) * 64, 0:2048], osb[cidx][:])


def phase_hgrn2(g, i, j, X, MOD, lng_d, lnb_d, last):
    w_in = dram_in(g, "hg_w_in%d" % j, [D, 10240])
    w_out = dram_in(g, "hg_w_out%d" % j, [D, D])
    hg_lb = dram_in(g, "hg_lb", [2, DEPTH, D])
    ng_d = dram_in(g, "hg_norm_g%d" % j, [128])
    phase_proj_blocks(g, X, MOD, 1, 0, w_in, 10240, g.P, list(range(NT)))
    phase_hg_scan(g, i, g.P, g.O, hg_lb)
    phase_readout(g, i, X, MOD, g.O, 2048, g.P, 4096, AF.Sigmoid, ng_d, w_out, lng_d, lnb_d, last, g.Ypart)

DNH = 32
NEGBIG = -30000.0


def dn_consts():
    p = np.arange(128)
    A_f = (p[:, None] <= p[None, :]).astype(np.float32)
    dnA = np.stack([A_f, A_f.T]).astype(np.float32)
    nms_f = np.where(p[None, :] < p[:, None], 0.0, NEGBIG).astype(np.float32)
    nms_b = np.where(p[None, :] > p[:, None], 0.0, NEGBIG).astype(np.float32)
    nmt_f = np.where(p[:, None] <= p[None, :], 0.0, NEGBIG).astype(np.float32)
    nmt_b = np.where(p[:, None] >= p[None, :], 0.0, NEGBIG).astype(np.float32)
    nm2 = np.stack([np.concatenate([nms_f, nmt_f], axis=1), np.concatenate([nms_b, nmt_b], axis=1)]).astype(np.float32)
    return {"c_dnA": dnA, "c_dnNM2": nm2}


def phase_dn_prep(g, j, P, QKV, GB, conv_d, alog_d, dtb_d):
    S, nb, c = g.S, g.nb, g.c
    with Phase(g) as ph:
        zt = ph.alloc("dz", [4, 8192], F32)
        S.memset('dve', zt[:], 0.0)
        for b in range(nb):
            S.dma('sp', P[b, 0:2, 0:8192], zt[0:2, :])
            S.dma('sp', P[b, 258:262, 0:8192], zt[0:4, :])
            S.dma('sp', P[b, 2310:2312, 0:8192], zt[0:2, :])
        wt = ph.alloc("cw", [128, 5, 2048], F32)
        xs = [ph.alloc("cx%d" % k, [128, 2048], F32) for k in range(3)]
        acc = [ph.alloc("cacc%d" % k, [128, 2048], F32) for k in range(2)]
        tmp = ph.alloc("ctmp", [128, 2048], F32)
        sq = ph.alloc("csq", [128, 16, 128], F32)
        red = ph.alloc("cred", [128, 16], F32)
        nega = ph.alloc("nega", [128, 64], F32)
        dtb = ph.alloc("dtb", [128, 64], F32)
        S.dma('sp', nega[:], alog_d.rearrange("a h -> (a h)")[None, :].broadcast_to([128, 64]))
        S.dma('sp', dtb[:], dtb_d.rearrange("a h -> (a h)")[None, :].broadcast_to([128, 64]))
        S.act(nega[:], nega[:], AF.Exp)
        S.ts('dve', nega[:], nega[:], -1.0, ALU.mult)
        ba = [ph.alloc("ba%d" % k, [128, 128], F32) for k in range(2)]
        n = 0
        for b in range(nb):
            for t in range(NT):
                r0 = prow(t)
                bt = ba[n % 2]
                n += 1
                S.dma('sp', bt[:], P[b, r0:r0 + 128, 12288:12416])
                S.act(bt[:, 0:64], bt[:, 0:64], AF.Sigmoid)
                S.tt('dve', bt[:, 64:128], bt[:, 64:128], dtb[:], ALU.add)
                S.act(bt[:, 64:128], bt[:, 64:128], AF.Exp)
                S.ts('dve', bt[:, 64:128], bt[:, 64:128], 1.0, ALU.add)
                S.act(bt[:, 64:128], bt[:, 64:128], AF.Ln)
                S.tt('dve', bt[:, 64:128], bt[:, 64:128], nega[:], ALU.mult)
                S.dma('sp', GB[b, t * 128:(t + 1) * 128, :], bt[:])
        n = 0
        for cb in range(4):
            for jt in range(5):
                S.dma('sp', wt[:, jt, :], conv_d[jt, cb * 2048:(cb + 1) * 2048][None, :].broadcast_to([128, 2048]))
            for b in range(nb):
                for t in range(NT):
                    r0 = prow(t)
                    a_ = acc[n % 2]
                    n += 1
                    for jt in range(5):
                        x_ = xs[jt % 3]
                        S.dma('sp' if jt % 2 == 0 else 'act', x_[:], P[b, r0 + jt - 2:r0 + jt - 2 + 128, cb * 2048:(cb + 1) * 2048])
                        if jt == 0:
                            S.tt('dve', a_[:], x_[:], wt[:, jt, :], ALU.mult)
                        else:
                            S.tt('pool', tmp[:], x_[:], wt[:, jt, :], ALU.mult)
                            S.tt('dve', a_[:], a_[:], tmp[:], ALU.add)
                    S.act(a_[:], a_[:], AF.Silu)
                    if cb < 2:
                        a3 = a_[:].rearrange("p (h d) -> p h d", h=16)
                        S.tt('pool', sq[:], a3, a3, ALU.mult)
                        S.op('dve', lambda t_: t_.tensor_reduce(red[:], sq[:], AX.X, ALU.add), reads=[sq], writes=[red])
                        S.ts('dve', red[:], red[:], EPS, ALU.add)
                        S.act(red[:], red[:], AF.Sqrt)
                        S.op('dve', lambda t_: t_.reciprocal(red[:], red[:]), reads=[red], writes=[red])
                        if cb == 0:
                            S.ts('dve', red[:], red[:], 128.0 ** -0.5, ALU.mult)
                        S.tt('dve', a3, a3, red[:].unsqueeze(2).broadcast_to([128, 16, 128]), ALU.mult)
                    S.dma('sp', QKV[b, t * 128:(t + 1) * 128, cb * 2048:(cb + 1) * 2048], a_[:])


def phase_dn_scan(g, QKV, GB, O):
    S, nb, c = g.S, g.nb, g.c
    with Phase(g) as ph:
        dnA_d = dram_in(g, "c_dnA", [2, 128, 128])
        nm2_d = dram_in(g, "c_dnNM2", [2, 128, 256])
        Am = [ph.alloc("dnA%d" % d, [128, 128], F32) for d in range(2)]
        NM2 = [ph.alloc("dnNM%d" % d, [128, 256], F32) for d in range(2)]
        for d in range(2):
            S.dma('sp', Am[d][:], dnA_d[d, :, :])
            S.dma('sp', NM2[d][:], nm2_d[d, :, :])
        ones = c['ones_f']
        negones = ph.alloc("negones", [128, 128], F32)
        S.memset('dve', negones[:], -1.0)
        ident = c['ident']
        qn = ph.alloc("dqn", [128, 2048], F32)
        kn = ph.alloc("dkn", [128, 2048], F32)
        vv = ph.alloc("dvv", [128, 4096], F32)
        gbt = ph.alloc("dgb", [128, 128], F32)
        ot = ph.alloc("dot", [128, 4096], F32)
        Sst = [ph.alloc("dS%d" % h, [128, 128], F32) for h in range(DNH)]
        hq = [ph.alloc("dhq%d" % k, [128, 4, 128], F32) for k in range(2)]
        cb = []
        for k in range(4):
            cb.append(dict(
                G1=ph.alloc("G1_%d" % k, [128, 128], F32),
                E2=ph.alloc("E2_%d" % k, [128, 256], F32),
                sm=ph.alloc("sm_%d" % k, [128, 2], F32),
                sx=ph.alloc("sx_%d" % k, [128, 3], F32),
                M=ph.alloc("M_%d" % k, [128, 128], F32),
                qk=ph.alloc("qk_%d" % k, [128, 128], F32),
                Q1=ph.alloc("Q1_%d" % k, [128, 128], F32),
                R=[ph.alloc("R%d_%d" % (r, k), [128, 256], F32) for r in range(2)],
                PQ=[ph.alloc("PQ%d_%d" % (r, k), [128, 256], F32) for r in range(2)],
                wT=ph.alloc("wT_%d" % k, [128, 128], F32),
                kd=ph.alloc("kd_%d" % k, [128, 128], F32),
                vn=ph.alloc("vn_%d" % k, [128, 128], F32),
                banks=(g.ps[2 * k], g.ps[2 * k + 1]),
            ))

        def chain(d, h, hv, B, hqs):
            bx, by = B['banks']
            kT, qT, KK, QKt = hqs[:, 0, :], hqs[:, 1, :], hqs[:, 2, :], hqs[:, 3, :]
            gcol = gbt[:, 64 + d * 32 + hv:64 + d * 32 + hv + 1]
            bcol = gbt[:, d * 32 + hv:d * 32 + hv + 1]
            G1, E2, sm, sx, M, qk, Q1 = B['G1'], B['E2'], B['sm'], B['sx'], B['M'], B['qk'], B['Q1']
            S.ts('dve', G1[:], Am[d][:], gcol, ALU.mult)
            yield
            S.mm(bx[:, 0:128], G1[:], ones[:], start=True, stop=False)
            S.mm(bx[:, 0:128], negones[:], G1[:], start=False, stop=True)
            S.mm(bx[:, 128:256], ones[:], G1[:], start=True, stop=False)
            S.mm(bx[:, 128:256], G1[:], negones[:], start=False, stop=True)
            S.mm(bx[:, 256:257], G1[:], ones[:, 0:1])
            S.mm(bx[:, 257:258], ones[:], gcol)
            yield
            S.tt('dve', E2[:], bx[:, 0:256], NM2[d][:], ALU.add)
            S.copy('dve', sm[:], bx[:, 256:258])
            yield
            S.act(E2[:], E2[:], AF.Exp)
            S.act(sx[:, 0:1], sm[:, 0:1], AF.Exp)
            S.act(sx[:, 1:2], sm[:, 0:1], AF.Exp, scale=-1.0, bias=sm[:, 1:2])
            S.act(sx[:, 2:3], sm[:, 1:2], AF.Exp)
            yield
            egc, ekd, gl = sx[:, 0:1], sx[:, 1:2], sx[:, 2:3]
            S.stt(M[:], KK, bcol, E2[:, 0:128], ALU.mult, ALU.mult)
            S.tt('pool', qk[:], QKt, E2[:, 128:256], ALU.mult)
            R = B['R'][0]
            S.ts('dve', R[:, 0:128], vv[:, hv * 128:(hv + 1) * 128], bcol, ALU.mult)
            S.ts('dve', R[:, 128:256], kn[:, h * 128:(h + 1) * 128], bcol, ALU.mult, egc, ALU.mult)
            S.ts('pool', B['kd'][:], kn[:, h * 128:(h + 1) * 128], ekd, ALU.mult)
            yield
            S.transpose(bx[:, 0:128], M[:], ident[:])
            yield
            S.copy('dve', Q1[:], bx[:, 0:128])
            yield
            Pk, Qk = M[:], Q1[:]
            ri = 0
            for lev in range(7):
                R = B['R'][ri]
                Rn = B['R'][1 - ri]
                S.mm(by[:, 0:256], Qk, R[:])
                if lev < 6:
                    S.mm(by[:, 256:384], Pk, Qk)
                    if lev < 5:
                        S.mm(by[:, 384:512], Qk, Pk)
                yield
                S.tt('dve', Rn[:], R[:], by[:, 0:256], ALU.subtract if lev == 0 else ALU.add)
                if lev < 6:
                    PQ = B['PQ'][lev % 2]
                    if lev < 5:
                        S.act(PQ[:], by[:, 256:512], AF.Copy)
                    else:
                        S.act(PQ[:, 0:128], by[:, 256:384], AF.Copy)
                    Qk, Pk = PQ[:, 0:128], PQ[:, 128:256]
                ri = 1 - ri
                yield
            R = B['R'][ri]
            u, w = R[:, 0:128], R[:, 128:256]
            S.transpose(bx[:, 0:128], w, ident[:])
            yield
            S.copy('dve', B['wT'][:], bx[:, 0:128])
            yield
            St = Sst[hv]
            S.mm(by[:, 0:128], B['wT'][:], St[:])
            S.mm(by[:, 128:256], qT, St[:])
            yield
            S.tt('dve', B['vn'][:], u, by[:, 0:128], ALU.subtract)
            yield
            S.mm(by[:, 256:384], qk[:], B['vn'][:])
            S.mm(by[:, 384:512], B['kd'][:], B['vn'][:])
            yield
            S.ts('dve', ot[:, hv * 128:(hv + 1) * 128], by[:, 128:256], egc, ALU.mult)
            S.tt('dve', ot[:, hv * 128:(hv + 1) * 128], ot[:, hv * 128:(hv + 1) * 128], by[:, 256:384], ALU.add)
            S.stt(St[:], St[:], gl, by[:, 384:512], ALU.mult, ALU.add)
            yield

        for b in range(nb):
            for d in range(2):
                order = list(range(NT)) if d == 0 else [1, 0] + list(range(NT - 1, 1, -1))
                for h in range(DNH):
                    S.memset('pool', Sst[h][:], 0.0)
                for t in order:
                    rows = slice(t * 128, (t + 1) * 128)
                    S.dma('sp', qn[:], QKV[b, rows, 0:2048])
                    S.dma('act', kn[:], QKV[b, rows, 2048:4096])
                    S.dma('sp', vv[:], QKV[b, rows, 4096:8192])
                    S.dma('act', gbt[:], GB[b, rows, :])
                    for hh in range(0, 16, 2):
                        gens = []
                        for k2 in range(2):
                            h = hh + k2
                            hqs = hq[k2]
                            pa = cb[2 * k2]['banks'][0]
                            pb = cb[2 * k2 + 1]['banks'][0]
                            S.transpose(pa[:, 0:128], kn[:, h * 128:(h + 1) * 128], ident[:])
                            S.transpose(pa[:, 128:256], qn[:, h * 128:(h + 1) * 128], ident[:])
                            S.act(hqs[:, 0:2, :], pa[:, 0:256].rearrange("p (a d) -> p a d", a=2), AF.Copy)
                            S.mm(pb[:, 0:128], hqs[:, 0, :], hqs[:, 0, :])
                            S.mm(pb[:, 128:256], hqs[:, 0, :], hqs[:, 1, :])
                            S.act(hqs[:, 2:4, :], pb[:, 0:256].rearrange("p (a d) -> p a d", a=2), AF.Copy)
                            gens += [chain(d, h, 2 * h + k, cb[2 * k2 + k], hqs) for k in range(2)]
                        while gens:
                            for gen in list(gens):
                                try:
                                    next(gen)
                                except StopIteration:
                                    gens.remove(gen)
                    S.dma('sp', O[d, b, rows, :], ot[:])


def phase_deltanet(g, i, j, X, MOD, lng_d, lnb_d, last):
    w_in = dram_in(g, "dn_w_in%d" % j, [D, 12416])
    w_out = dram_in(g, "dn_w_out%d" % j, [4096, D])
    conv_d = dram_in(g, "dn_conv%d" % j, [5, 8192])
    alog_d = dram_in(g, "dn_a_log%d" % j, [2, 32])
    dtb_d = dram_in(g, "dn_dt_bias%d" % j, [2, 32])
    ng_d = dram_in(g, "dn_norm_g%d" % j, [128])
    phase_proj_blocks(g, X, MOD, 1, 0, w_in, 12416, g.P, list(range(NT)))
    phase_dn_prep(g, j, g.P, g.QKV, g.GB, conv_d, alog_d, dtb_d)
    phase_dn_scan(g, g.QKV, g.GB, g.O)
    phase_readout(g, i, X, MOD, g.O, 4096, g.P, 8192, AF.Silu, ng_d, w_out, lng_d, lnb_d, last, g.Ypart)

GRID_W = 64
ROPE_FREQS = 32
ROPE_BASE = 10000.0


def rope_tables():
    row = np.repeat(np.arange(L // GRID_W), GRID_W).astype(np.float32)
    col = (np.arange(L) % GRID_W).astype(np.float32)
    inv = (ROPE_BASE ** (-np.arange(ROPE_FREQS, dtype=np.float32) / ROPE_FREQS)).astype(np.float32)
    ang = np.stack([row[:, None] * inv, col[:, None] * inv], axis=1).astype(np.float32)
    return np.cos(ang).reshape(L, 64).astype(np.float32), np.sin(ang).reshape(L, 64).astype(np.float32)


def const_inputs():
    jj, ii = np.meshgrid(np.arange(128), np.arange(128), indexing='ij')
    cos, sin = rope_tables()
    return {
        "c_ident": np.eye(128, dtype=np.float32),
        "c_triu": (jj <= ii).astype(np.float32),
        "c_trius": (jj < ii).astype(np.float32),
        "c_cos": cos, "c_sin": sin,
        **hg_consts(), **dn_consts(),
    }


def build(nb, layers, moe_layers, debug=False, moe_experts=None, skip_mixer=False):
    nc = bass.Bass("TRN2", target_bir_lowering=False)
    g = init_globals(nc, nb)
    S = g.S
    load_consts(g)
    x_in = dram_in(g, "x_in", [nb, L, D])
    ctx_in = dram_in(g, "ctx_in", [nb, LC, D])
    c_all = dram_in(g, "c_all", [nb + 1, D])
    cos_d = dram_in(g, "c_cos", [L, 64])
    sin_d = dram_in(g, "c_sin", [L, 64])
    X = dram_scratch(g, "Xs", [nb, T, D])
    P = dram_scratch(g, "Ps", [nb, PROWS, PW])
    g.P = P
    g.O = dram_scratch(g, "Os", [2, nb, T, 4096])
    g.Ypart = dram_scratch(g, "Yp", [nb, T, D])
    g.QKV = dram_scratch(g, "QKVs", [nb, T, 8192])
    g.GB = dram_scratch(g, "GBs", [nb, T, 128])
    g.Wb = dram_scratch(g, "Wbs", [16 * NQ, 128, PIECE], BF16)
    out = nc.dram_tensor("out", [nb, L, D], F32, kind="ExternalOutput").ap()
    DRAM_NAMES.add("out")
    DRAM_NAMES.add("xdbg")
    for b in range(nb):
        S.dma('sp', X[b, 0:LC, :], ctx_in[b, :, :])
        for k in range(4):
            S.dma('sp', X[b, LC + k * 512:LC + (k + 1) * 512, :], x_in[b, k * 512:(k + 1) * 512, :])
    rw_d = dram_in(g, "router_w", [D, 16])
    rb_d = dram_in(g, "router_b", [16])
    for i in layers:
        last = i == DEPTH - 1
        MOD = dram_scratch(g, "MOD%d" % i, [nb + 1, NMOD * D])
        ada_w = dram_in(g, "ada_w%d" % i, [D, NMOD * D])
        ada_b = dram_in(g, "ada_b%d" % i, [NMOD * D])
        ln_g = dram_in(g, "ln_g%d" % i, [2, D])
        ln_b = dram_in(g, "ln_b%d" % i, [2, D])
        phase_mod(g, i, c_all, ada_w, ada_b, MOD)
        kind = i % 3
        j = i // 3
        if skip_mixer:
            pass
        elif kind == 0:
            w_in = dram_in(g, "attn_w_in%d" % j, [D, 3072])
            w_out = dram_in(g, "attn_w_out%d" % j, [D, D])
            qg = dram_in(g, "attn_q_g%d" % j, [128])
            kg = dram_in(g, "attn_k_g%d" % j, [128])
            phase_proj_tm(g, i, X, MOD, 1, 0, w_in, 3072, P, list(range(NT)))
            phase_attn_core(g, i, X, MOD, P, w_out, qg, kg, ln_g[0, :], ln_b[0, :], cos_d, sin_d, last)
        elif kind == 1:
            phase_deltanet(g, i, j, X, MOD, ln_g[0, :], ln_b[0, :], last)
        else:
            phase_hgrn2(g, i, j, X, MOD, ln_g[0, :], ln_b[0, :], last)
        if i in moe_layers:
            wg = wu = wd = None
            if moe_experts is None or len(moe_experts) > 0:
                wg = dram_in(g, "moe_w_gate%d" % i, [16, D, 1024])
                wu = dram_in(g, "moe_w_up%d" % i, [16, D, 1024])
                wd = dram_in(g, "moe_w_down%d" % i, [16, 1024, D])
            tiles = list(range(2, NT)) if last else list(range(NT))
            exps = moe_experts if moe_experts is not None else range(16)
            if wg is not None:
                phase_moe_precast(g, wg, wu, wd, g.Wb, experts=exps)
            phase_moe(g, i, X, MOD, rw_d, rb_d, g.Wb, ln_g[1, :], ln_b[1, :], tiles, experts=exps, dbg=G.moe_dbg)
    S.barrier()
    for b in range(nb):
        for k in range(4):
            S.dma('sp', out[b, k * 512:(k + 1) * 512, :], X[b, LC + k * 512:LC + (k + 1) * 512, :], is_output=True)
    if debug:
        xdbg = nc.dram_tensor("xdbg", [nb, T, D], F32, kind="ExternalOutput").ap()
        for b in range(nb):
            for k in range(6):
                S.dma('sp', xdbg[b, k * 384:(k + 1) * 384, :], X[b, k * 384:(k + 1) * 384, :], is_output=True)
    S.finish()
    return nc, g


def make_in_map(inp, g, b0, nb, consts, cache=None):
    if cache is None:
        cache = {}
    m = {}
    m["x_in"] = np.ascontiguousarray(inp["x"][b0:b0 + nb])
    m["ctx_in"] = np.ascontiguousarray(inp["ctx"][b0:b0 + nb])
    m["c_all"] = np.ascontiguousarray(np.concatenate([inp["c"][b0:b0 + nb], inp["c_ctx"][None, :]], axis=0))
    full = {}
    for name in g.din:
        if name in m:
            full[name] = m[name]
            continue
        if name in cache:
            full[name] = cache[name]
            continue
        if name in consts:
            v = consts[name]
        else:
            base = name.rstrip("0123456789")
            idx = int(name[len(base):]) if len(name) > len(base) else None
            if base in ("router_w", "router_b", "hg_lb"):
                v = np.ascontiguousarray(inp[base])
            else:
                v = np.ascontiguousarray(inp[base][idx])
        cache[name] = v
        full[name] = v
    return {k: full[k] for k in g.din}


NB_PER_CORE = 2
N_CORES = 8


def kernel(**inputs):
    inputs = {k: np.asarray(v) for k, v in inputs.items()}
    nc, g = build(NB_PER_CORE, [0, 1, 2, 3], [0, 1, 2, 3])
    consts = const_inputs()
    cache = {}
    in_maps = []
    for cid in range(N_CORES):
        m = make_in_map(inputs, g, cid * NB_PER_CORE, NB_PER_CORE, consts, cache)
        in_maps.append(m)
    res = run_bass_kernel_spmd(nc, in_maps, core_ids=list(range(N_CORES)))
    out = np.concatenate([np.asarray(r["out"]) for r in res.results], axis=0)
    return np.ascontiguousarray(out.astype(np.float32, copy=False))
```
